# Optimizing a Trainium2 kernel written in Bass

```python
import math
import jax, jax.numpy as jnp
from jax import lax
import numpy as np

D_MODEL = 2048
BATCH = 2
SEQ = 8192
DEPTH = 4

CHUNK = 128
NORM_EPS = 1e-6
RET_HEADS = 4
RET_HEAD_DIM = 128
RET_WIDTH = RET_HEADS * RET_HEAD_DIM
RET_GN_EPS = 1e-6
ROPE_BASE = 10000.0
SSM_HEADS = 16
SSM_HEAD_DIM = 64
SSM_WIDTH = SSM_HEADS * SSM_HEAD_DIM
SSM_GROUPS = 2
SSM_STATE = 128
SSM_CONV = 4
SSM_CONV_DIM = SSM_WIDTH + 2 * SSM_GROUPS * SSM_STATE
SSM_NORM_EPS = 1e-5
RWKV_HEADS = 8
RWKV_HEAD_DIM = 64
RWKV_WIDTH = RWKV_HEADS * RWKV_HEAD_DIM
RWKV_DECAY_RANK = 96
RWKV_AAA_RANK = 96
RWKV_MV_RANK = 64
RWKV_GATE_RANK = 256
RWKV_LN_EPS = 64e-5
D_MIX = RET_WIDTH + SSM_WIDTH + RWKV_WIDTH
RET_COLS = 4 * RET_WIDTH
SSM_COLS = SSM_WIDTH + SSM_CONV_DIM + SSM_HEADS
RWKV_COLS = 3 * RWKV_WIDTH + RWKV_DECAY_RANK + RWKV_AAA_RANK + RWKV_GATE_RANK
SSM_OFF = RET_COLS
RWKV_OFF = RET_COLS + SSM_COLS
IN_COLS = RET_COLS + SSM_COLS + RWKV_COLS
D_FF = ((8 * D_MODEL + 3 * 256 - 1) // (3 * 256)) * 256

kernel_name = 'hymba_style_retention_rwkv7_mamba2_hybrid'


def rms_norm(x, w, eps=NORM_EPS):
    xf = x.astype(jnp.float32)
    y = xf * lax.rsqrt(jnp.mean(xf * xf, axis=-1, keepdims=True) + eps)
    return (y * w.astype(jnp.float32)).astype(x.dtype)


def head_norm(y, eps):
    yc = y - jnp.mean(y, axis=-1, keepdims=True)
    return yc * lax.rsqrt(jnp.mean(yc * yc, axis=-1, keepdims=True) + eps)


def token_shift(x):
    return jnp.pad(x, ((0, 0), (1, 0), (0, 0)))[:, :-1]


def rotary(x, pos):
    half = x.shape[-1] // 2
    inv_freq = ROPE_BASE ** (-jnp.arange(half, dtype=jnp.float32) / half)
    ang = pos.astype(jnp.float32)[:, None] * inv_freq[None, :]
    cos = jnp.cos(ang)[None, :, None, :]
    sin = jnp.sin(ang)[None, :, None, :]
    x1, x2 = x[..., :half], x[..., half:]
    return jnp.concatenate([x1 * cos - x2 * sin, x1 * sin + x2 * cos], axis=-1)


def chunk_state_scan(contrib, decay):
    def step(s, inp):
        c, d = inp
        return s * d + c, s
    _, prev = lax.scan(step, jnp.zeros_like(contrib[0]), (contrib, decay))
    return prev


def retention_mixer(q, k, v, g):
    Bsz, S, _ = q.shape
    H, Dh, C = RET_HEADS, RET_HEAD_DIM, CHUNK
    N = S // C
    shp = (Bsz, S, H, Dh)
    pos = jnp.arange(S)
    q = rotary(q.reshape(shp), pos)
    k = rotary(k.reshape(shp), pos) * (Dh ** -0.5)
    v = v.reshape(shp)
    log_gamma = jnp.log1p(-jnp.exp2(-5.0 - jnp.arange(H, dtype=jnp.float32)))
    idx = jnp.arange(C, dtype=jnp.float32)
    rel = idx[:, None] - idx[None, :]
    decay_in = jnp.where(rel >= 0, jnp.exp(jnp.maximum(rel, 0.0)[None] * log_gamma[:, None, None]), 0.0)
    qc, kc, vc = (t.reshape(Bsz, N, C, H, Dh) for t in (q, k, v))
    scores = jnp.einsum('bnihd,bnjhd->bnhij', qc, kc) * decay_in
    inner = jnp.einsum('bnhij,bnjhe->bnihe', scores, vc)
    zeta = jnp.exp((C - 1.0 - idx)[None, :] * log_gamma[:, None])
    contrib = jnp.einsum('bnjhd,hj,bnjhe->nbhde', kc, zeta, vc)
    chunk_decay = jnp.broadcast_to(jnp.exp(C * log_gamma)[None, None, :, None, None], (N, 1, H, 1, 1))
    prev = chunk_state_scan(contrib, chunk_decay)
    xi = jnp.exp((idx + 1.0)[None, :] * log_gamma[:, None])
    cross = jnp.einsum('bnihd,hi,nbhde->bnihe', qc, xi, prev)
    y = head_norm((inner + cross).reshape(shp), RET_GN_EPS)
    return y.reshape(Bsz, S, RET_WIDTH) * jax.nn.silu(g)


def ssd_chunked(xs, dt, A, Bm, Cm):
    Bsz, S, H, P = xs.shape
    G, Nst, L = SSM_GROUPS, SSM_STATE, CHUNK
    J = H // G
    Nc = S // L
    x = (xs * dt[..., None]).reshape(Bsz, Nc, L, G, J, P)
    a = (dt * A).reshape(Bsz, Nc, L, G, J).transpose(0, 3, 4, 1, 2)
    Bc = Bm.reshape(Bsz, Nc, L, G, Nst)
    Cc = Cm.reshape(Bsz, Nc, L, G, Nst)
    a_cum = jnp.cumsum(a, axis=-1)
    seg = a_cum[..., :, None] - a_cum[..., None, :]
    causal = jnp.tril(jnp.ones((L, L), dtype=bool))
    decay_in = jnp.exp(jnp.where(causal, seg, -jnp.inf))
    cb = jnp.einsum('bclgn,bcsgn->bgcls', Cc, Bc)
    y_diag = jnp.einsum('bgjcls,bcsgjp->bclgjp', cb[:, :, None] * decay_in, x)
    decay_to_end = jnp.exp(a_cum[..., -1:] - a_cum).transpose(0, 3, 4, 1, 2)
    states = jnp.einsum('bclgn,bclgjp->cbgjpn', Bc, x * decay_to_end[..., None])
    chunk_decay = jnp.exp(a_cum[..., -1]).transpose(3, 0, 1, 2)[..., None, None]
    prev = chunk_state_scan(states, chunk_decay)
    decay_from_start = jnp.exp(a_cum).transpose(0, 3, 4, 1, 2)
    y_off = jnp.einsum('bclgn,cbgjpn->bclgjp', Cc, prev) * decay_from_start[..., None]
    return (y_diag + y_off).reshape(Bsz, S, H, P)


def mamba2_mixer(z, xbc, dt_raw, conv_w, conv_b, dt_bias, a_log, d_skip, norm_w):
    Bsz, S, _ = z.shape
    GN = SSM_GROUPS * SSM_STATE
    xbc = lax.conv_general_dilated(
        xbc, conv_w.astype(xbc.dtype)[:, None, :], window_strides=(1,),
        padding=((SSM_CONV - 1, 0),), dimension_numbers=('NWC', 'WIO', 'NWC'),
        feature_group_count=SSM_CONV_DIM) + conv_b
    xbc = jax.nn.silu(xbc)
    xs = xbc[..., :SSM_WIDTH].reshape(Bsz, S, SSM_HEADS, SSM_HEAD_DIM)
    Bm = xbc[..., SSM_WIDTH:SSM_WIDTH + GN].reshape(Bsz, S, SSM_GROUPS, SSM_STATE)
    Cm = xbc[..., SSM_WIDTH + GN:].reshape(Bsz, S, SSM_GROUPS, SSM_STATE)
    dt = jax.nn.softplus(dt_raw + dt_bias)
    A = -jnp.exp(a_log.astype(jnp.float32))
    y = ssd_chunked(xs, dt, A, Bm, Cm) + xs * d_skip[:, None]
    y = (y.reshape(Bsz, S, SSM_WIDTH) * jax.nn.silu(z)).reshape(Bsz, S, SSM_GROUPS, SSM_WIDTH // SSM_GROUPS)
    y = y * lax.rsqrt(jnp.mean(y * y, axis=-1, keepdims=True) + SSM_NORM_EPS)
    return y.reshape(Bsz, S, SSM_WIDTH) * norm_w


def rwkv7_scan(r, w, k, v, kk, a):
    def step(state, inp):
        r_t, w_t, k_t, v_t, kk_t, a_t = inp
        sa = jnp.einsum('bhvk,bhk->bhv', state, -kk_t)
        state = (state * w_t[:, :, None, :] + sa[..., None] * (kk_t * a_t)[:, :, None, :]
                 + v_t[..., None] * k_t[:, :, None, :])
        return state, jnp.einsum('bhvk,bhk->bhv', state, r_t)
    Bsz, _, H, D = r.shape
    xs = tuple(jnp.moveaxis(t, 1, 0) for t in (r, w, k, v, kk, a))
    s0 = jnp.zeros((Bsz, H, D, D), dtype=r.dtype)
    _, y = lax.scan(step, s0, xs)
    return jnp.moveaxis(y, 0, 1)


def rwkv7_mixer(p, mu, w0, w2, a0, a2, g2, k_k, k_a, r_k, ln_w, ln_b, v_res=None):
    Bsz, S, _ = p.shape
    W = RWKV_WIDTH
    hd = (Bsz, S, RWKV_HEADS, RWKV_HEAD_DIM)
    p = p + (token_shift(p) - p) * mu
    r, k, v = p[..., :W], p[..., W:2 * W], p[..., 2 * W:3 * W]
    o1 = 3 * W + RWKV_DECAY_RANK
    o2 = o1 + RWKV_AAA_RANK
    xw, xa, xg = p[..., 3 * W:o1], p[..., o1:o2], p[..., o2:]
    w_log = -jax.nn.softplus(-(w0 + jnp.tanh(xw) @ w2)) - 0.5
    decay = jnp.exp(-jnp.exp(w_log))
    if v_res is not None:
        pv, mu_v, v0, v2, v_first = v_res
        pv = pv + (token_shift(pv) - pv) * mu_v
        v = v + (v_first - v) * jax.nn.sigmoid(v0 + pv @ v2)
    a = jax.nn.sigmoid(a0 + xa @ a2)
    g = jax.nn.sigmoid(xg) @ g2
    kk = (k * k_k).reshape(hd)
    kk = kk / jnp.maximum(jnp.sqrt(jnp.sum(kk * kk, axis=-1, keepdims=True)), 1e-12)
    k = k * (1.0 + (a - 1.0) * k_a)
    rh, kh, vh, ah, wh = (t.reshape(hd) for t in (r, k, v, a, decay))
    y = rwkv7_scan(rh, wh, kh, vh, kk, ah)
    y = head_norm(y, RWKV_LN_EPS).reshape(Bsz, S, W) * ln_w + ln_b
    bonus = jnp.sum(rh * kh * r_k, axis=-1, keepdims=True) * vh
    return (y + bonus.reshape(Bsz, S, W)) * g, v


def setup_inputs(seed: int = 0) -> dict:
    key = jax.random.key(seed)
    ks = iter(jax.random.split(key, 48))
    f32 = jnp.float32
    L, Lv, D = DEPTH, DEPTH - 1, D_MODEL

    def nrm(shape, scale):
        return jax.random.normal(next(ks), shape, f32) * scale

    def unif(shape, lo, hi):
        return jax.random.uniform(next(ks), shape, f32, lo, hi)

    x = nrm((BATCH, SEQ, D), 1.0)
    norm_mix_w = 1.0 + nrm((L, D), 0.02)
    w_in_first = nrm((D, IN_COLS), D ** -0.5)
    w_in_rest = nrm((Lv, D, IN_COLS + RWKV_MV_RANK), D ** -0.5)
    ssm_conv_w = nrm((L, SSM_CONV, SSM_CONV_DIM), SSM_CONV ** -0.5)
    ssm_conv_b = nrm((L, SSM_CONV_DIM), 0.02)
    dt0 = jnp.exp(unif((L, SSM_HEADS), math.log(1e-3), math.log(1e-1)))
    ssm_dt_bias = dt0 + jnp.log(-jnp.expm1(-dt0))
    ssm_a_log = jnp.log(unif((L, SSM_HEADS), 1.0, 16.0))
    ssm_d = 1.0 + nrm((L, SSM_HEADS), 0.02)
    ssm_norm_w = 1.0 + nrm((L, SSM_WIDTH), 0.02)
    rwkv_mu = unif((L, RWKV_COLS), 0.0, 1.0)
    rwkv_mu_v = unif((Lv, RWKV_MV_RANK), 0.0, 1.0)
    ramp = jnp.arange(RWKV_WIDTH, dtype=f32) / (RWKV_WIDTH - 1)
    rwkv_w0 = -6.5 + 5.0 * ramp ** 0.9 + nrm((L, RWKV_WIDTH), 0.1)
    rwkv_w2 = nrm((L, RWKV_DECAY_RANK, RWKV_WIDTH), 0.5 * RWKV_DECAY_RANK ** -0.5)
    rwkv_a0 = nrm((L, RWKV_WIDTH), 0.1)
    rwkv_a2 = nrm((L, RWKV_AAA_RANK, RWKV_WIDTH), 0.5 * RWKV_AAA_RANK ** -0.5)
    rwkv_v0 = 1.0 + nrm((Lv, RWKV_WIDTH), 0.1)
    rwkv_v2 = nrm((Lv, RWKV_MV_RANK, RWKV_WIDTH), 0.5 * RWKV_MV_RANK ** -0.5)
    rwkv_g2 = nrm((L, RWKV_GATE_RANK, RWKV_WIDTH), RWKV_GATE_RANK ** -0.5)
    rwkv_k_k = 0.85 + nrm((L, RWKV_WIDTH), 0.02)
    rwkv_k_a = 1.0 + nrm((L, RWKV_WIDTH), 0.02)
    rwkv_r_k = nrm((L, RWKV_HEADS, RWKV_HEAD_DIM), 0.1)
    rwkv_ln_w = 1.0 + nrm((L, RWKV_WIDTH), 0.02)
    rwkv_ln_b = nrm((L, RWKV_WIDTH), 0.02)
    w_out = nrm((L, D_MIX, D), D_MIX ** -0.5)
    norm_ffn_w = 1.0 + nrm((L, D), 0.02)
    ffn_w_gate = nrm((L, D, D_FF), D ** -0.5)
    ffn_w_up = nrm((L, D, D_FF), D ** -0.5)
    ffn_w_down = nrm((L, D_FF, D), D_FF ** -0.5)
    final_norm_w = 1.0 + nrm((D,), 0.02)
    return {
        'x': x, 'norm_mix_w': norm_mix_w, 'w_in_first': w_in_first, 'w_in_rest': w_in_rest,
        'ssm_conv_w': ssm_conv_w, 'ssm_conv_b': ssm_conv_b, 'ssm_dt_bias': ssm_dt_bias,
        'ssm_a_log': ssm_a_log, 'ssm_d': ssm_d, 'ssm_norm_w': ssm_norm_w,
        'rwkv_mu': rwkv_mu, 'rwkv_mu_v': rwkv_mu_v, 'rwkv_w0': rwkv_w0, 'rwkv_w2': rwkv_w2,
        'rwkv_a0': rwkv_a0, 'rwkv_a2': rwkv_a2, 'rwkv_v0': rwkv_v0, 'rwkv_v2': rwkv_v2,
        'rwkv_g2': rwkv_g2, 'rwkv_k_k': rwkv_k_k, 'rwkv_k_a': rwkv_k_a, 'rwkv_r_k': rwkv_r_k,
        'rwkv_ln_w': rwkv_ln_w, 'rwkv_ln_b': rwkv_ln_b, 'w_out': w_out,
        'norm_ffn_w': norm_ffn_w, 'ffn_w_gate': ffn_w_gate, 'ffn_w_up': ffn_w_up,
        'ffn_w_down': ffn_w_down, 'final_norm_w': final_norm_w,
    }


def reference(x, norm_mix_w, w_in_first, w_in_rest, ssm_conv_w, ssm_conv_b, ssm_dt_bias,
              ssm_a_log, ssm_d, ssm_norm_w, rwkv_mu, rwkv_mu_v, rwkv_w0, rwkv_w2,
              rwkv_a0, rwkv_a2, rwkv_v0, rwkv_v2, rwkv_g2, rwkv_k_k, rwkv_k_a, rwkv_r_k,
              rwkv_ln_w, rwkv_ln_b, w_out, norm_ffn_w, ffn_w_gate, ffn_w_up, ffn_w_down,
              final_norm_w):
    v_first = None
    for l in range(DEPTH):
        h = rms_norm(x, norm_mix_w[l])
        w_in = w_in_first if l == 0 else w_in_rest[l - 1]
        proj = (h @ w_in).astype(jnp.float32)
        pr = proj[..., :RET_COLS]
        y_ret = retention_mixer(pr[..., :RET_WIDTH], pr[..., RET_WIDTH:2 * RET_WIDTH],
                                pr[..., 2 * RET_WIDTH:3 * RET_WIDTH], pr[..., 3 * RET_WIDTH:])
        ps = proj[..., SSM_OFF:SSM_OFF + SSM_COLS]
        y_ssm = mamba2_mixer(ps[..., :SSM_WIDTH], ps[..., SSM_WIDTH:SSM_WIDTH + SSM_CONV_DIM],
                             ps[..., SSM_WIDTH + SSM_CONV_DIM:], ssm_conv_w[l], ssm_conv_b[l],
                             ssm_dt_bias[l], ssm_a_log[l], ssm_d[l], ssm_norm_w[l])
        pw = proj[..., RWKV_OFF:RWKV_OFF + RWKV_COLS]
        v_res = None if l == 0 else (proj[..., IN_COLS:], rwkv_mu_v[l - 1], rwkv_v0[l - 1],
                                     rwkv_v2[l - 1], v_first)
        y_rwkv, v_l = rwkv7_mixer(pw, rwkv_mu[l], rwkv_w0[l], rwkv_w2[l], rwkv_a0[l], rwkv_a2[l],
                                  rwkv_g2[l], rwkv_k_k[l], rwkv_k_a[l], rwkv_r_k[l],
                                  rwkv_ln_w[l], rwkv_ln_b[l], v_res)
        if l == 0:
            v_first = v_l
        y = jnp.concatenate([y_ret, y_ssm, y_rwkv], axis=-1).astype(x.dtype)
        x = x + y @ w_out[l]
        h = rms_norm(x, norm_ffn_w[l])
        x = x + (jax.nn.silu(h @ ffn_w_gate[l]) * (h @ ffn_w_up[l])) @ ffn_w_down[l]
    return rms_norm(x, final_norm_w)
```

```python
from contextlib import ExitStack
import numpy as np
import concourse.bass as bass
import concourse.mybir as mybir
from concourse.bass_utils import run_bass_kernel_spmd

F32 = mybir.dt.float32
BF16 = mybir.dt.bfloat16
AF = mybir.ActivationFunctionType
ALU = mybir.AluOpType

D_MODEL = 2048; BATCH = 2; SEQ = 8192; DEPTH = 4
IN_COLS = 6608; MV = 64; D_FF = 5632
TT = 512
NTOK = 2048
KC = 16
FC = 44
GW = 256
NG_IN = 27
PROJ_ROWS = NG_IN * GW


class Buf:
    def __init__(self, ap, name, parent=None):
        self.ap = ap; self.name = name
        self.root = parent.root if parent is not None else self
        if parent is None:
            self._w = None
            self._r = {}

    w = property(lambda self: self.root._w, lambda self, v: setattr(self.root, '_w', v))
    r = property(lambda self: self.root._r, lambda self, v: setattr(self.root, '_r', v))

    def __getitem__(self, idx):
        return self.ap[idx]


class KB:
    ROT = 24000

    def __init__(self, nc, es):
        self.nc = nc; self.es = es
        self.eng = {'pe': nc.tensor, 'dve': nc.vector, 'act': nc.scalar, 'pool': nc.gpsimd, 'sp': nc.sync}
        self.sems = {}
        self.cur = {}
        self.dcnt = {}
        self.waited = {e: {} for e in self.eng}
        self.nsem = 0
        self.out_sems = []
        self.dq = 0
        self.ninst = 0

    def _newsem(self, key):
        h = self.es.enter_context(self.nc.semaphore("s%d" % self.nsem)); self.nsem += 1
        self.sems[key] = h
        return h

    def sb(self, name, shape, dtype):
        t = self.es.enter_context(self.nc.sbuf_tensor("sb_" + name, list(shape), dtype))
        return Buf(t, name)

    def ps(self, name, shape=(128, 512), dtype=F32):
        t = self.es.enter_context(self.nc.psum_tensor("pp_" + name, list(shape), dtype))
        b = Buf(t, name); b.psum = True
        return b

    def _eng_token(self, e):
        key, cnt = self.cur.get(e, (None, 0))
        if key is None or cnt >= self.ROT:
            key = (e, len([k for k in self.sems if k[0] == e]))
            self._newsem(key); cnt = 0
        cnt += 1
        self.cur[e] = (key, cnt)
        return key, cnt

    def _wait(self, e, deps):
        w = self.waited[e]
        for key, val in deps.items():
            if w.get(key, 0) >= val:
                continue
            self.eng[e].wait_ge(self.sems[key], val)
            w[key] = val

    def _deps(self, reads, writes, e):
        deps = {}
        def add(k, v):
            if deps.get(k, 0) < v:
                deps[k] = v
        for b in reads:
            if b.w is not None:
                add(*b.w)
            if getattr(b.root, 'psum', False):
                for k, v in b.r.items():
                    if k[0] != e:
                        add(k, v)
        for b in writes:
            if b.w is not None:
                add(*b.w)
            for k, v in b.r.items():
                add(k, v)
        return deps

    def op(self, e, fn, reads=(), writes=(), pe_acc=False):
        deps = self._deps(reads, writes, e)
        if pe_acc:
            deps = {k: v for k, v in deps.items() if k[0] != 'pe'}
        self._wait(e, deps)
        key, cnt = self._eng_token(e)
        ins = fn(self.eng[e])
        ins.then_inc(self.sems[key], 1)
        self.ninst += 1
        for b in reads:
            if b.r.get(key, 0) < cnt:
                b.r[key] = cnt
        for b in writes:
            b.w = (key, cnt); b.r = {}
        return ins

    def dma(self, out_ap, in_ap, reads=(), writes=(), q=None, is_out=False):
        if q is None:
            q = ('sp', 'pool')[self.dq % 2]; self.dq += 1
        deps = self._deps(reads, writes, q)
        self._wait(q, deps)
        tgt = writes[0] if writes else reads[0]
        key = ('dma', tgt.root.name, 'w' if writes else 'r')
        if key not in self.sems:
            self._newsem(key); self.dcnt[key] = 0
        self.dcnt[key] += 16
        val = self.dcnt[key]
        self.eng[q].dma_start(out=out_ap, in_=in_ap).then_inc(self.sems[key], 16)
        self.ninst += 1
        for b in reads:
            if b.r.get(key, 0) < val:
                b.r[key] = val
        for b in writes:
            b.w = (key, val); b.r = {}
        if is_out:
            self.out_sems = [(k, v) for k, v in self.out_sems if k != key] + [(key, val)]

    def finish(self):
        for key, val in self.out_sems:
            self.eng['sp'].wait_ge(self.sems[key], val)


class Dense:
    def __init__(self, kb, sq=None):
        self.kb = kb
        self.xT = kb.sb("xT", [128, KC, TT], F32)
        self.hT = kb.sb("hT", [128, KC, TT], BF16)
        self.sq = sq if sq is not None else kb.sb("sq", [128, KC, TT], BF16)
        self.rstd = kb.sb("rstd", [128, TT], F32)
        self.ones = kb.sb("ones", [128, 128], BF16)
        self.wst = [kb.sb("wst%d" % i, [128, KC * GW], F32) for i in range(2)]
        self.wbf = [kb.sb("wbf%d" % i, [128, KC * GW], BF16) for i in range(2)]
        self.wi = 0
        self.ost = [kb.sb("ost%d" % i, [128, TT], F32) for i in range(2)]
        self.oi = 0
        self.pss = kb.ps("ps_ss")
        self.psm = [kb.ps("ps_m%d" % i) for i in range(4)]
        self.pi = 0
        self.ci = 0
        kb.op('dve', lambda e: e.memset(self.ones[:], 1.0), writes=[self.ones])

    def load_w(self, w_ap, nk, gw):
        kb = self.kb
        i = self.wi; self.wi ^= 1
        st, bf = self.wst[i], self.wbf[i]
        n = nk * gw
        kb.dma(st[:, 0:n], w_ap, writes=[st])
        ce = ('dve', 'pool', 'act')[self.ci % 3]; self.ci += 1
        if ce == 'act':
            kb.op(ce, lambda e: e.copy(out=bf[:, 0:n], in_=st[:, 0:n]), reads=[st], writes=[bf])
        else:
            kb.op(ce, lambda e: e.tensor_copy(out=bf[:, 0:n], in_=st[:, 0:n]), reads=[st], writes=[bf])
        return bf, bf[:, 0:n].rearrange("p (k m) -> p k m", m=gw)

    def next_ps(self):
        p = self.psm[self.pi]; self.pi = (self.pi + 1) % 4
        return p

    def rmsnorm(self, nw, off, eps, dst_f32=None):
        kb = self.kb; xT, sq, pss, rstd = self.xT, self.sq, self.pss, self.rstd
        kb.op('act', lambda e: e.activation(out=sq[:], in_=xT[:], func=AF.Square), reads=[xT], writes=[sq])
        for k in range(KC):
            kb.op('pe', lambda e, k=k: e.matmul(pss[:], lhsT=self.ones[:], rhs=sq[:, k, :], start=(k == 0), stop=(k == KC - 1)),
                  reads=[self.ones, sq], writes=[pss], pe_acc=(k > 0))
        kb.op('act', lambda e: e.activation(out=rstd[:], in_=pss[:], func=AF.Sqrt, scale=1.0 / D_MODEL, bias=self.epsb(eps)),
              reads=[pss, self.eps_buf], writes=[rstd])
        kb.op('dve', lambda e: e.reciprocal(out=rstd[:], in_=rstd[:]), reads=[rstd], writes=[rstd])
        dst = self.hT if dst_f32 is None else dst_f32
        for k in range(KC):
            en = 'dve'
            kb.op(en, lambda e, k=k: e.scalar_tensor_tensor(out=dst[:, k, :], in0=xT[:, k, :], scalar=nw[:, off + k:off + k + 1], in1=rstd[:],
                                                          op0=ALU.mult, op1=ALU.mult),
                  reads=[xT, nw, rstd], writes=[dst])

    def epsb(self, eps):
        return self.eps_buf[:, self.eps_idx[eps]:self.eps_idx[eps] + 1]

    def init_eps(self, vals):
        kb = self.kb
        self.eps_buf = kb.sb("epsb", [128, len(vals)], F32)
        self.eps_idx = {v: i for i, v in enumerate(vals)}
        for i, v in enumerate(vals):
            kb.op('dve', lambda e, i=i, v=v: e.memset(self.eps_buf[:, i:i + 1], v), writes=[self.eps_buf])

    def gemm(self, w_dram, ng, nk, gw, rhs, consume):
        kb = self.kb
        for g in range(ng):
            bf, wv = self.load_w(w_dram[g], nk, gw)
            for m in range(gw // 128):
                ps = self.next_ps()
                for k in range(nk):
                    kb.op('pe', lambda e, k=k, m=m: e.matmul(ps[:], lhsT=wv[:, k, m * 128:(m + 1) * 128], rhs=rhs[:, k, :],
                                                            start=(k == 0), stop=(k == nk - 1)),
                          reads=[bf, rhs], writes=[ps], pe_acc=(k > 0))
                consume(g * (gw // 128) + m, ps)

    def gemm_down(self, w_dram, rhs, consume):
        kb = self.kb
        for g in range(16):
            ps = self.next_ps()
            for hf in range(2):
                bf, wv = self.load_w(w_dram[g, hf], FC // 2, 128)
                for k in range(FC // 2):
                    kk = hf * (FC // 2) + k
                    kb.op('pe', lambda e, k=k, kk=kk: e.matmul(ps[:], lhsT=wv[:, k, :], rhs=rhs[:, kk, :],
                                                              start=(kk == 0), stop=(kk == FC - 1)),
                          reads=[bf, rhs], writes=[ps], pe_acc=(kk > 0))
            consume(g, ps)


def build_dense(has_c, has_a, final):
    nc = bass.Bass("TRN2", target_bir_lowering=False)
    dr = lambda n, s, kind="ExternalInput": nc.dram_tensor(n, list(s), F32, kind=kind).ap()
    xT_d = dr("xT", [D_MODEL, NTOK])
    vecs = dr("vecs", [128, 64])
    if has_c:
        yT_d = dr("yT", [D_MODEL, NTOK])
        wout_d = dr("wout", [8, 128, KC * GW])
        wg_d = dr("wg", [22, 128, KC * GW])
        wu_d = dr("wu", [22, 128, KC * GW])
        wd_d = dr("wd", [16, 2, 128, (FC // 2) * 128])
        xo_d = dr("xo", [D_MODEL, NTOK], "ExternalOutput")
    if has_a:
        win_d = dr("win", [NG_IN, 128, KC * GW])
        pj_d = dr("pj", [PROJ_ROWS, NTOK], "ExternalOutput")
    with ExitStack() as es:
        kb = KB(nc, es)
        if has_c:
            aT = kb.sb("aT", [128, FC, TT], BF16)
            dn = Dense(kb, sq=Buf(aT.ap[:, 0:KC, :], "aT", parent=aT))
        else:
            dn = Dense(kb)
        dn.init_eps([1e-6, 1e-5])
        vb = kb.sb("vecs_sb", [128, 64], F32)
        kb.dma(vb[:], vecs, writes=[vb], q='sp')
        xT = dn.xT
        if has_c:
            yT = kb.sb("yT_sb", [128, KC, TT], F32)
            yb = dn.hT
            sg = [kb.sb("sg%d" % i, [128, TT], F32) for i in range(2)]
            psg = [kb.ps("ps_g%d" % i) for i in range(2)]
        for t in range(NTOK // TT):
            ts = slice(t * TT, (t + 1) * TT)
            kb.dma(xT[:], xT_d.rearrange("(k p) n -> p k n", p=128)[:, :, ts], writes=[xT], q='sp')
            if has_c:
                kb.dma(yT[:], yT_d.rearrange("(k p) n -> p k n", p=128)[:, :, ts], writes=[yT], q='pool')
                sq = dn.sq
                kb.op('act', lambda e: e.activation(out=sq[:, 4:12, :], in_=yT[:, 4:12, :], func=AF.Square), reads=[yT], writes=[sq])
                for g in range(2):
                    for j in range(4):
                        k = 4 + g * 4 + j
                        kb.op('pe', lambda e, k=k, j=j: e.matmul(dn.pss[:], lhsT=dn.ones[:], rhs=sq[:, k, :], start=(j == 0), stop=(j == 3)),
                              reads=[dn.ones, sq], writes=[dn.pss], pe_acc=(j > 0))
                    kb.op('act', lambda e: e.activation(out=dn.rstd[:], in_=dn.pss[:], func=AF.Sqrt, scale=1.0 / 512, bias=dn.epsb(1e-5)),
                          reads=[dn.pss, dn.eps_buf], writes=[dn.rstd])
                    kb.op('dve', lambda e: e.reciprocal(out=dn.rstd[:], in_=dn.rstd[:]), reads=[dn.rstd], writes=[dn.rstd])
                    for j in range(4):
                        k = 4 + g * 4 + j
                        kb.op('dve', lambda e, k=k: e.scalar_tensor_tensor(out=yb[:, k, :], in0=yT[:, k, :], scalar=vb[:, 48 + k - 4:49 + k - 4],
                                                                      in1=dn.rstd[:], op0=ALU.mult, op1=ALU.mult),
                              reads=[yT, vb, dn.rstd], writes=[yb])
                for k in list(range(0, 4)) + list(range(12, 16)):
                    kb.op('pool', lambda e, k=k: e.tensor_copy(out=yb[:, k, :], in_=yT[:, k, :]), reads=[yT], writes=[yb])
                def c_out(r, ps):
                    kb.op('dve', lambda e: e.tensor_tensor(out=xT[:, r, :], in0=ps[:], in1=xT[:, r, :], op=ALU.add), reads=[ps, xT], writes=[xT])
                dn.gemm(wout_d, 8, KC, GW, yb, c_out)
                dn.rmsnorm(vb, 16, 1e-6)
                for g in range(22):
                    bfg, wgv = dn.load_w(wg_d[g], KC, GW)
                    bfu, wuv = dn.load_w(wu_d[g], KC, GW)
                    for m in range(2):
                        pg, pu = psg
                        for k in range(KC):
                            kb.op('pe', lambda e, k=k, m=m: e.matmul(pg[:], lhsT=wgv[:, k, m * 128:(m + 1) * 128], rhs=dn.hT[:, k, :],
                                                                    start=(k == 0), stop=(k == KC - 1)),
                                  reads=[bfg, dn.hT], writes=[pg], pe_acc=(k > 0))
                        for k in range(KC):
                            kb.op('pe', lambda e, k=k, m=m: e.matmul(pu[:], lhsT=wuv[:, k, m * 128:(m + 1) * 128], rhs=dn.hT[:, k, :],
                                                                    start=(k == 0), stop=(k == KC - 1)),
                                  reads=[bfu, dn.hT], writes=[pu], pe_acc=(k > 0))
                        s_ = sg[m]
                        kb.op('act', lambda e: e.activation(out=s_[:], in_=pg[:], func=AF.Silu), reads=[pg], writes=[s_])
                        r = g * 2 + m
                        kb.op('dve', lambda e, r=r: e.tensor_tensor(out=aT[:, r, :], in0=pu[:], in1=s_[:], op=ALU.mult),
                              reads=[pu, s_], writes=[aT])
                dn.gemm_down(wd_d, aT, c_out)
                if final:
                    dn.rmsnorm(vb, 32, 1e-6, dst_f32=yT)
                    kb.dma(xo_d.rearrange("(k p) n -> p k n", p=128)[:, :, ts], yT[:], reads=[yT], is_out=True, q='sp')
                else:
                    kb.dma(xo_d.rearrange("(k p) n -> p k n", p=128)[:, :, ts], xT[:], reads=[xT], is_out=True, q='sp')
            if has_a:
                dn.rmsnorm(vb, 0, 1e-6)
                def c_in(r, ps):
                    o = dn.ost[dn.oi]; dn.oi ^= 1
                    if r % 2 == 0:
                        kb.op('act', lambda e: e.copy(out=o[:], in_=ps[:]), reads=[ps], writes=[o])
                    else:
                        kb.op('dve', lambda e: e.tensor_copy(out=o[:], in_=ps[:]), reads=[ps], writes=[o])
                    kb.dma(pj_d[r * 128:(r + 1) * 128, ts], o[:], reads=[o], is_out=True)
                dn.gemm(win_d, NG_IN, KC, GW, dn.hT, c_in)
        kb.finish()
    return nc


def arrange_w(W, gw, ng=None):
    K, M = W.shape
    if ng is None:
        ng = (M + gw - 1) // gw
    if ng * gw != M:
        Wp = np.zeros((K, ng * gw), np.float32); Wp[:, :M] = W; W = Wp
    return np.ascontiguousarray(W.reshape(K // 128, 128, ng, gw).transpose(2, 1, 0, 3)).reshape(ng, 128, (K // 128) * gw)


def vec_pk(v):
    return np.ascontiguousarray(v.reshape(-1, 128).T)


class V:
    def __init__(self, b, ap):
        self.b = b; self.ap = ap

    def __getitem__(self, idx):
        return V(self.b, self.ap[idx])

    def rr(self, pat, **kw):
        return V(self.b, self.ap.rearrange(pat, **kw))


class TB(Buf):
    def __getitem__(self, idx):
        return V(self, self.ap[idx])


class MX:
    def __init__(self, kb):
        self.kb = kb
        self.bank = [TB(kb.es.enter_context(kb.nc.psum_tensor("pp_b%d" % i, [128, 512], F32)), "bank%d" % i) for i in range(8)]
        self.bi = 0
        for b in self.bank:
            b.psum = True

    def sb(self, name, shape, dtype=F32):
        t = self.kb.es.enter_context(self.kb.nc.sbuf_tensor("sb_" + name, list(shape), dtype))
        return TB(t, name)

    def pz(self):
        b = self.bank[self.bi]; self.bi = (self.bi + 1) % 6
        return b

    @staticmethod
    def _rw(vs):
        return [v.b for v in vs if isinstance(v, V)]

    @staticmethod
    def _a(x):
        return x.ap if isinstance(x, V) else x

    def tt(self, e, out, a, b, op):
        self.kb.op(e, lambda en: en.tensor_tensor(out=out.ap, in0=a.ap, in1=b.ap, op=op), reads=self._rw([a, b]), writes=[out.b])

    def ts(self, e, out, a, s1, op0, s2=None, op1=None):
        if op1 is None:
            self.kb.op(e, lambda en: en.tensor_scalar(out=out.ap, in0=a.ap, scalar1=self._a(s1), scalar2=None, op0=op0),
                       reads=self._rw([a, s1]), writes=[out.b])
        else:
            self.kb.op(e, lambda en: en.tensor_scalar(out=out.ap, in0=a.ap, scalar1=self._a(s1), scalar2=self._a(s2), op0=op0, op1=op1),
                       reads=self._rw([a, s1, s2]), writes=[out.b])

    def stt(self, out, a, s, b, op0, op1):
        self.kb.op('dve', lambda en: en.scalar_tensor_tensor(out=out.ap, in0=a.ap, scalar=self._a(s), in1=b.ap, op0=op0, op1=op1),
                   reads=self._rw([a, s, b]), writes=[out.b])

    def act(self, out, a, func, bias=None, scale=1.0):
        if bias is None:
            self.kb.op('act', lambda en: en.activation(out=out.ap, in_=a.ap, func=func, scale=self._a(scale)),
                       reads=self._rw([a, scale]), writes=[out.b])
        else:
            self.kb.op('act', lambda en: en.activation(out=out.ap, in_=a.ap, func=func, bias=self._a(bias), scale=self._a(scale)),
                       reads=self._rw([a, bias, scale]), writes=[out.b])

    def cp(self, e, out, a):
        if e == 'act':
            self.kb.op(e, lambda en: en.copy(out=out.ap, in_=a.ap), reads=[a.b], writes=[out.b])
        else:
            self.kb.op(e, lambda en: en.tensor_copy(out=out.ap, in_=a.ap), reads=[a.b], writes=[out.b])

    def mm(self, out, lhsT, rhs, start=True, stop=True):
        self.kb.op('pe', lambda en: en.matmul(out.ap, lhsT=lhsT.ap, rhs=rhs.ap, start=start, stop=stop),
                   reads=[lhsT.b, rhs.b], writes=[out.b], pe_acc=(not start))

    def scan(self, out, d0, d1, init, op0, op1):
        self.kb.op('dve', lambda en: en.tensor_tensor_scan(out=out.ap, data0=d0.ap, data1=d1.ap, initial=init, op0=op0, op1=op1),
                   reads=[d0.b, d1.b], writes=[out.b])

    def memset(self, e, out, val):
        self.kb.op(e, lambda en: en.memset(out.ap, val), writes=[out.b])

    def dma_in(self, out, src_ap, q=None):
        self.kb.dma(out.ap, src_ap, writes=[out.b], q=q)

    def dma_out(self, dst_ap, src, q=None):
        self.kb.dma(dst_ap, src.ap, reads=[src.b], is_out=True, q=q)


PP_CONVW = 0
PP_CONVB = 16
PP_DSKIP = 20
PP_DTB = 22
PP_ALOG = 23
PP_MU = 24
PP_W0 = 32; PP_A0 = 33; PP_V0 = 34; PP_KK = 35; PP_KA = 36; PP_RK = 37; PP_LNW = 38; PP_LNB = 39
PP_GC = 40
PP_ZETA = 41
CF_MASKD = 0; CF_XI = 1; CF_NEGM = 2; CF_NML = 3; CF_NMU = 4; CF_MU = 5; CF_MUI = 6; CF_NMUI = 7
NCF = 8
CM_ONES128 = 0; CM_BONES = 1; CM_BONES64 = 2; CM_SEL = 3; CM_SEL2 = 7; CM_I4 = 9; CM_IDENT = 10
NCM = 11
CB_IDENT = 0; CB_SWAP = 1
NCB = 2
EXPM05 = float(np.exp(-0.5))


def build_mixer(S, has_vres):
    nc = bass.Bass("TRN2", target_bir_lowering=False)
    dr = lambda n, s, kind="ExternalInput", dt=F32: nc.dram_tensor(n, list(s), dt, kind=kind).ap()
    ret_in = dr("ret_in", [4, 128, S]); rope = dr("rope", [2, 128, S])
    ssd_zx = dr("ssd_z", [2, 128, S]); ssd_c = dr("ssd_c", [4, 128, 3 + S]); ssd_dt = dr("ssd_dt", [4, S])
    rw_in = dr("rw_in", [8, 128, 1 + S])
    if has_vres:
        vfirst = dr("vfirst", [128, S])
    pp_d = dr("pp", [128, 64]); cf_d = dr("cf", [128, NCF, 512]); cm_d = dr("cm", [128, NCM, 128])
    cb_d = dr("cb", [128, NCB, 128], dt=BF16); rww_d = dr("rww", [128, 5, 128])
    y_ret = dr("y_ret", [128, S], "ExternalOutput"); y_ssd = dr("y_ssd", [2, 128, S], "ExternalOutput")
    y_rw = dr("y_rw", [128, S], "ExternalOutput")
    if not has_vres:
        v_out = dr("v_out", [128, S], "ExternalOutput")
    NTL = S // TT
    with ExitStack() as es:
        kb = KB(nc, es); m = MX(kb)
        T = TT
        pp = m.sb("pp", [128, 64]); cf = m.sb("cf", [128, NCF, 512]); cm = m.sb("cm", [128, NCM, 128])
        cb = m.sb("cb", [128, NCB, 128], BF16); rww = m.sb("rww", [128, 5, 128]); rwb = m.sb("rwb", [128, 5, 128], BF16)
        m.dma_in(pp[:], pp_d); m.dma_in(cf[:], cf_d); m.dma_in(cm[:], cm_d); m.dma_in(cb[:], cb_d); m.dma_in(rww[:], rww_d)
        m.cp('dve', rwb[:], rww[:])
        ident = cb[:, CB_IDENT, :]; swp = cb[:, CB_SWAP, :]
        col = lambda c, n=128: pp[0:n, c:c + 1]
        epsb = m.sb("epsb", [128, 4])
        for i, v_ in enumerate((1e-6, 64e-5, 1e-24)):
            m.memset('dve', epsb[:, i:i + 1], v_)
        ones_f = m.sb("ones_f", [128, 128])
        m.memset('dve', ones_f[:], 1.0)
        omu = m.sb("omu", [128, 8])
        m.ts('dve', omu[:], pp[:, PP_MU:PP_MU + 8], -1.0, ALU.mult, 1.0, ALU.add)
        negA = m.sb("negA", [4, 1])
        m.act(negA[:], pp[0:4, PP_ALOG:PP_ALOG + 1], AF.Exp)
        m.ts('dve', negA[:], negA[:], -1.0, ALU.mult)

        big1 = m.sb("big1", [128, 4112]); big2 = m.sb("big2", [128, 8 * T])
        vw = lambda big, lo, hi, f, nm: TB(big.ap[:, lo:hi].rearrange("p (f s) -> p f s", f=f), nm, parent=big)
        r_in = vw(big2, 0, 4 * T, 4, "r_in"); r_cs = vw(big2, 4 * T, 6 * T, 2, "r_cs")
        qb = m.sb("qb", [128, T], BF16); kbb = m.sb("kbb", [128, T], BF16); vbb = m.sb("vbb", [128, T], BF16)
        rt1 = m.sb("rt1", [128, T]); rt2 = m.sb("rt2", [128, T])
        qr = m.sb("qr", [128, T], BF16); kr = m.sb("kr", [128, T], BF16); qx = m.sb("qx", [128, T], BF16)
        vtok = m.sb("vtok", [128, T], BF16); kztok = m.sb("kztok", [128, T], BF16); pT = m.sb("pT", [128, T], BF16)
        rS = m.sb("rS", [128, 128]); rSb = [m.sb("rSb%d" % i, [128, 128], BF16) for i in range(2)]
        ry = m.sb("ry", [128, T]); rysq = m.sb("rysq", [128, T]); rmean = m.sb("rmean", [128, T]); rvar = m.sb("rvar", [128, T])
        rsg = m.sb("rsg", [128, T])
        m.memset('dve', rS[:], 0.0); m.memset('pool', rSb[0][:], 0.0)
        rsi = [0]

        def retention(t):
            ts_ = slice(t * T, (t + 1) * T)
            m.dma_in(r_in[:], ret_in.rearrange("f p s -> p f s")[:, :, ts_])
            m.dma_in(r_cs[:], rope.rearrange("f p s -> p f s")[:, :, ts_])
            q, k, v, g = (r_in[:, i, :] for i in range(4))
            cos, sin = r_cs[:, 0, :], r_cs[:, 1, :]
            m.cp('pool', qb[:], q); m.cp('pool', kbb[:], k); m.cp('act', vbb[:], v)
            for src, srcb, dst in ((q, qb, qr), (k, kbb, kr)):
                p = m.pz()
                m.mm(p[:], swp, srcb[:])
                m.tt('pool', rt1[:], src, cos, ALU.mult)
                m.tt('dve', rt2[:], p[:], sin, ALU.mult)
                m.tt('dve', dst[:], rt1[:], rt2[:], ALU.add)
            m.tt('pool', qx[:], qr[:], cf[:, CF_XI, :], ALU.mult)
            pv_, pk_, psc = m.pz(), m.pz(), m.pz()
            for c in range(4):
                cs = slice(c * 128, (c + 1) * 128)
                m.mm(pv_[:, cs], vbb[:, cs], ident)
                m.mm(pk_[:, cs], kr[:, cs], ident)
                m.mm(psc[:, cs], kr[:, cs], qr[:, cs])
            m.cp('act', vtok[:], pv_[:])
            m.ts('dve', kztok[:], pk_[:], col(PP_ZETA), ALU.mult)
            m.tt('dve', pT[:], psc[:], cf[:, CF_MASKD, :], ALU.mult)
            po, pst = m.pz(), m.pz()
            for c in range(4):
                cs = slice(c * 128, (c + 1) * 128)
                m.mm(pst[:, cs], kztok[:, cs], vtok[:, cs])
            for c in range(4):
                cs = slice(c * 128, (c + 1) * 128)
                sb_ = rSb[rsi[0]]
                m.mm(po[:, cs], vtok[:, cs], pT[:, cs], start=True, stop=False)
                m.mm(po[:, cs], sb_[:], qx[:, cs], start=False, stop=True)
                m.stt(rS[:], rS[:], col(PP_GC), pst[:, cs], ALU.mult, ALU.add)
                rsi[0] ^= 1
                m.cp('act', rSb[rsi[0]][:], rS[:])
            m.cp('act', ry[:], po[:])
            m.act(rysq[:], po[:], AF.Square)
            pm, pq = m.pz(), m.pz()
            m.mm(pm[:], cm[:, CM_ONES128, :], ry[:])
            m.mm(pq[:], cm[:, CM_ONES128, :], rysq[:])
            m.cp('act', rmean[:], pm[:])
            m.tt('dve', rvar[:], rmean[:], rmean[:], ALU.mult)
            m.tt('dve', rvar[:], pq[:], rvar[:], ALU.subtract)
            m.act(rvar[:], rvar[:], AF.Sqrt, bias=epsb[:, 0:1])
            m.kb.op('dve', lambda en: en.reciprocal(out=rvar.ap[:], in_=rvar.ap[:]), reads=[rvar], writes=[rvar])
            m.tt('dve', ry[:], ry[:], rmean[:], ALU.subtract)
            m.tt('dve', ry[:], ry[:], rvar[:], ALU.mult)
            m.act(rsg[:], g, AF.Silu)
            m.tt('dve', ry[:], ry[:], rsg[:], ALU.mult)
            m.dma_out(y_ret[:, ts_], ry[:])

        s_z = vw(big2, 6 * T, 8 * T, 2, "s_z"); s_c = vw(big1, 0, 4 * (3 + T), 4, "s_c"); s_dt = m.sb("s_dt", [4, T])
        s_cv = vw(big1, 4 * (3 + T), 4 * (3 + T) + 4 * T, 4, "s_cv"); s_Bb = m.sb("s_Bb", [128, T], BF16); s_Cb = m.sb("s_Cb", [128, T], BF16)
        s_a = m.sb("s_a", [4, T]); s_ac = m.sb("s_ac", [4, T])
        s_ab = m.sb("s_ab", [128, 2, T]); s_E = m.sb("s_E", [128, 2, T]); s_dte = m.sb("s_dte", [128, 2, T])
        s_xdt = m.sb("s_xdt", [128, 2, T]); s_xdtb = m.sb("s_xdtb", [128, 2, T], BF16); s_xdteb = m.sb("s_xdteb", [128, 2, T], BF16)
        s_xtokP = m.sb("s_xtokP", [128, 4, 4, 128], BF16)
        s_xetok = m.sb("s_xetok", [128, 4, 256], BF16)
        s_Btok = m.sb("s_Btok", [128, T], BF16)
        s_negcol = m.sb("s_negcol", [128, 16]); s_cdec = m.sb("s_cdec", [128, 4, 4])
        s_cbT = m.sb("s_cbT", [128, T]); s_seg = rt2; s_G = [m.sb("s_G%d" % i, [128, T], BF16) for i in range(2)]
        s_S = m.sb("s_S", [128, 256]); s_Sb = [m.sb("s_Sb%d" % i, [128, 256], BF16) for i in range(2)]
        s_t1 = ry; s_sz = rt1; s_y = m.sb("s_y", [128, 2, T])
        m.memset('dve', s_S[:], 0.0); m.memset('pool', s_Sb[0][:], 0.0); m.memset('pool', s_xtokP[:], 0.0)
        ssi = [0]

        def ssd(t):
            ts_ = slice(t * T, (t + 1) * T)
            m.dma_in(s_z[:], ssd_zx.rearrange("f p s -> p f s")[:, :, ts_])
            m.dma_in(s_c[:], ssd_c.rearrange("f p s -> p f s")[:, :, t * T:t * T + T + 3])
            m.dma_in(s_dt[:], ssd_dt[:, ts_])
            for b in range(4):
                o = s_cv[:, b, :]
                m.act(o, s_c[:, b, 0:T], AF.Identity, bias=col(PP_CONVB + b), scale=col(PP_CONVW + b * 4))
                for k in range(1, 4):
                    m.stt(o, s_c[:, b, k:k + T], col(PP_CONVW + b * 4 + k), o, ALU.mult, ALU.add)
                m.act(o, o, AF.Silu)
            m.cp('pool', s_Bb[:], s_cv[:, 2, :]); m.cp('pool', s_Cb[:], s_cv[:, 3, :])
            m.act(s_dt[:], s_dt[:], AF.Exp, bias=pp[0:4, PP_DTB:PP_DTB + 1])
            m.act(s_dt[:], s_dt[:], AF.Ln, bias=1.0)
            m.ts('dve', s_a[:], s_dt[:], negA[:, 0:1], ALU.mult)
            for c in range(4):
                cs = slice(c * 128, (c + 1) * 128)
                m.scan(s_ac[:, cs], ones_f[0:4, :], s_a[:, cs], 0.0, ALU.mult, ALU.add)
            for b in range(2):
                p1, p2 = m.pz(), m.pz()
                m.mm(p1[:], cm[0:4, CM_SEL2 + b, :], s_ac[:])
                m.mm(p2[:], cm[0:4, CM_SEL2 + b, :], s_dt[:])
                m.cp('act', s_ab[:, b, :], p1[:])
                m.act(s_E[:, b, :], p1[:], AF.Exp)
                m.tt('dve', s_xdt[:, b, :], s_cv[:, b, :], p2[:], ALU.mult)
                for c in range(4):
                    cs = slice(c * 128, (c + 1) * 128)
                    m.act(s_dte[:, b, cs], s_ab[:, b, cs], AF.Exp, bias=s_ab[:, b, c * 128 + 127:c * 128 + 128], scale=-1.0)
                m.cp('pool', s_xdtb[:, b, :], s_xdt[:, b, :])
                m.tt('pool', s_xdteb[:, b, :], s_xdt[:, b, :], s_dte[:, b, :], ALU.mult)
            for b in range(2):
                p1, p2 = m.pz(), m.pz()
                for c in range(4):
                    cs = slice(c * 128, (c + 1) * 128)
                    m.mm(p1[:, cs], s_xdtb[:, b, cs], ident)
                    m.mm(p2[:, cs], s_xdteb[:, b, cs], ident)
                for hl in range(2):
                    h = b * 2 + hl
                    m.cp('act', s_xtokP[:, :, h, hl * 64:(hl + 1) * 64], p1[:].rr("p (c x) -> p c x", c=4)[:, :, hl * 64:(hl + 1) * 64])
                m.cp('dve', s_xetok[:, :, b * 128:(b + 1) * 128], p2[:].rr("p (c x) -> p c x", c=4))
            pB, pcb, pcol = m.pz(), m.pz(), m.pz()
            for c in range(4):
                cs = slice(c * 128, (c + 1) * 128)
                m.mm(pB[:, cs], s_Bb[:, cs], ident)
                m.mm(pcb[:, cs], s_Bb[:, cs], s_Cb[:, cs])
                m.mm(pcol[:, c * 4:(c + 1) * 4], s_ac[:, cs], cm[0:4, CM_I4, 0:4])
            m.cp('act', s_Btok[:], pB[:])
            m.cp('act', s_cbT[:], pcb[:])
            m.ts('dve', s_negcol[:], pcol[:, 0:16], -1.0, ALU.mult)
            py = [m.bank[6], m.bank[7]]
            for h in range(4):
                b, hl = h // 2, h % 2
                pab = m.pz()
                m.mm(pab[:], cm[0:4, CM_SEL + h, :], s_ac[:])
                m.tt('dve', s_seg[:], pab[:], cf[:, CF_NEGM, :], ALU.add)
                m.act(s_cdec[:, h, :], pab[:].rr("p (c x) -> p c x", c=4)[:, :, 127], AF.Exp)
                for c in range(4):
                    cs = slice(c * 128, (c + 1) * 128)
                    m.act(s_seg[:, cs], s_seg[:, cs], AF.Exp, bias=s_negcol[:, c * 4 + h:c * 4 + h + 1])
                G = s_G[h % 2]
                m.tt('dve', G[:], s_cbT[:], s_seg[:], ALU.mult)
                if hl == 1:
                    for c in range(4):
                        cs = slice(c * 128, (c + 1) * 128)
                        m.mm(py[b][:, cs], s_xtokP[:, c, h - 1, :], s_G[0][:, cs], start=True, stop=False)
                        m.mm(py[b][:, cs], s_xtokP[:, c, h, :], s_G[1][:, cs], start=False, stop=True)
            pS = [m.pz(), m.pz()]
            for c in range(4):
                m.mm(pS[c // 2][:, (c % 2) * 256:(c % 2 + 1) * 256], s_Btok[:, c * 128:(c + 1) * 128], s_xetok[:, c, :])
            poff = [m.pz(), m.pz()]
            for c in range(4):
                cs = slice(c * 128, (c + 1) * 128)
                sb_ = s_Sb[ssi[0]]
                for b in range(2):
                    m.mm(poff[b][:, cs], sb_[:, b * 128:(b + 1) * 128], s_Cb[:, cs])
                for h in range(4):
                    hs = slice(h * 64, (h + 1) * 64)
                    m.stt(s_S[:, hs], s_S[:, hs], s_cdec[:, h, c:c + 1], pS[c // 2][:, (c % 2) * 256 + h * 64:(c % 2) * 256 + (h + 1) * 64],
                          ALU.mult, ALU.add)
                ssi[0] ^= 1
                m.cp('act', s_Sb[ssi[0]][:], s_S[:])
            for b in range(2):
                m.tt('dve', s_t1[:], poff[b][:], s_E[:, b, :], ALU.mult)
                m.tt('dve', s_t1[:], s_t1[:], py[b][:], ALU.add)
                m.stt(s_t1[:], s_cv[:, b, :], col(PP_DSKIP + b), s_t1[:], ALU.mult, ALU.add)
                m.act(s_sz[:], s_z[:, b, :], AF.Silu)
                m.tt('dve', s_y[:, b, :], s_t1[:], s_sz[:], ALU.mult)
            m.dma_out(y_ssd.rearrange("f p s -> p f s")[:, :, ts_], s_y[:])

        w_in = vw(big1, 0, 8 * (1 + T), 8, "w_in"); w_mx = vw(big2, 0, 8 * T, 8, "w_mx"); w_tmp = rt1
        w_thb = m.sb("w_thb", [128, T], BF16); w_xab = m.sb("w_xab", [128, T], BF16); w_sgb = m.sb("w_sgb", [128, 2, T], BF16)
        w_pvb = m.sb("w_pvb", [128, T], BF16)
        w_lw = m.sb("w_lw", [128, T]); w_a = m.sb("w_a", [128, T]); w_g = m.sb("w_g", [128, T]); w_v = m.sb("w_v", [128, T])
        w_kk = m.sb("w_kk", [128, T]); w_km = m.sb("w_km", [128, T]); w_b = m.sb("w_b", [128, T]); w_bon = m.sb("w_bon", [128, T])
        w_L = rt2; w_W = m.sb("w_W", [128, T]); w_Wi = m.sb("w_Wi", [128, T]); w_Wp = rsg
        w_vf = m.sb("w_vf", [128, T])
        NCH = T // 64
        BDn = ("RT", "KT", "BT", "KA", "VF")
        BDf = {n: m.sb("w_bd" + n, [128, NCH, 128], BF16) for n in BDn}
        for n in BDn:
            m.memset('pool', BDf[n][:], 0.0)
        w_Vt = m.sb("w_Vt", [128, 4, 128], BF16); w_KTt = m.sb("w_KTt", [128, 4, 128], BF16); w_nBTt = m.sb("w_nBTt", [128, 4, 128], BF16)
        w_N = [m.sb("w_N%d" % i, [128, 4, 128], BF16) for i in range(2)]
        w_NT = [m.sb("w_NT%d" % i, [128, 4, 128], BF16) for i in range(2)]
        w_AkkT = m.sb("w_AkkT", [128, 4, 128], BF16); w_ArkT = m.sb("w_ArkT", [128, 4, 128], BF16); w_nArbT = m.sb("w_nArbT", [128, 4, 128], BF16)
        w_Z = m.sb("w_Z", [128, 4, 256]); w_Zb = m.sb("w_Zb", [128, 4, 256], BF16)
        w_RpT = m.sb("w_RpT", [128, 4, 128], BF16); w_Y0T = m.sb("w_Y0T", [128, 4, 128]); w_Gp = m.sb("w_Gp", [128, 4, 128], BF16)
        w_DdW = m.sb("w_DdW", [128, 4, 128]); w_st1 = m.sb("w_st1", [128, 128])
        w_ST = m.sb("w_ST", [128, 128]); w_STb = [m.sb("w_STb%d" % i, [128, 128], BF16) for i in range(2)]
        w_y = m.sb("w_y", [128, T]); w_ysq = rysq; w_mean = rmean; w_var = rvar
        m.memset('dve', w_ST[:], 0.0); m.memset('pool', w_STb[0][:], 0.0)
        wsi = [0]

        def rwkv(t):
            ts_ = slice(t * T, (t + 1) * T)
            m.dma_in(w_in[:], rw_in.rearrange("f p s -> p f s")[:, :, t * T:t * T + T + 1])
            if has_vres:
                m.dma_in(w_vf[:], vfirst[:, ts_])
            for b in range(8):
                m.ts('pool', w_tmp[:], w_in[:, b, 0:T], pp[:, PP_MU + b:PP_MU + b + 1], ALU.mult)
                m.stt(w_mx[:, b, :], w_in[:, b, 1:T + 1], omu[:, b:b + 1], w_tmp[:], ALU.mult, ALU.add)
            r_, k_, vm = w_mx[:, 0, :], w_mx[:, 1, :], w_mx[:, 2, :]
            m.act(w_thb[:], w_mx[:, 3, :], AF.Tanh)
            m.cp('pool', w_xab[:], w_mx[:, 4, :])
            m.act(w_sgb[:], w_mx[:, 5:7, :], AF.Sigmoid)
            p = m.pz(); m.mm(p[:], rwb[:, 0, :], w_thb[:])
            m.act(w_lw[:], p[:], AF.Sigmoid, bias=col(PP_W0))
            m.ts('dve', w_lw[:], w_lw[:], -EXPM05, ALU.mult)
            p = m.pz(); m.mm(p[:], rwb[:, 1, :], w_xab[:])
            m.act(w_a[:], p[:], AF.Sigmoid, bias=col(PP_A0))
            p = m.pz()
            m.mm(p[:], rwb[:, 2, :], w_sgb[:, 0, :], start=True, stop=False)
            m.mm(p[:], rwb[:, 3, :], w_sgb[:, 1, :], start=False, stop=True)
            m.cp('act', w_g[:], p[:])
            if has_vres:
                m.cp('pool', w_pvb[:], w_mx[:, 7, :])
                p = m.pz(); m.mm(p[:], rwb[:, 4, :], w_pvb[:])
                m.act(w_tmp[:], p[:], AF.Sigmoid, bias=col(PP_V0))
                m.tt('dve', w_v[:], w_vf[:], vm, ALU.subtract)
                m.tt('dve', w_v[:], w_v[:], w_tmp[:], ALU.mult)
                m.tt('dve', w_v[:], w_v[:], vm, ALU.add)
            else:
                m.cp('pool', w_v[:], vm)
                m.dma_out(v_out[:, ts_], w_v[:])
            m.ts('dve', w_kk[:], k_, col(PP_KK), ALU.mult)
            m.tt('pool', w_tmp[:], w_kk[:], w_kk[:], ALU.mult)
            p = m.pz(); m.mm(p[:], cm[:, CM_BONES, :], w_tmp[:])
            m.act(w_tmp[:], p[:], AF.Sqrt)
            m.ts('dve', w_tmp[:], w_tmp[:], 1e-12, ALU.max)
            m.kb.op('dve', lambda en: en.reciprocal(out=w_tmp.ap[:], in_=w_tmp.ap[:]), reads=[w_tmp], writes=[w_tmp])
            m.tt('dve', w_kk[:], w_kk[:], w_tmp[:], ALU.mult)
            m.ts('dve', w_km[:], w_a[:], -1.0, ALU.add, col(PP_KA), ALU.mult)
            m.stt(w_km[:], w_km[:], 1.0, k_, ALU.add, ALU.mult)
            m.tt('pool', w_b[:], w_kk[:], w_a[:], ALU.mult)
            m.stt(w_tmp[:], r_, col(PP_RK), w_km[:], ALU.mult, ALU.mult)
            p = m.pz(); m.mm(p[:], cm[:, CM_BONES, :], w_tmp[:])
            m.tt('dve', w_bon[:], p[:], w_v[:], ALU.mult)
            for c in range(NCH):
                cs = slice(c * 64, (c + 1) * 64)
                m.scan(w_L[:, cs], ones_f[:, 0:64], w_lw[:, cs], 0.0, ALU.mult, ALU.add)
            m.act(w_W[:], w_L[:], AF.Exp)
            m.act(w_Wi[:], w_L[:], AF.Exp, scale=-1.0)
            m.tt('pool', w_Wp[:], w_L[:], w_lw[:], ALU.subtract)
            m.act(w_Wp[:], w_Wp[:], AF.Exp)
            for name, x0, x1 in (("RT", r_, w_W[:]), ("KT", w_km[:], w_Wi[:]), ("BT", w_b[:], w_Wi[:]), ("KA", w_kk[:], w_Wp[:])):
                for hh in range(2):
                    ps_ = slice(hh * 64, (hh + 1) * 64)
                    m.tt('dve', BDf[name][ps_, :, hh * 64:(hh + 1) * 64], x0[ps_, :].rr("p (c x) -> p c x", x=64),
                         x1[ps_, :].rr("p (c x) -> p c x", x=64), ALU.mult)
            for hh in range(2):
                ps_ = slice(hh * 64, (hh + 1) * 64)
                m.cp('pool', BDf["VF"][ps_, :, hh * 64:(hh + 1) * 64], w_v[ps_, :].rr("p (c x) -> p c x", x=64))
            RT, KT, BT, KA, VF = (BDf[n] for n in BDn)
            stage = float(getattr(build_mixer, "stage", 9))
            if stage <= 1:
                m.dma_out(y_rw[:, ts_], w_bon[:])
                return
            if stage <= 1.65:
                m.cp('dve', w_y[:], w_bon[:])
            mk = lambda i: cf[:, i, :].rr("p (c x) -> p c x", c=4)
            p4 = lambda pb: pb[:].rr("p (c x) -> p c x", c=4)
            pY = m.bank[6]
            for qd in range(NCH // 4 if stage > 1.25 else 0):
                c0 = qd * 4
                pa, pb_, pc, pd = m.pz(), m.pz(), m.pz(), m.pz()
                for c in range(4):
                    cs = slice(c * 128, (c + 1) * 128)
                    m.mm(pa[:, cs], VF[:, c0 + c, :], ident)
                    m.mm(pb_[:, cs], KA[:, c0 + c, :], ident)
                    m.mm(pc[:, cs], KT[:, c0 + c, :], ident)
                    m.mm(pd[:, cs], BT[:, c0 + c, :], ident)
                m.cp('act', w_Vt[:], p4(pa))
                m.cp('act', w_Z[:, :, 128:256], p4(pb_))
                m.cp('dve', w_Zb[:, :, 128:256], p4(pb_))
                m.cp('act', w_KTt[:], p4(pc))
                m.ts('dve', w_nBTt[:], p4(pd), -1.0, ALU.mult)
                if stage <= 1.5:
                    continue
                if stage <= 1.55:
                    m.dma_out(y_rw[:, ts_], w_bon[:])
                    return
                pa, pb_, pc, pd, pe_ = m.pz(), m.pz(), m.pz(), m.pz(), m.pz()
                for c in range(4):
                    cs = slice(c * 128, (c + 1) * 128)
                    m.mm(pa[:, cs], KA[:, c0 + c, :], BT[:, c0 + c, :])
                    m.mm(pb_[:, cs], BT[:, c0 + c, :], KA[:, c0 + c, :])
                    m.mm(pc[:, cs], KT[:, c0 + c, :], KA[:, c0 + c, :])
                    m.mm(pd[:, cs], KT[:, c0 + c, :], RT[:, c0 + c, :])
                    m.mm(pe_[:, cs], BT[:, c0 + c, :], RT[:, c0 + c, :])
                m.tt('dve', w_N[0][:], p4(pa), mk(CF_NML), ALU.mult)
                m.tt('dve', w_NT[0][:], p4(pb_), mk(CF_NMU), ALU.mult)
                m.tt('dve', w_AkkT[:], p4(pc), mk(CF_MU), ALU.mult)
                m.tt('dve', w_ArkT[:], p4(pd), mk(CF_MUI), ALU.mult)
                m.tt('dve', w_nArbT[:], p4(pe_), mk(CF_NMUI), ALU.mult)
                if stage <= 1.7:
                    continue
                pa = m.pz()
                for c in range(4):
                    m.mm(pa[:, c * 128:(c + 1) * 128], w_AkkT[:, c, :], w_Vt[:, c, :])
                m.cp('act', w_Z[:, :, 0:128], p4(pa))
                m.cp('dve', w_Zb[:, :, 0:128], p4(pa))
                if stage <= 2:
                    continue
                for j in range(6):
                    cur, nxt = j % 2, (j + 1) % 2
                    pz1, pz2 = m.pz(), m.pz()
                    for c in range(4):
                        pzz = (pz1, pz2)[c // 2]
                        m.mm(pzz[:, (c % 2) * 256:(c % 2 + 1) * 256], w_NT[cur][:, c, :], w_Zb[:, c, :])
                    for hf, pzz in enumerate((pz1, pz2)):
                        zs = w_Z[:, hf * 2:hf * 2 + 2, :]
                        m.tt('dve', zs, pzz[:].rr("p (c x) -> p c x", c=2), zs, ALU.add)
                        m.cp('act', w_Zb[:, hf * 2:hf * 2 + 2, :], zs)
                    if j < 5:
                        pn, pnt = m.pz(), m.pz()
                        for c in range(4):
                            cs = slice(c * 128, (c + 1) * 128)
                            m.mm(pn[:, cs], w_NT[cur][:, c, :], w_N[cur][:, c, :])
                            m.mm(pnt[:, cs], w_N[cur][:, c, :], w_NT[cur][:, c, :])
                        m.cp('act', w_N[nxt][:], p4(pn))
                        m.cp('dve', w_NT[nxt][:], p4(pnt))
                if stage <= 3:
                    continue
                U0 = lambda c: w_Zb[:, c, 0:128]
                Ktp = lambda c: w_Zb[:, c, 128:256]
                pa, pb_, pc, pd = m.pz(), m.pz(), m.pz(), m.pz()
                for c in range(4):
                    cs = slice(c * 128, (c + 1) * 128)
                    m.mm(pa[:, cs], Ktp(c), w_nArbT[:, c, :])
                    m.mm(pb_[:, cs], w_Vt[:, c, :], w_ArkT[:, c, :], start=True, stop=False)
                    m.mm(pb_[:, cs], U0(c), w_nArbT[:, c, :], start=False, stop=True)
                    m.mm(pc[:, cs], Ktp(c), w_nBTt[:, c, :])
                    m.mm(pd[:, cs], w_KTt[:, c, :], w_Vt[:, c, :], start=True, stop=False)
                    m.mm(pd[:, cs], w_nBTt[:, c, :], U0(c), start=False, stop=True)
                m.tt('dve', w_RpT[:], p4(pa), RT[:, c0:c0 + 4, :], ALU.add)
                m.cp('act', w_Y0T[:], p4(pb_))
                m.cp('act', w_Gp[:], p4(pc))
                for c in range(4):
                    wc = w_W[:, (c0 + c) * 64 + 63:(c0 + c) * 64 + 64]
                    m.ts('dve', w_DdW[:, c, :], pd[:, c * 128:(c + 1) * 128], wc, ALU.mult)
                if stage <= 4:
                    continue
                for c in range(4):
                    cs = slice(c * 128, (c + 1) * 128)
                    wc = w_W[:, (c0 + c) * 64 + 63:(c0 + c) * 64 + 64]
                    stb = w_STb[wsi[0]]
                    m.mm(pY[:, cs], stb[:], w_RpT[:, c, :])
                    pst = m.pz()
                    m.mm(pst[:, 0:128], w_Gp[:, c, :], stb[:])
                    m.stt(w_st1[:], pst[:, 0:128], wc, w_DdW[:, c, :], ALU.mult, ALU.add)
                    m.stt(w_ST[:], w_ST[:], wc, w_st1[:], ALU.mult, ALU.add)
                    wsi[0] ^= 1
                    m.cp('act', w_STb[wsi[0]][:], w_ST[:])
                for hh in range(2):
                    ps_ = slice(hh * 64, (hh + 1) * 64)
                    m.tt('dve', w_y[ps_, qd * 256:(qd + 1) * 256].rr("p (c x) -> p c x", x=64),
                         p4(pY)[ps_, :, hh * 64:(hh + 1) * 64], w_Y0T[ps_, :, hh * 64:(hh + 1) * 64], ALU.add)
            m.tt('pool', w_ysq[:], w_y[:], w_y[:], ALU.mult)
            pm, pq = m.pz(), m.pz()
            m.mm(pm[:], cm[:, CM_BONES64, :], w_y[:])
            m.mm(pq[:], cm[:, CM_BONES64, :], w_ysq[:])
            m.cp('act', w_mean[:], pm[:])
            m.tt('dve', w_var[:], w_mean[:], w_mean[:], ALU.mult)
            m.tt('dve', w_var[:], pq[:], w_var[:], ALU.subtract)
            m.act(w_var[:], w_var[:], AF.Sqrt, bias=epsb[:, 1:2])
            m.kb.op('dve', lambda en: en.reciprocal(out=w_var.ap[:], in_=w_var.ap[:]), reads=[w_var], writes=[w_var])
            m.tt('dve', w_y[:], w_y[:], w_mean[:], ALU.subtract)
            m.tt('dve', w_y[:], w_y[:], w_var[:], ALU.mult)
            m.ts('dve', w_y[:], w_y[:], col(PP_LNW), ALU.mult, col(PP_LNB), ALU.add)
            m.tt('dve', w_y[:], w_y[:], w_bon[:], ALU.add)
            m.tt('dve', w_y[:], w_y[:], w_g[:], ALU.mult)
            m.dma_out(y_rw[:, ts_], w_y[:])

        which = getattr(build_mixer, "which", "rsw")
        for t in range(NTL):
            if "r" in which:
                retention(t)
            if "s" in which:
                ssd(t)
            if "w" in which:
                rwkv(t)
        kb.finish()
        print("mixer instrs", kb.ninst, "sems", kb.nsem, flush=True)
    return nc


def arrange_wd(W):
    return np.ascontiguousarray(W.reshape(2, 22, 128, 16, 128).transpose(3, 0, 2, 1, 4)).reshape(16, 2, 128, 22 * 128)


import ml_dtypes


def mixer_consts(c):
    gamma = 1.0 - 2.0 ** (-5 - c)
    idx = np.arange(128)
    cf = np.zeros((128, NCF, 512), np.float64)
    i4 = np.tile(idx, 4)
    rel = i4[None, :] - idx[:, None]
    cf[:, CF_MASKD, :] = np.where(rel >= 0, gamma ** np.maximum(rel, 0), 0.0) * (128 ** -0.5)
    cf[:, CF_XI, :] = (gamma ** (i4 + 1.0))[None, :]
    cf[:, CF_NEGM, :] = np.where(rel >= 0, 0.0, -30000.0)
    same = (idx[:, None] // 64) == (idx[None, :] // 64)
    lo = same & ((idx[:, None] % 64) > (idx[None, :] % 64))
    up = same & ((idx[:, None] % 64) < (idx[None, :] % 64))
    upi = same & ((idx[:, None] % 64) <= (idx[None, :] % 64))
    cf[:, CF_NML, :] = np.tile(-lo.astype(np.float64), (1, 4))
    cf[:, CF_NMU, :] = np.tile(-up.astype(np.float64), (1, 4))
    cf[:, CF_MU, :] = np.tile(up.astype(np.float64), (1, 4))
    cf[:, CF_MUI, :] = np.tile(upi.astype(np.float64), (1, 4))
    cf[:, CF_NMUI, :] = np.tile(-upi.astype(np.float64), (1, 4))
    cm = np.zeros((128, NCM, 128), np.float64)
    cm[:, CM_ONES128, :] = 1.0 / 128
    cm[:, CM_BONES, :] = same
    cm[:, CM_BONES64, :] = same / 64.0
    for h in range(4):
        cm[h, CM_SEL + h, :] = 1.0
    for b in range(2):
        for mcol in range(128):
            cm[2 * b + mcol // 64, CM_SEL2 + b, mcol] = 1.0
    cm[0:4, CM_I4, 0:4] = np.eye(4)
    cm[:, CM_IDENT, :] = np.eye(128)
    cb = np.zeros((128, NCB, 128), np.float32)
    cb[:, CB_IDENT, :] = np.eye(128)
    cb[(idx + 64) % 128, CB_SWAP, idx] = 1.0
    gc = gamma ** 128
    zeta = gamma ** (127.0 - idx) * (128 ** -0.5)
    return cf.astype(np.float32), cm.astype(np.float32), cb.astype(ml_dtypes.bfloat16), np.float32(gc), zeta.astype(np.float32)


def rope_tables(S):
    half = 64
    inv_freq = (10000.0 ** (-np.arange(half, dtype=np.float32) / half)).astype(np.float32)
    ang = np.arange(S, dtype=np.float32)[:, None] * inv_freq[None, :]
    cos = np.cos(ang.astype(np.float64)); sin = np.sin(ang.astype(np.float64))
    t = np.zeros((2, 128, S), np.float32)
    t[0, :64] = cos.T; t[0, 64:] = cos.T
    t[1, :64] = -sin.T; t[1, 64:] = sin.T
    return t


def mixer_params(c, l, P, gc, zeta):
    pp = np.zeros((128, 64), np.float32); p = np.arange(128)
    g = c // 2
    chans = [c * 256 + p, c * 256 + 128 + p, 1024 + g * 128 + p, 1280 + g * 128 + p]
    for b, ch in enumerate(chans):
        for k in range(4):
            pp[:, PP_CONVW + b * 4 + k] = P['ssm_conv_w'][l, k, ch]
        pp[:, PP_CONVB + b] = P['ssm_conv_b'][l, ch]
    for b in range(2):
        pp[:, PP_DSKIP + b] = P['ssm_d'][l, 4 * c + 2 * b + p // 64]
    pp[0:4, PP_DTB] = P['ssm_dt_bias'][l, 4 * c:4 * c + 4]
    pp[0:4, PP_ALOG] = P['ssm_a_log'][l, 4 * c:4 * c + 4]
    mu = P['rwkv_mu'][l]
    for b in range(3):
        pp[:, PP_MU + b] = mu[b * 512 + c * 128 + p]
    pp[0:96, PP_MU + 3] = mu[1536:1632]; pp[0:96, PP_MU + 4] = mu[1632:1728]
    pp[:, PP_MU + 5] = mu[1728:1856]; pp[:, PP_MU + 6] = mu[1856:1984]
    sl = slice(c * 128, (c + 1) * 128)
    if l > 0:
        pp[0:64, PP_MU + 7] = P['rwkv_mu_v'][l - 1]
        pp[:, PP_V0] = P['rwkv_v0'][l - 1, sl]
    pp[:, PP_W0] = P['rwkv_w0'][l, sl]; pp[:, PP_A0] = P['rwkv_a0'][l, sl]
    pp[:, PP_KK] = P['rwkv_k_k'][l, sl]; pp[:, PP_KA] = P['rwkv_k_a'][l, sl]
    pp[:, PP_RK] = P['rwkv_r_k'][l].reshape(512)[sl]
    pp[:, PP_LNW] = P['rwkv_ln_w'][l, sl]; pp[:, PP_LNB] = P['rwkv_ln_b'][l, sl]
    pp[:, PP_GC] = gc; pp[:, PP_ZETA] = zeta
    rww = np.zeros((128, 5, 128), np.float32)
    rww[0:96, 0] = P['rwkv_w2'][l][:, sl]; rww[0:96, 1] = P['rwkv_a2'][l][:, sl]
    rww[:, 2] = P['rwkv_g2'][l][0:128, sl]; rww[:, 3] = P['rwkv_g2'][l][128:256, sl]
    if l > 0:
        rww[0:64, 4] = P['rwkv_v2'][l - 1][:, sl]
    return pp, rww


def mixer_inputs(c, PJ):
    S = PJ.shape[1]; g = c // 2
    pad = lambda a, n: np.concatenate([np.zeros((a.shape[0], n), np.float32), a], axis=1)
    rows = lambda a: np.concatenate([a, np.zeros((128 - a.shape[0], a.shape[1]), np.float32)], axis=0) if a.shape[0] < 128 else a
    ret_in = np.stack([PJ[f * 512 + c * 128:f * 512 + (c + 1) * 128] for f in range(4)])
    ssd_z = np.stack([PJ[2048 + c * 256 + b * 128:2048 + c * 256 + (b + 1) * 128] for b in range(2)])
    ssd_c = np.stack([pad(PJ[3072 + c * 256:3072 + c * 256 + 128], 3), pad(PJ[3072 + c * 256 + 128:3072 + c * 256 + 256], 3),
                      pad(PJ[4096 + g * 128:4096 + (g + 1) * 128], 3), pad(PJ[4352 + g * 128:4352 + (g + 1) * 128], 3)])
    ssd_dt = np.ascontiguousarray(PJ[4608 + 4 * c:4608 + 4 * c + 4])
    R0 = 4624
    blocks = [PJ[R0 + c * 128:R0 + (c + 1) * 128], PJ[R0 + 512 + c * 128:R0 + 512 + (c + 1) * 128],
              PJ[R0 + 1024 + c * 128:R0 + 1024 + (c + 1) * 128], rows(PJ[6160:6256]), rows(PJ[6256:6352]),
              PJ[6352:6480], PJ[6480:6608], rows(PJ[6608:6672])]
    rw_in = np.stack([pad(b_, 1) for b_ in blocks])
    return {"ret_in": np.ascontiguousarray(ret_in), "ssd_z": np.ascontiguousarray(ssd_z), "ssd_c": np.ascontiguousarray(ssd_c),
            "ssd_dt": ssd_dt, "rw_in": np.ascontiguousarray(rw_in)}


_PROGS = {}


def _prog(key, fn):
    if key not in _PROGS:
        _PROGS[key] = fn()
    return _PROGS[key]


def kernel(**inputs):
    P = {k: np.asarray(v, dtype=np.float32) for k, v in inputs.items()}
    x = P['x']
    NCORE = 8
    cores = [(b, c) for b in range(BATCH) for c in range(4)]
    xT = [np.ascontiguousarray(x[b, c * NTOK:(c + 1) * NTOK, :].T) for b, c in cores]
    consts = [mixer_consts(c) for c in range(4)]
    rope = rope_tables(SEQ)
    vfirst = [None] * NCORE
    yT = [None] * NCORE
    out = np.zeros((BATCH, SEQ, D_MODEL), np.float32)
    for l in range(DEPTH + 1):
        has_c, has_a, final = l > 0, l < DEPTH, l == DEPTH
        nc = _prog(("dense", has_c, has_a, final), lambda: build_dense(has_c, has_a, final))
        vecs = np.zeros((128, 64), np.float32)
        common = {"vecs": vecs}
        if has_a:
            vecs[:, 0:16] = vec_pk(P['norm_mix_w'][l])
            common["win"] = arrange_w(P['w_in_first'] if l == 0 else P['w_in_rest'][l - 1], GW, NG_IN)
        if has_c:
            vecs[:, 16:32] = vec_pk(P['norm_ffn_w'][l - 1])
            vecs[:, 32:48] = vec_pk(P['final_norm_w'])
            vecs[:, 48:56] = vec_pk(P['ssm_norm_w'][l - 1])
            common["wout"] = arrange_w(P['w_out'][l - 1], GW)
            common["wg"] = arrange_w(P['ffn_w_gate'][l - 1], GW)
            common["wu"] = arrange_w(P['ffn_w_up'][l - 1], GW)
            common["wd"] = arrange_wd(P['ffn_w_down'][l - 1])
        in_maps = []
        for i in range(NCORE):
            d = dict(common); d["xT"] = xT[i]
            if has_c:
                d["yT"] = yT[i]
            in_maps.append(d)
        res = run_bass_kernel_spmd(nc, in_maps, core_ids=list(range(NCORE))).results
        if has_c:
            xT = [np.asarray(res[i]["xo"]) for i in range(NCORE)]
        if final:
            break
        ncm = _prog(("mixer", l > 0), lambda: build_mixer(SEQ, l > 0))
        in_maps = []
        for b in range(BATCH):
            PJ = np.concatenate([np.asarray(res[b * 4 + c]["pj"]) for c in range(4)], axis=1)
            for c in range(4):
                cf, cm, cb, gc, zeta = consts[c]
                pp, rww = mixer_params(c, l, P, gc, zeta)
                d = mixer_inputs(c, PJ)
                d.update({"rope": rope, "pp": pp, "cf": cf, "cm": cm, "cb": cb, "rww": rww})
                if l > 0:
                    d["vfirst"] = vfirst[b * 4 + c]
                in_maps.append(d)
            del PJ
        res2 = run_bass_kernel_spmd(ncm, in_maps, core_ids=list(range(NCORE))).results
        for b in range(BATCH):
            Y = np.zeros((D_MODEL, SEQ), np.float32)
            for c in range(4):
                r = res2[b * 4 + c]
                Y[c * 128:(c + 1) * 128] = np.asarray(r["y_ret"])
                Y[512 + c * 256:512 + (c + 1) * 256] = np.asarray(r["y_ssd"]).reshape(256, SEQ)
                Y[1536 + c * 128:1536 + (c + 1) * 128] = np.asarray(r["y_rw"])
                if l == 0:
                    vfirst[b * 4 + c] = np.ascontiguousarray(np.asarray(r["v_out"]))
            for c in range(4):
                yT[b * 4 + c] = np.ascontiguousarray(Y[:, c * NTOK:(c + 1) * NTOK])
    for i, (b, c) in enumerate(cores):
        out[b, c * NTOK:(c + 1) * NTOK, :] = xT[i].T
    return out
```

```python
from contextlib import ExitStack
import numpy as np
import concourse.bass as bass
import concourse.mybir as mybir
from concourse.bass_utils import run_bass_kernel_spmd

F32 = mybir.dt.float32
BF16 = mybir.dt.bfloat16
AF = mybir.ActivationFunctionType
ALU = mybir.AluOpType

D_MODEL = 2048; BATCH = 2; SEQ = 8192; DEPTH = 4
IN_COLS = 6608; MV = 64; D_FF = 5632
TT = 512
NTOK = 2048
KC = 16
FC = 44
GW = 256
NG_IN = 27
PROJ_ROWS = NG_IN * GW


class Buf:
    def __init__(self, ap, name, parent=None):
        self.ap = ap; self.name = name
        self.root = parent.root if parent is not None else self
        if parent is None:
            self._w = None
            self._r = {}

    w = property(lambda self: self.root._w, lambda self, v: setattr(self.root, '_w', v))
    r = property(lambda self: self.root._r, lambda self, v: setattr(self.root, '_r', v))

    def __getitem__(self, idx):
        return self.ap[idx]


class KB:
    ROT = 24000

    def __init__(self, nc, es):
        self.nc = nc; self.es = es
        self.eng = {'pe': nc.tensor, 'dve': nc.vector, 'act': nc.scalar, 'pool': nc.gpsimd, 'sp': nc.sync}
        self.sems = {}
        self.cur = {}
        self.dcnt = {}
        self.waited = {e: {} for e in self.eng}
        self.nsem = 0
        self.out_sems = []
        self.dq = 0
        self.ninst = 0

    def _newsem(self, key):
        h = self.es.enter_context(self.nc.semaphore("s%d" % self.nsem)); self.nsem += 1
        self.sems[key] = h
        return h

    def sb(self, name, shape, dtype):
        t = self.es.enter_context(self.nc.sbuf_tensor("sb_" + name, list(shape), dtype))
        return Buf(t, name)

    def ps(self, name, shape=(128, 512), dtype=F32):
        t = self.es.enter_context(self.nc.psum_tensor("pp_" + name, list(shape), dtype))
        b = Buf(t, name); b.psum = True
        return b

    def _eng_token(self, e):
        key, cnt = self.cur.get(e, (None, 0))
        if key is None or cnt >= self.ROT:
            key = (e, len([k for k in self.sems if k[0] == e]))
            self._newsem(key); cnt = 0
        cnt += 1
        self.cur[e] = (key, cnt)
        return key, cnt

    def _wait(self, e, deps):
        w = self.waited[e]
        for key, val in deps.items():
            if w.get(key, 0) >= val:
                continue
            self.eng[e].wait_ge(self.sems[key], val)
            w[key] = val

    def _deps(self, reads, writes, e):
        deps = {}
        def add(k, v):
            if deps.get(k, 0) < v:
                deps[k] = v
        for b in reads:
            if b.w is not None:
                add(*b.w)
            if getattr(b.root, 'psum', False):
                for k, v in b.r.items():
                    if k[0] != e:
                        add(k, v)
        for b in writes:
            if getattr(b.root, 'multi', False):
                continue
            if b.w is not None:
                add(*b.w)
            for k, v in b.r.items():
                add(k, v)
        return deps

    def op(self, e, fn, reads=(), writes=(), pe_acc=False):
        deps = self._deps(reads, writes, e)
        if pe_acc:
            deps = {k: v for k, v in deps.items() if k[0] != 'pe'}
        self._wait(e, deps)
        key, cnt = self._eng_token(e)
        ins = fn(self.eng[e])
        ins.then_inc(self.sems[key], 1)
        self.ninst += 1
        for b in reads:
            if b.r.get(key, 0) < cnt:
                b.r[key] = cnt
        for b in writes:
            b.w = (key, cnt); b.r = {}
        return ins

    def dma(self, out_ap, in_ap, reads=(), writes=(), q=None, is_out=False):
        if q is None:
            q = ('sp', 'pool')[self.dq % 2]; self.dq += 1
        deps = self._deps(reads, writes, q)
        self._wait(q, deps)
        tgt = writes[0] if writes else reads[0]
        key = ('dma', tgt.root.name, 'w' if writes else 'r')
        if key not in self.sems:
            self._newsem(key); self.dcnt[key] = 0
        self.dcnt[key] += 16
        val = self.dcnt[key]
        self.eng[q].dma_start(out=out_ap, in_=in_ap).then_inc(self.sems[key], 16)
        self.ninst += 1
        for b in reads:
            if b.r.get(key, 0) < val:
                b.r[key] = val
        for b in writes:
            b.w = (key, val); b.r = {}
        if is_out:
            self.out_sems = [(k, v) for k, v in self.out_sems if k != key] + [(key, val)]

    def finish(self):
        for key, val in self.out_sems:
            self.eng['sp'].wait_ge(self.sems[key], val)


class Dense:
    def __init__(self, kb, sq=None):
        self.kb = kb
        self.xT = kb.sb("xT", [128, KC, TT], F32)
        self.hT = kb.sb("hT", [128, KC, TT], BF16)
        self.sq = sq if sq is not None else kb.sb("sq", [128, KC, TT], BF16)
        self.rstd = kb.sb("rstd", [128, TT], F32)
        self.ones = kb.sb("ones", [128, 128], BF16)
        self.wst = [kb.sb("wst%d" % i, [128, KC * GW], F32) for i in range(2)]
        self.wbf = [kb.sb("wbf%d" % i, [128, KC * GW], BF16) for i in range(2)]
        self.wi = 0
        self.ost = [kb.sb("ost%d" % i, [128, TT], F32) for i in range(2)]
        self.oi = 0
        self.pss = kb.ps("ps_ss")
        self.psm = [kb.ps("ps_m%d" % i) for i in range(4)]
        self.pi = 0
        self.ci = 0
        kb.op('dve', lambda e: e.memset(self.ones[:], 1.0), writes=[self.ones])

    def load_w(self, w_ap, nk, gw, cache=None, first=True):
        kb = self.kb
        i = self.wi; self.wi ^= 1
        st, bf = self.wst[i], self.wbf[i]
        n = nk * gw
        if cache is not None and not first:
            kb.dma(bf[:, 0:n], cache[1], reads=[cache[0]], writes=[bf], q='sp')
            return bf, bf[:, 0:n].rearrange("p (k m) -> p k m", m=gw)
        kb.dma(st[:, 0:n], w_ap, writes=[st], q='sp')
        ce = ('dve', 'pool', 'act')[self.ci % 3]; self.ci += 1
        if ce == 'act':
            kb.op(ce, lambda e: e.copy(out=bf[:, 0:n], in_=st[:, 0:n]), reads=[st], writes=[bf])
        else:
            kb.op(ce, lambda e: e.tensor_copy(out=bf[:, 0:n], in_=st[:, 0:n]), reads=[st], writes=[bf])
        if cache is not None:
            kb.dma(cache[1], bf[:, 0:n], reads=[bf], writes=[cache[0]], q='act')
        return bf, bf[:, 0:n].rearrange("p (k m) -> p k m", m=gw)

    def next_ps(self):
        p = self.psm[self.pi]; self.pi = (self.pi + 1) % 4
        return p

    def rmsnorm(self, nw, off, eps, dst_f32=None):
        kb = self.kb; xT, sq, pss, rstd = self.xT, self.sq, self.pss, self.rstd
        kb.op('act', lambda e: e.activation(out=sq[:], in_=xT[:], func=AF.Square), reads=[xT], writes=[sq])
        for k in range(KC):
            kb.op('pe', lambda e, k=k: e.matmul(pss[:], lhsT=self.ones[:], rhs=sq[:, k, :], start=(k == 0), stop=(k == KC - 1)),
                  reads=[self.ones, sq], writes=[pss], pe_acc=(k > 0))
        kb.op('act', lambda e: e.activation(out=rstd[:], in_=pss[:], func=AF.Sqrt, scale=1.0 / D_MODEL, bias=self.epsb(eps)),
              reads=[pss, self.eps_buf], writes=[rstd])
        kb.op('dve', lambda e: e.reciprocal(out=rstd[:], in_=rstd[:]), reads=[rstd], writes=[rstd])
        dst = self.hT if dst_f32 is None else dst_f32
        for k in range(KC):
            en = 'dve'
            kb.op(en, lambda e, k=k: e.scalar_tensor_tensor(out=dst[:, k, :], in0=xT[:, k, :], scalar=nw[:, off + k:off + k + 1], in1=rstd[:],
                                                          op0=ALU.mult, op1=ALU.mult),
                  reads=[xT, nw, rstd], writes=[dst])

    def epsb(self, eps):
        return self.eps_buf[:, self.eps_idx[eps]:self.eps_idx[eps] + 1]

    def init_eps(self, vals):
        kb = self.kb
        self.eps_buf = kb.sb("epsb", [128, len(vals)], F32)
        self.eps_idx = {v: i for i, v in enumerate(vals)}
        for i, v in enumerate(vals):
            kb.op('dve', lambda e, i=i, v=v: e.memset(self.eps_buf[:, i:i + 1], v), writes=[self.eps_buf])

    def gemm(self, w_dram, ng, nk, gw, rhs, consume, cache=None, first=True):
        kb = self.kb
        for g in range(ng):
            bf, wv = self.load_w(w_dram[g], nk, gw, None if cache is None else (cache[0], cache[1][g]), first)
            for m in range(gw // 128):
                ps = self.next_ps()
                for k in range(nk):
                    kb.op('pe', lambda e, k=k, m=m: e.matmul(ps[:], lhsT=wv[:, k, m * 128:(m + 1) * 128], rhs=rhs[:, k, :],
                                                            start=(k == 0), stop=(k == nk - 1)),
                          reads=[bf, rhs], writes=[ps], pe_acc=(k > 0))
                consume(g * (gw // 128) + m, ps)

    def gemm_down(self, w_dram, rhs, consume, cache=None, first=True):
        kb = self.kb
        for g in range(16):
            ps = self.next_ps()
            for hf in range(2):
                bf, wv = self.load_w(w_dram[g, hf], FC // 2, 128, None if cache is None else (cache[0], cache[1][g, hf]), first)
                for k in range(FC // 2):
                    kk = hf * (FC // 2) + k
                    kb.op('pe', lambda e, k=k, kk=kk: e.matmul(ps[:], lhsT=wv[:, k, :], rhs=rhs[:, kk, :],
                                                              start=(kk == 0), stop=(kk == FC - 1)),
                          reads=[bf, rhs], writes=[ps], pe_acc=(kk > 0))
            consume(g, ps)


def build_dense(has_c, has_a, final):
    nc = bass.Bass("TRN2", target_bir_lowering=False)
    dr = lambda n, s, kind="ExternalInput": nc.dram_tensor(n, list(s), F32, kind=kind).ap()
    xT_d = dr("xT", [D_MODEL, NTOK])
    vecs = dr("vecs", [128, 64])
    if has_c:
        yT_d = dr("yT", [D_MODEL, NTOK])
        wout_d = dr("wout", [8, 128, KC * GW])
        wg_d = dr("wg", [22, 128, KC * GW])
        wu_d = dr("wu", [22, 128, KC * GW])
        wd_d = dr("wd", [16, 2, 128, (FC // 2) * 128])
        xo_d = dr("xo", [D_MODEL, NTOK], "ExternalOutput")
    if has_a:
        win_d = dr("win", [NG_IN, 128, KC * GW])
        pj_d = dr("pj", [PROJ_ROWS, NTOK], "ExternalOutput")
    scr = lambda n, shp: nc.dram_tensor(n, list(shp), BF16).ap()
    wscr = Buf(None, "wscr"); wscr.multi = True
    if has_c:
        c_out_s = (wscr, scr("wout_bf", [8, 128, KC * GW])); c_g = (wscr, scr("wg_bf", [22, 128, KC * GW]))
        c_u = (wscr, scr("wu_bf", [22, 128, KC * GW])); c_d = (wscr, scr("wd_bf", [16, 2, 128, (FC // 2) * 128]))
    if has_a:
        c_in_s = (wscr, scr("win_bf", [NG_IN, 128, KC * GW]))
    with ExitStack() as es:
        kb = KB(nc, es)
        if has_c:
            aT = kb.sb("aT", [128, FC, TT], BF16)
            dn = Dense(kb, sq=Buf(aT.ap[:, 0:KC, :], "aT", parent=aT))
        else:
            dn = Dense(kb)
        dn.init_eps([1e-6, 1e-5])
        vb = kb.sb("vecs_sb", [128, 64], F32)
        kb.dma(vb[:], vecs, writes=[vb], q='sp')
        xT = dn.xT
        if has_c:
            yT = kb.sb("yT_sb", [128, KC, TT], F32)
            yb = dn.hT
            sg = [kb.sb("sg%d" % i, [128, TT], F32) for i in range(2)]
            psg = [kb.ps("ps_g%d" % i) for i in range(2)]
        for t in range(NTOK // TT):
            ts = slice(t * TT, (t + 1) * TT)
            kb.dma(xT[:], xT_d.rearrange("(k p) n -> p k n", p=128)[:, :, ts], writes=[xT], q='sp')
            if has_c:
                kb.dma(yT[:], yT_d.rearrange("(k p) n -> p k n", p=128)[:, :, ts], writes=[yT], q='pool')
                sq = dn.sq
                kb.op('act', lambda e: e.activation(out=sq[:, 4:12, :], in_=yT[:, 4:12, :], func=AF.Square), reads=[yT], writes=[sq])
                for g in range(2):
                    for j in range(4):
                        k = 4 + g * 4 + j
                        kb.op('pe', lambda e, k=k, j=j: e.matmul(dn.pss[:], lhsT=dn.ones[:], rhs=sq[:, k, :], start=(j == 0), stop=(j == 3)),
                              reads=[dn.ones, sq], writes=[dn.pss], pe_acc=(j > 0))
                    kb.op('act', lambda e: e.activation(out=dn.rstd[:], in_=dn.pss[:], func=AF.Sqrt, scale=1.0 / 512, bias=dn.epsb(1e-5)),
                          reads=[dn.pss, dn.eps_buf], writes=[dn.rstd])
                    kb.op('dve', lambda e: e.reciprocal(out=dn.rstd[:], in_=dn.rstd[:]), reads=[dn.rstd], writes=[dn.rstd])
                    for j in range(4):
                        k = 4 + g * 4 + j
                        kb.op('dve', lambda e, k=k: e.scalar_tensor_tensor(out=yb[:, k, :], in0=yT[:, k, :], scalar=vb[:, 48 + k - 4:49 + k - 4],
                                                                      in1=dn.rstd[:], op0=ALU.mult, op1=ALU.mult),
                              reads=[yT, vb, dn.rstd], writes=[yb])
                for k in list(range(0, 4)) + list(range(12, 16)):
                    kb.op('pool', lambda e, k=k: e.tensor_copy(out=yb[:, k, :], in_=yT[:, k, :]), reads=[yT], writes=[yb])
                def c_out(r, ps):
                    kb.op('dve', lambda e: e.tensor_tensor(out=xT[:, r, :], in0=ps[:], in1=xT[:, r, :], op=ALU.add), reads=[ps, xT], writes=[xT])
                dn.gemm(wout_d, 8, KC, GW, yb, c_out, c_out_s, t == 0)
                dn.rmsnorm(vb, 16, 1e-6)
                for g in range(22):
                    bfg, wgv = dn.load_w(wg_d[g], KC, GW, (wscr, c_g[1][g]), t == 0)
                    bfu, wuv = dn.load_w(wu_d[g], KC, GW, (wscr, c_u[1][g]), t == 0)
                    for m in range(2):
                        pg, pu = psg
                        for k in range(KC):
                            kb.op('pe', lambda e, k=k, m=m: e.matmul(pg[:], lhsT=wgv[:, k, m * 128:(m + 1) * 128], rhs=dn.hT[:, k, :],
                                                                    start=(k == 0), stop=(k == KC - 1)),
                                  reads=[bfg, dn.hT], writes=[pg], pe_acc=(k > 0))
                        for k in range(KC):
                            kb.op('pe', lambda e, k=k, m=m: e.matmul(pu[:], lhsT=wuv[:, k, m * 128:(m + 1) * 128], rhs=dn.hT[:, k, :],
                                                                    start=(k == 0), stop=(k == KC - 1)),
                                  reads=[bfu, dn.hT], writes=[pu], pe_acc=(k > 0))
                        s_ = sg[m]
                        kb.op('act', lambda e: e.activation(out=s_[:], in_=pg[:], func=AF.Silu), reads=[pg], writes=[s_])
                        r = g * 2 + m
                        kb.op('dve', lambda e, r=r: e.tensor_tensor(out=aT[:, r, :], in0=pu[:], in1=s_[:], op=ALU.mult),
                              reads=[pu, s_], writes=[aT])
                dn.gemm_down(wd_d, aT, c_out, c_d, t == 0)
                if final:
                    dn.rmsnorm(vb, 32, 1e-6, dst_f32=yT)
                    kb.dma(xo_d.rearrange("(k p) n -> p k n", p=128)[:, :, ts], yT[:], reads=[yT], is_out=True, q='sp')
                else:
                    kb.dma(xo_d.rearrange("(k p) n -> p k n", p=128)[:, :, ts], xT[:], reads=[xT], is_out=True, q='sp')
            if has_a:
                dn.rmsnorm(vb, 0, 1e-6)
                def c_in(r, ps):
                    o = dn.ost[dn.oi]; dn.oi ^= 1
                    if r % 2 == 0:
                        kb.op('act', lambda e: e.copy(out=o[:], in_=ps[:]), reads=[ps], writes=[o])
                    else:
                        kb.op('dve', lambda e: e.tensor_copy(out=o[:], in_=ps[:]), reads=[ps], writes=[o])
                    kb.dma(pj_d[r * 128:(r + 1) * 128, ts], o[:], reads=[o], is_out=True)
                dn.gemm(win_d, NG_IN, KC, GW, dn.hT, c_in, c_in_s, t == 0)
        kb.finish()
    return nc


def arrange_w(W, gw, ng=None):
    K, M = W.shape
    if ng is None:
        ng = (M + gw - 1) // gw
    if ng * gw != M:
        Wp = np.zeros((K, ng * gw), np.float32); Wp[:, :M] = W; W = Wp
    return np.ascontiguousarray(W.reshape(K // 128, 128, ng, gw).transpose(2, 1, 0, 3)).reshape(ng, 128, (K // 128) * gw)


def vec_pk(v):
    return np.ascontiguousarray(v.reshape(-1, 128).T)


class V:
    def __init__(self, b, ap):
        self.b = b; self.ap = ap

    def __getitem__(self, idx):
        return V(self.b, self.ap[idx])

    def rr(self, pat, **kw):
        return V(self.b, self.ap.rearrange(pat, **kw))


class TB(Buf):
    def __getitem__(self, idx):
        return V(self, self.ap[idx])


class MX:
    def __init__(self, kb):
        self.kb = kb
        self.bank = [TB(kb.es.enter_context(kb.nc.psum_tensor("pp_b%d" % i, [128, 512], F32)), "bank%d" % i) for i in range(8)]
        self.bi = 0
        for b in self.bank:
            b.psum = True

    def sb(self, name, shape, dtype=F32):
        t = self.kb.es.enter_context(self.kb.nc.sbuf_tensor("sb_" + name, list(shape), dtype))
        return TB(t, name)

    def pz(self):
        b = self.bank[self.bi]; self.bi = (self.bi + 1) % 6
        return b

    @staticmethod
    def _rw(vs):
        return [v.b for v in vs if isinstance(v, V)]

    @staticmethod
    def _a(x):
        return x.ap if isinstance(x, V) else x

    def tt(self, e, out, a, b, op):
        self.kb.op(e, lambda en: en.tensor_tensor(out=out.ap, in0=a.ap, in1=b.ap, op=op), reads=self._rw([a, b]), writes=[out.b])

    def ts(self, e, out, a, s1, op0, s2=None, op1=None):
        if op1 is None:
            self.kb.op(e, lambda en: en.tensor_scalar(out=out.ap, in0=a.ap, scalar1=self._a(s1), scalar2=None, op0=op0),
                       reads=self._rw([a, s1]), writes=[out.b])
        else:
            self.kb.op(e, lambda en: en.tensor_scalar(out=out.ap, in0=a.ap, scalar1=self._a(s1), scalar2=self._a(s2), op0=op0, op1=op1),
                       reads=self._rw([a, s1, s2]), writes=[out.b])

    def stt(self, out, a, s, b, op0, op1):
        self.kb.op('dve', lambda en: en.scalar_tensor_tensor(out=out.ap, in0=a.ap, scalar=self._a(s), in1=b.ap, op0=op0, op1=op1),
                   reads=self._rw([a, s, b]), writes=[out.b])

    def act(self, out, a, func, bias=None, scale=1.0):
        if bias is None:
            self.kb.op('act', lambda en: en.activation(out=out.ap, in_=a.ap, func=func, scale=self._a(scale)),
                       reads=self._rw([a, scale]), writes=[out.b])
        else:
            self.kb.op('act', lambda en: en.activation(out=out.ap, in_=a.ap, func=func, bias=self._a(bias), scale=self._a(scale)),
                       reads=self._rw([a, bias, scale]), writes=[out.b])

    def cp(self, e, out, a):
        if e == 'act':
            self.kb.op(e, lambda en: en.copy(out=out.ap, in_=a.ap), reads=[a.b], writes=[out.b])
        else:
            self.kb.op(e, lambda en: en.tensor_copy(out=out.ap, in_=a.ap), reads=[a.b], writes=[out.b])

    def mm(self, out, lhsT, rhs, start=True, stop=True):
        self.kb.op('pe', lambda en: en.matmul(out.ap, lhsT=lhsT.ap, rhs=rhs.ap, start=start, stop=stop),
                   reads=[lhsT.b, rhs.b], writes=[out.b], pe_acc=(not start))

    def scan(self, out, d0, d1, init, op0, op1):
        self.kb.op('dve', lambda en: en.tensor_tensor_scan(out=out.ap, data0=d0.ap, data1=d1.ap, initial=init, op0=op0, op1=op1),
                   reads=[d0.b, d1.b], writes=[out.b])

    def memset(self, e, out, val):
        self.kb.op(e, lambda en: en.memset(out.ap, val), writes=[out.b])

    def dma_in(self, out, src_ap, q=None):
        self.kb.dma(out.ap, src_ap, writes=[out.b], q=q)

    def dma_out(self, dst_ap, src, q=None):
        self.kb.dma(dst_ap, src.ap, reads=[src.b], is_out=True, q=q)


PP_CONVW = 0
PP_CONVB = 16
PP_DSKIP = 20
PP_DTB = 22
PP_ALOG = 23
PP_MU = 24
PP_W0 = 32; PP_A0 = 33; PP_V0 = 34; PP_KK = 35; PP_KA = 36; PP_RK = 37; PP_LNW = 38; PP_LNB = 39
PP_GC = 40
PP_ZETA = 41
CF_MASKD = 0; CF_XI = 1; CF_NEGM = 2; CF_NML = 3; CF_NMU = 4; CF_MU = 5; CF_MUI = 6; CF_NMUI = 7
NCF = 8
CM_ONES128 = 0; CM_BONES = 1; CM_BONES64 = 2; CM_SEL = 3; CM_SEL2 = 7; CM_I4 = 9; CM_IDENT = 10
NCM = 11
CB_IDENT = 0; CB_SWAP = 1
NCB = 2
EXPM05 = float(np.exp(-0.5))


def build_mixer(S, has_vres):
    nc = bass.Bass("TRN2", target_bir_lowering=False)
    dr = lambda n, s, kind="ExternalInput", dt=F32: nc.dram_tensor(n, list(s), dt, kind=kind).ap()
    ret_in = dr("ret_in", [4, 128, S]); rope = dr("rope", [2, 128, S])
    ssd_zx = dr("ssd_z", [2, 128, S]); ssd_c = dr("ssd_c", [4, 128, 3 + S]); ssd_dt = dr("ssd_dt", [4, S])
    rw_in = dr("rw_in", [8, 128, 1 + S])
    if has_vres:
        vfirst = dr("vfirst", [128, S])
    pp_d = dr("pp", [128, 64]); cf_d = dr("cf", [128, NCF, 512]); cm_d = dr("cm", [128, NCM, 128])
    cb_d = dr("cb", [128, NCB, 128], dt=BF16); rww_d = dr("rww", [128, 5, 128])
    y_ret = dr("y_ret", [128, S], "ExternalOutput"); y_ssd = dr("y_ssd", [2, 128, S], "ExternalOutput")
    y_rw = dr("y_rw", [128, S], "ExternalOutput")
    if not has_vres:
        v_out = dr("v_out", [128, S], "ExternalOutput")
    NTL = S // TT
    with ExitStack() as es:
        kb = KB(nc, es); m = MX(kb)
        T = TT
        pp = m.sb("pp", [128, 64]); cf = m.sb("cf", [128, NCF, 512]); cm = m.sb("cm", [128, NCM, 128])
        cb = m.sb("cb", [128, NCB, 128], BF16); rww = m.sb("rww", [128, 5, 128]); rwb = m.sb("rwb", [128, 5, 128], BF16)
        m.dma_in(pp[:], pp_d); m.dma_in(cf[:], cf_d); m.dma_in(cm[:], cm_d); m.dma_in(cb[:], cb_d); m.dma_in(rww[:], rww_d)
        m.cp('dve', rwb[:], rww[:])
        ident = cb[:, CB_IDENT, :]; swp = cb[:, CB_SWAP, :]
        col = lambda c, n=128: pp[0:n, c:c + 1]
        epsb = m.sb("epsb", [128, 4])
        for i, v_ in enumerate((1e-6, 64e-5, 1e-24)):
            m.memset('dve', epsb[:, i:i + 1], v_)
        ones_f = m.sb("ones_f", [128, 128])
        m.memset('dve', ones_f[:], 1.0)
        omu = m.sb("omu", [128, 8])
        m.ts('dve', omu[:], pp[:, PP_MU:PP_MU + 8], -1.0, ALU.mult, 1.0, ALU.add)
        negA = m.sb("negA", [4, 1])
        m.act(negA[:], pp[0:4, PP_ALOG:PP_ALOG + 1], AF.Exp)
        m.ts('dve', negA[:], negA[:], -1.0, ALU.mult)

        big1 = m.sb("big1", [128, 4112]); big2 = m.sb("big2", [128, 8 * T])
        vw = lambda big, lo, hi, f, nm: TB(big.ap[:, lo:hi].rearrange("p (f s) -> p f s", f=f), nm, parent=big)
        r_in = vw(big2, 0, 4 * T, 4, "r_in"); r_cs = vw(big2, 4 * T, 6 * T, 2, "r_cs")
        qb = m.sb("qb", [128, T], BF16); kbb = m.sb("kbb", [128, T], BF16); vbb = m.sb("vbb", [128, T], BF16)
        rt1 = m.sb("rt1", [128, T]); rt2 = m.sb("rt2", [128, T])
        qr = m.sb("qr", [128, T], BF16); kr = m.sb("kr", [128, T], BF16); qx = m.sb("qx", [128, T], BF16)
        vtok = m.sb("vtok", [128, T], BF16); kztok = m.sb("kztok", [128, T], BF16); pT = m.sb("pT", [128, T], BF16)
        rS = m.sb("rS", [128, 128]); rSb = [m.sb("rSb%d" % i, [128, 128], BF16) for i in range(2)]
        ry = m.sb("ry", [128, T]); rysq = m.sb("rysq", [128, T]); rmean = m.sb("rmean", [128, T]); rvar = m.sb("rvar", [128, T])
        rsg = m.sb("rsg", [128, T])
        m.memset('dve', rS[:], 0.0); m.memset('pool', rSb[0][:], 0.0)
        rsi = [0]

        def retention(t):
            ts_ = slice(t * T, (t + 1) * T)
            m.dma_in(r_in[:], ret_in.rearrange("f p s -> p f s")[:, :, ts_])
            m.dma_in(r_cs[:], rope.rearrange("f p s -> p f s")[:, :, ts_])
            q, k, v, g = (r_in[:, i, :] for i in range(4))
            cos, sin = r_cs[:, 0, :], r_cs[:, 1, :]
            m.cp('pool', qb[:], q); m.cp('pool', kbb[:], k); m.cp('act', vbb[:], v)
            for src, srcb, dst in ((q, qb, qr), (k, kbb, kr)):
                p = m.pz()
                m.mm(p[:], swp, srcb[:])
                m.tt('pool', rt1[:], src, cos, ALU.mult)
                m.tt('dve', rt2[:], p[:], sin, ALU.mult)
                m.tt('dve', dst[:], rt1[:], rt2[:], ALU.add)
            m.tt('pool', qx[:], qr[:], cf[:, CF_XI, :], ALU.mult)
            pv_, pk_, psc = m.pz(), m.pz(), m.pz()
            for c in range(4):
                cs = slice(c * 128, (c + 1) * 128)
                m.mm(pv_[:, cs], vbb[:, cs], ident)
                m.mm(pk_[:, cs], kr[:, cs], ident)
                m.mm(psc[:, cs], kr[:, cs], qr[:, cs])
            m.cp('act', vtok[:], pv_[:])
            m.ts('dve', kztok[:], pk_[:], col(PP_ZETA), ALU.mult)
            m.tt('dve', pT[:], psc[:], cf[:, CF_MASKD, :], ALU.mult)
            po, pst = m.pz(), m.pz()
            for c in range(4):
                cs = slice(c * 128, (c + 1) * 128)
                m.mm(pst[:, cs], kztok[:, cs], vtok[:, cs])
            for c in range(4):
                cs = slice(c * 128, (c + 1) * 128)
                sb_ = rSb[rsi[0]]
                m.mm(po[:, cs], vtok[:, cs], pT[:, cs], start=True, stop=False)
                m.mm(po[:, cs], sb_[:], qx[:, cs], start=False, stop=True)
                m.stt(rS[:], rS[:], col(PP_GC), pst[:, cs], ALU.mult, ALU.add)
                rsi[0] ^= 1
                m.cp('act', rSb[rsi[0]][:], rS[:])
            m.cp('act', ry[:], po[:])
            m.act(rysq[:], po[:], AF.Square)
            pm, pq = m.pz(), m.pz()
            m.mm(pm[:], cm[:, CM_ONES128, :], ry[:])
            m.mm(pq[:], cm[:, CM_ONES128, :], rysq[:])
            m.cp('act', rmean[:], pm[:])
            m.tt('dve', rvar[:], rmean[:], rmean[:], ALU.mult)
            m.tt('dve', rvar[:], pq[:], rvar[:], ALU.subtract)
            m.act(rvar[:], rvar[:], AF.Sqrt, bias=epsb[:, 0:1])
            m.kb.op('dve', lambda en: en.reciprocal(out=rvar.ap[:], in_=rvar.ap[:]), reads=[rvar], writes=[rvar])
            m.tt('dve', ry[:], ry[:], rmean[:], ALU.subtract)
            m.tt('dve', ry[:], ry[:], rvar[:], ALU.mult)
            m.act(rsg[:], g, AF.Silu)
            m.tt('dve', ry[:], ry[:], rsg[:], ALU.mult)
            m.dma_out(y_ret[:, ts_], ry[:])

        s_z = vw(big2, 6 * T, 8 * T, 2, "s_z"); s_c = vw(big1, 0, 4 * (3 + T), 4, "s_c"); s_dt = m.sb("s_dt", [4, T])
        s_cv = vw(big1, 4 * (3 + T), 4 * (3 + T) + 4 * T, 4, "s_cv"); s_Bb = m.sb("s_Bb", [128, T], BF16); s_Cb = m.sb("s_Cb", [128, T], BF16)
        s_a = m.sb("s_a", [4, T]); s_ac = m.sb("s_ac", [4, T])
        s_ab = m.sb("s_ab", [128, 2, T]); s_E = m.sb("s_E", [128, 2, T]); s_dte = m.sb("s_dte", [128, 2, T])
        s_xdt = m.sb("s_xdt", [128, 2, T]); s_xdtb = m.sb("s_xdtb", [128, 2, T], BF16); s_xdteb = m.sb("s_xdteb", [128, 2, T], BF16)
        s_xtokP = m.sb("s_xtokP", [128, 4, 4, 128], BF16)
        s_xetok = m.sb("s_xetok", [128, 4, 256], BF16)
        s_Btok = m.sb("s_Btok", [128, T], BF16)
        s_negcol = m.sb("s_negcol", [128, 16]); s_cdec = m.sb("s_cdec", [128, 4, 4])
        s_cbT = m.sb("s_cbT", [128, T]); s_seg = rt2; s_G = [m.sb("s_G%d" % i, [128, T], BF16) for i in range(2)]
        s_S = m.sb("s_S", [128, 256]); s_Sb = [m.sb("s_Sb%d" % i, [128, 256], BF16) for i in range(2)]
        s_t1 = ry; s_sz = rt1; s_y = m.sb("s_y", [128, 2, T])
        m.memset('dve', s_S[:], 0.0); m.memset('pool', s_Sb[0][:], 0.0); m.memset('pool', s_xtokP[:], 0.0)
        ssi = [0]

        def ssd(t):
            ts_ = slice(t * T, (t + 1) * T)
            m.dma_in(s_z[:], ssd_zx.rearrange("f p s -> p f s")[:, :, ts_])
            m.dma_in(s_c[:], ssd_c.rearrange("f p s -> p f s")[:, :, t * T:t * T + T + 3])
            m.dma_in(s_dt[:], ssd_dt[:, ts_])
            for b in range(4):
                o = s_cv[:, b, :]
                m.act(o, s_c[:, b, 0:T], AF.Identity, bias=col(PP_CONVB + b), scale=col(PP_CONVW + b * 4))
                for k in range(1, 4):
                    m.stt(o, s_c[:, b, k:k + T], col(PP_CONVW + b * 4 + k), o, ALU.mult, ALU.add)
                m.act(o, o, AF.Silu)
            m.cp('pool', s_Bb[:], s_cv[:, 2, :]); m.cp('pool', s_Cb[:], s_cv[:, 3, :])
            m.act(s_dt[:], s_dt[:], AF.Exp, bias=pp[0:4, PP_DTB:PP_DTB + 1])
            m.act(s_dt[:], s_dt[:], AF.Ln, bias=1.0)
            m.ts('dve', s_a[:], s_dt[:], negA[:, 0:1], ALU.mult)
            for c in range(4):
                cs = slice(c * 128, (c + 1) * 128)
                m.scan(s_ac[:, cs], ones_f[0:4, :], s_a[:, cs], 0.0, ALU.mult, ALU.add)
            for b in range(2):
                p1, p2 = m.pz(), m.pz()
                m.mm(p1[:], cm[0:4, CM_SEL2 + b, :], s_ac[:])
                m.mm(p2[:], cm[0:4, CM_SEL2 + b, :], s_dt[:])
                m.cp('act', s_ab[:, b, :], p1[:])
                m.act(s_E[:, b, :], p1[:], AF.Exp)
                m.tt('dve', s_xdt[:, b, :], s_cv[:, b, :], p2[:], ALU.mult)
                for c in range(4):
                    cs = slice(c * 128, (c + 1) * 128)
                    m.act(s_dte[:, b, cs], s_ab[:, b, cs], AF.Exp, bias=s_ab[:, b, c * 128 + 127:c * 128 + 128], scale=-1.0)
                m.cp('pool', s_xdtb[:, b, :], s_xdt[:, b, :])
                m.tt('pool', s_xdteb[:, b, :], s_xdt[:, b, :], s_dte[:, b, :], ALU.mult)
            for b in range(2):
                p1, p2 = m.pz(), m.pz()
                for c in range(4):
                    cs = slice(c * 128, (c + 1) * 128)
                    m.mm(p1[:, cs], s_xdtb[:, b, cs], ident)
                    m.mm(p2[:, cs], s_xdteb[:, b, cs], ident)
                for hl in range(2):
                    h = b * 2 + hl
                    m.cp('act', s_xtokP[:, :, h, hl * 64:(hl + 1) * 64], p1[:].rr("p (c x) -> p c x", c=4)[:, :, hl * 64:(hl + 1) * 64])
                m.cp('dve', s_xetok[:, :, b * 128:(b + 1) * 128], p2[:].rr("p (c x) -> p c x", c=4))
            pB, pcb, pcol = m.pz(), m.pz(), m.pz()
            for c in range(4):
                cs = slice(c * 128, (c + 1) * 128)
                m.mm(pB[:, cs], s_Bb[:, cs], ident)
                m.mm(pcb[:, cs], s_Bb[:, cs], s_Cb[:, cs])
                m.mm(pcol[:, c * 4:(c + 1) * 4], s_ac[:, cs], cm[0:4, CM_I4, 0:4])
            m.cp('act', s_Btok[:], pB[:])
            m.cp('act', s_cbT[:], pcb[:])
            m.ts('dve', s_negcol[:], pcol[:, 0:16], -1.0, ALU.mult)
            py = [m.bank[6], m.bank[7]]
            for h in range(4):
                b, hl = h // 2, h % 2
                pab = m.pz()
                m.mm(pab[:], cm[0:4, CM_SEL + h, :], s_ac[:])
                m.tt('dve', s_seg[:], pab[:], cf[:, CF_NEGM, :], ALU.add)
                m.act(s_cdec[:, h, :], pab[:].rr("p (c x) -> p c x", c=4)[:, :, 127], AF.Exp)
                for c in range(4):
                    cs = slice(c * 128, (c + 1) * 128)
                    m.act(s_seg[:, cs], s_seg[:, cs], AF.Exp, bias=s_negcol[:, c * 4 + h:c * 4 + h + 1])
                G = s_G[h % 2]
                m.tt('dve', G[:], s_cbT[:], s_seg[:], ALU.mult)
                if hl == 1:
                    for c in range(4):
                        cs = slice(c * 128, (c + 1) * 128)
                        m.mm(py[b][:, cs], s_xtokP[:, c, h - 1, :], s_G[0][:, cs], start=True, stop=False)
                        m.mm(py[b][:, cs], s_xtokP[:, c, h, :], s_G[1][:, cs], start=False, stop=True)
            pS = [m.pz(), m.pz()]
            for c in range(4):
                m.mm(pS[c // 2][:, (c % 2) * 256:(c % 2 + 1) * 256], s_Btok[:, c * 128:(c + 1) * 128], s_xetok[:, c, :])
            poff = [m.pz(), m.pz()]
            for c in range(4):
                cs = slice(c * 128, (c + 1) * 128)
                sb_ = s_Sb[ssi[0]]
                for b in range(2):
                    m.mm(poff[b][:, cs], sb_[:, b * 128:(b + 1) * 128], s_Cb[:, cs])
                for h in range(4):
                    hs = slice(h * 64, (h + 1) * 64)
                    m.stt(s_S[:, hs], s_S[:, hs], s_cdec[:, h, c:c + 1], pS[c // 2][:, (c % 2) * 256 + h * 64:(c % 2) * 256 + (h + 1) * 64],
                          ALU.mult, ALU.add)
                ssi[0] ^= 1
                m.cp('act', s_Sb[ssi[0]][:], s_S[:])
            for b in range(2):
                m.tt('dve', s_t1[:], poff[b][:], s_E[:, b, :], ALU.mult)
                m.tt('dve', s_t1[:], s_t1[:], py[b][:], ALU.add)
                m.stt(s_t1[:], s_cv[:, b, :], col(PP_DSKIP + b), s_t1[:], ALU.mult, ALU.add)
                m.act(s_sz[:], s_z[:, b, :], AF.Silu)
                m.tt('dve', s_y[:, b, :], s_t1[:], s_sz[:], ALU.mult)
            m.dma_out(y_ssd.rearrange("f p s -> p f s")[:, :, ts_], s_y[:])

        w_in = vw(big1, 0, 8 * (1 + T), 8, "w_in"); w_mx = vw(big2, 0, 8 * T, 8, "w_mx"); w_tmp = rt1
        w_thb = m.sb("w_thb", [128, T], BF16); w_xab = m.sb("w_xab", [128, T], BF16); w_sgb = m.sb("w_sgb", [128, 2, T], BF16)
        w_pvb = m.sb("w_pvb", [128, T], BF16)
        w_lw = m.sb("w_lw", [128, T]); w_a = m.sb("w_a", [128, T]); w_g = m.sb("w_g", [128, T]); w_v = m.sb("w_v", [128, T])
        w_kk = m.sb("w_kk", [128, T]); w_km = m.sb("w_km", [128, T]); w_b = m.sb("w_b", [128, T]); w_bon = m.sb("w_bon", [128, T])
        w_L = rt2; w_W = m.sb("w_W", [128, T]); w_Wi = m.sb("w_Wi", [128, T]); w_Wp = rsg
        w_vf = m.sb("w_vf", [128, T])
        NCH = T // 64
        BDn = ("RT", "KT", "BT", "KA", "VF")
        BDf = {n: m.sb("w_bd" + n, [128, NCH, 128], BF16) for n in BDn}
        for n in BDn:
            m.memset('pool', BDf[n][:], 0.0)
        w_Vt = m.sb("w_Vt", [128, 4, 128], BF16); w_KTt = m.sb("w_KTt", [128, 4, 128], BF16); w_nBTt = m.sb("w_nBTt", [128, 4, 128], BF16)
        w_N = [m.sb("w_N%d" % i, [128, 4, 128], BF16) for i in range(2)]
        w_NT = [m.sb("w_NT%d" % i, [128, 4, 128], BF16) for i in range(2)]
        w_AkkT = m.sb("w_AkkT", [128, 4, 128], BF16); w_ArkT = m.sb("w_ArkT", [128, 4, 128], BF16); w_nArbT = m.sb("w_nArbT", [128, 4, 128], BF16)
        w_Z = m.sb("w_Z", [128, 4, 256]); w_Zb = m.sb("w_Zb", [128, 4, 256], BF16)
        w_RpT = m.sb("w_RpT", [128, 4, 128], BF16); w_Y0T = m.sb("w_Y0T", [128, 4, 128]); w_Gp = m.sb("w_Gp", [128, 4, 128], BF16)
        w_DdW = m.sb("w_DdW", [128, 4, 128]); w_st1 = m.sb("w_st1", [128, 128])
        w_ST = m.sb("w_ST", [128, 128]); w_STb = [m.sb("w_STb%d" % i, [128, 128], BF16) for i in range(2)]
        w_y = m.sb("w_y", [128, T]); w_ysq = rysq; w_mean = rmean; w_var = rvar
        m.memset('dve', w_ST[:], 0.0); m.memset('pool', w_STb[0][:], 0.0)
        wsi = [0]

        def rwkv(t):
            ts_ = slice(t * T, (t + 1) * T)
            m.dma_in(w_in[:], rw_in.rearrange("f p s -> p f s")[:, :, t * T:t * T + T + 1])
            if has_vres:
                m.dma_in(w_vf[:], vfirst[:, ts_])
            for b in range(8):
                m.ts('pool', w_tmp[:], w_in[:, b, 0:T], pp[:, PP_MU + b:PP_MU + b + 1], ALU.mult)
                m.stt(w_mx[:, b, :], w_in[:, b, 1:T + 1], omu[:, b:b + 1], w_tmp[:], ALU.mult, ALU.add)
            r_, k_, vm = w_mx[:, 0, :], w_mx[:, 1, :], w_mx[:, 2, :]
            m.act(w_thb[:], w_mx[:, 3, :], AF.Tanh)
            m.cp('pool', w_xab[:], w_mx[:, 4, :])
            m.act(w_sgb[:], w_mx[:, 5:7, :], AF.Sigmoid)
            p = m.pz(); m.mm(p[:], rwb[:, 0, :], w_thb[:])
            m.act(w_lw[:], p[:], AF.Sigmoid, bias=col(PP_W0))
            m.ts('dve', w_lw[:], w_lw[:], -EXPM05, ALU.mult)
            p = m.pz(); m.mm(p[:], rwb[:, 1, :], w_xab[:])
            m.act(w_a[:], p[:], AF.Sigmoid, bias=col(PP_A0))
            p = m.pz()
            m.mm(p[:], rwb[:, 2, :], w_sgb[:, 0, :], start=True, stop=False)
            m.mm(p[:], rwb[:, 3, :], w_sgb[:, 1, :], start=False, stop=True)
            m.cp('act', w_g[:], p[:])
            if has_vres:
                m.cp('pool', w_pvb[:], w_mx[:, 7, :])
                p = m.pz(); m.mm(p[:], rwb[:, 4, :], w_pvb[:])
                m.act(w_tmp[:], p[:], AF.Sigmoid, bias=col(PP_V0))
                m.tt('dve', w_v[:], w_vf[:], vm, ALU.subtract)
                m.tt('dve', w_v[:], w_v[:], w_tmp[:], ALU.mult)
                m.tt('dve', w_v[:], w_v[:], vm, ALU.add)
            else:
                m.cp('pool', w_v[:], vm)
                m.dma_out(v_out[:, ts_], w_v[:])
            m.ts('dve', w_kk[:], k_, col(PP_KK), ALU.mult)
            m.tt('pool', w_tmp[:], w_kk[:], w_kk[:], ALU.mult)
            p = m.pz(); m.mm(p[:], cm[:, CM_BONES, :], w_tmp[:])
            m.act(w_tmp[:], p[:], AF.Sqrt)
            m.ts('dve', w_tmp[:], w_tmp[:], 1e-12, ALU.max)
            m.kb.op('dve', lambda en: en.reciprocal(out=w_tmp.ap[:], in_=w_tmp.ap[:]), reads=[w_tmp], writes=[w_tmp])
            m.tt('dve', w_kk[:], w_kk[:], w_tmp[:], ALU.mult)
            m.ts('dve', w_km[:], w_a[:], -1.0, ALU.add, col(PP_KA), ALU.mult)
            m.stt(w_km[:], w_km[:], 1.0, k_, ALU.add, ALU.mult)
            m.tt('pool', w_b[:], w_kk[:], w_a[:], ALU.mult)
            m.stt(w_tmp[:], r_, col(PP_RK), w_km[:], ALU.mult, ALU.mult)
            p = m.pz(); m.mm(p[:], cm[:, CM_BONES, :], w_tmp[:])
            m.tt('dve', w_bon[:], p[:], w_v[:], ALU.mult)
            for c in range(NCH):
                cs = slice(c * 64, (c + 1) * 64)
                m.scan(w_L[:, cs], ones_f[:, 0:64], w_lw[:, cs], 0.0, ALU.mult, ALU.add)
            m.act(w_W[:], w_L[:], AF.Exp)
            m.act(w_Wi[:], w_L[:], AF.Exp, scale=-1.0)
            m.tt('pool', w_Wp[:], w_L[:], w_lw[:], ALU.subtract)
            m.act(w_Wp[:], w_Wp[:], AF.Exp)
            for name, x0, x1 in (("RT", r_, w_W[:]), ("KT", w_km[:], w_Wi[:]), ("BT", w_b[:], w_Wi[:]), ("KA", w_kk[:], w_Wp[:])):
                for hh in range(2):
                    ps_ = slice(hh * 64, (hh + 1) * 64)
                    m.tt('dve', BDf[name][ps_, :, hh * 64:(hh + 1) * 64], x0[ps_, :].rr("p (c x) -> p c x", x=64),
                         x1[ps_, :].rr("p (c x) -> p c x", x=64), ALU.mult)
            for hh in range(2):
                ps_ = slice(hh * 64, (hh + 1) * 64)
                m.cp('pool', BDf["VF"][ps_, :, hh * 64:(hh + 1) * 64], w_v[ps_, :].rr("p (c x) -> p c x", x=64))
            RT, KT, BT, KA, VF = (BDf[n] for n in BDn)
            stage = float(getattr(build_mixer, "stage", 9))
            if stage <= 1:
                m.dma_out(y_rw[:, ts_], w_bon[:])
                return
            if stage <= 1.65:
                m.cp('dve', w_y[:], w_bon[:])
            mk = lambda i: cf[:, i, :].rr("p (c x) -> p c x", c=4)
            p4 = lambda pb: pb[:].rr("p (c x) -> p c x", c=4)
            pY = m.bank[6]
            for qd in range(NCH // 4 if stage > 1.25 else 0):
                c0 = qd * 4
                pa, pb_, pc, pd = m.pz(), m.pz(), m.pz(), m.pz()
                for c in range(4):
                    cs = slice(c * 128, (c + 1) * 128)
                    m.mm(pa[:, cs], VF[:, c0 + c, :], ident)
                    m.mm(pb_[:, cs], KA[:, c0 + c, :], ident)
                    m.mm(pc[:, cs], KT[:, c0 + c, :], ident)
                    m.mm(pd[:, cs], BT[:, c0 + c, :], ident)
                m.cp('act', w_Vt[:], p4(pa))
                m.cp('act', w_Z[:, :, 128:256], p4(pb_))
                m.cp('dve', w_Zb[:, :, 128:256], p4(pb_))
                m.cp('act', w_KTt[:], p4(pc))
                m.ts('dve', w_nBTt[:], p4(pd), -1.0, ALU.mult)
                if stage <= 1.5:
                    continue
                if stage <= 1.55:
                    m.dma_out(y_rw[:, ts_], w_bon[:])
                    return
                pa, pb_, pc, pd, pe_ = m.pz(), m.pz(), m.pz(), m.pz(), m.pz()
                for c in range(4):
                    cs = slice(c * 128, (c + 1) * 128)
                    m.mm(pa[:, cs], KA[:, c0 + c, :], BT[:, c0 + c, :])
                    m.mm(pb_[:, cs], BT[:, c0 + c, :], KA[:, c0 + c, :])
                    m.mm(pc[:, cs], KT[:, c0 + c, :], KA[:, c0 + c, :])
                    m.mm(pd[:, cs], KT[:, c0 + c, :], RT[:, c0 + c, :])
                    m.mm(pe_[:, cs], BT[:, c0 + c, :], RT[:, c0 + c, :])
                m.tt('dve', w_N[0][:], p4(pa), mk(CF_NML), ALU.mult)
                m.tt('dve', w_NT[0][:], p4(pb_), mk(CF_NMU), ALU.mult)
                m.tt('dve', w_AkkT[:], p4(pc), mk(CF_MU), ALU.mult)
                m.tt('dve', w_ArkT[:], p4(pd), mk(CF_MUI), ALU.mult)
                m.tt('dve', w_nArbT[:], p4(pe_), mk(CF_NMUI), ALU.mult)
                if stage <= 1.7:
                    continue
                pa = m.pz()
                for c in range(4):
                    m.mm(pa[:, c * 128:(c + 1) * 128], w_AkkT[:, c, :], w_Vt[:, c, :])
                m.cp('act', w_Z[:, :, 0:128], p4(pa))
                m.cp('dve', w_Zb[:, :, 0:128], p4(pa))
                if stage <= 2:
                    continue
                for j in range(6):
                    cur, nxt = j % 2, (j + 1) % 2
                    pz1, pz2 = m.pz(), m.pz()
                    for c in range(4):
                        pzz = (pz1, pz2)[c // 2]
                        m.mm(pzz[:, (c % 2) * 256:(c % 2 + 1) * 256], w_NT[cur][:, c, :], w_Zb[:, c, :])
                    for hf, pzz in enumerate((pz1, pz2)):
                        zs = w_Z[:, hf * 2:hf * 2 + 2, :]
                        m.tt('dve', zs, pzz[:].rr("p (c x) -> p c x", c=2), zs, ALU.add)
                        m.cp('act', w_Zb[:, hf * 2:hf * 2 + 2, :], zs)
                    if j < 5:
                        pn, pnt = m.pz(), m.pz()
                        for c in range(4):
                            cs = slice(c * 128, (c + 1) * 128)
                            m.mm(pn[:, cs], w_NT[cur][:, c, :], w_N[cur][:, c, :])
                            m.mm(pnt[:, cs], w_N[cur][:, c, :], w_NT[cur][:, c, :])
                        m.cp('act', w_N[nxt][:], p4(pn))
                        m.cp('dve', w_NT[nxt][:], p4(pnt))
                if stage <= 3:
                    continue
                U0 = lambda c: w_Zb[:, c, 0:128]
                Ktp = lambda c: w_Zb[:, c, 128:256]
                pa, pb_, pc, pd = m.pz(), m.pz(), m.pz(), m.pz()
                for c in range(4):
                    cs = slice(c * 128, (c + 1) * 128)
                    m.mm(pa[:, cs], Ktp(c), w_nArbT[:, c, :])
                    m.mm(pb_[:, cs], w_Vt[:, c, :], w_ArkT[:, c, :], start=True, stop=False)
                    m.mm(pb_[:, cs], U0(c), w_nArbT[:, c, :], start=False, stop=True)
                    m.mm(pc[:, cs], Ktp(c), w_nBTt[:, c, :])
                    m.mm(pd[:, cs], w_KTt[:, c, :], w_Vt[:, c, :], start=True, stop=False)
                    m.mm(pd[:, cs], w_nBTt[:, c, :], U0(c), start=False, stop=True)
                m.tt('dve', w_RpT[:], p4(pa), RT[:, c0:c0 + 4, :], ALU.add)
                m.cp('act', w_Y0T[:], p4(pb_))
                m.cp('act', w_Gp[:], p4(pc))
                for c in range(4):
                    wc = w_W[:, (c0 + c) * 64 + 63:(c0 + c) * 64 + 64]
                    m.ts('dve', w_DdW[:, c, :], pd[:, c * 128:(c + 1) * 128], wc, ALU.mult)
                if stage <= 4:
                    continue
                for c in range(4):
                    cs = slice(c * 128, (c + 1) * 128)
                    wc = w_W[:, (c0 + c) * 64 + 63:(c0 + c) * 64 + 64]
                    stb = w_STb[wsi[0]]
                    m.mm(pY[:, cs], stb[:], w_RpT[:, c, :])
                    pst = m.pz()
                    m.mm(pst[:, 0:128], w_Gp[:, c, :], stb[:])
                    m.stt(w_st1[:], pst[:, 0:128], wc, w_DdW[:, c, :], ALU.mult, ALU.add)
                    m.stt(w_ST[:], w_ST[:], wc, w_st1[:], ALU.mult, ALU.add)
                    wsi[0] ^= 1
                    m.cp('act', w_STb[wsi[0]][:], w_ST[:])
                for hh in range(2):
                    ps_ = slice(hh * 64, (hh + 1) * 64)
                    m.tt('dve', w_y[ps_, qd * 256:(qd + 1) * 256].rr("p (c x) -> p c x", x=64),
                         p4(pY)[ps_, :, hh * 64:(hh + 1) * 64], w_Y0T[ps_, :, hh * 64:(hh + 1) * 64], ALU.add)
            m.tt('pool', w_ysq[:], w_y[:], w_y[:], ALU.mult)
            pm, pq = m.pz(), m.pz()
            m.mm(pm[:], cm[:, CM_BONES64, :], w_y[:])
            m.mm(pq[:], cm[:, CM_BONES64, :], w_ysq[:])
            m.cp('act', w_mean[:], pm[:])
            m.tt('dve', w_var[:], w_mean[:], w_mean[:], ALU.mult)
            m.tt('dve', w_var[:], pq[:], w_var[:], ALU.subtract)
            m.act(w_var[:], w_var[:], AF.Sqrt, bias=epsb[:, 1:2])
            m.kb.op('dve', lambda en: en.reciprocal(out=w_var.ap[:], in_=w_var.ap[:]), reads=[w_var], writes=[w_var])
            m.tt('dve', w_y[:], w_y[:], w_mean[:], ALU.subtract)
            m.tt('dve', w_y[:], w_y[:], w_var[:], ALU.mult)
            m.ts('dve', w_y[:], w_y[:], col(PP_LNW), ALU.mult, col(PP_LNB), ALU.add)
            m.tt('dve', w_y[:], w_y[:], w_bon[:], ALU.add)
            m.tt('dve', w_y[:], w_y[:], w_g[:], ALU.mult)
            m.dma_out(y_rw[:, ts_], w_y[:])

        which = getattr(build_mixer, "which", "rsw")
        for t in range(NTL):
            if "r" in which:
                retention(t)
            if "s" in which:
                ssd(t)
            if "w" in which:
                rwkv(t)
        kb.finish()
        print("mixer instrs", kb.ninst, "sems", kb.nsem, flush=True)
    return nc


def arrange_wd(W):
    return np.ascontiguousarray(W.reshape(2, 22, 128, 16, 128).transpose(3, 0, 2, 1, 4)).reshape(16, 2, 128, 22 * 128)


import ml_dtypes


def mixer_consts(c):
    gamma = 1.0 - 2.0 ** (-5 - c)
    idx = np.arange(128)
    cf = np.zeros((128, NCF, 512), np.float64)
    i4 = np.tile(idx, 4)
    rel = i4[None, :] - idx[:, None]
    cf[:, CF_MASKD, :] = np.where(rel >= 0, gamma ** np.maximum(rel, 0), 0.0) * (128 ** -0.5)
    cf[:, CF_XI, :] = (gamma ** (i4 + 1.0))[None, :]
    cf[:, CF_NEGM, :] = np.where(rel >= 0, 0.0, -30000.0)
    same = (idx[:, None] // 64) == (idx[None, :] // 64)
    lo = same & ((idx[:, None] % 64) > (idx[None, :] % 64))
    up = same & ((idx[:, None] % 64) < (idx[None, :] % 64))
    upi = same & ((idx[:, None] % 64) <= (idx[None, :] % 64))
    cf[:, CF_NML, :] = np.tile(-lo.astype(np.float64), (1, 4))
    cf[:, CF_NMU, :] = np.tile(-up.astype(np.float64), (1, 4))
    cf[:, CF_MU, :] = np.tile(up.astype(np.float64), (1, 4))
    cf[:, CF_MUI, :] = np.tile(upi.astype(np.float64), (1, 4))
    cf[:, CF_NMUI, :] = np.tile(-upi.astype(np.float64), (1, 4))
    cm = np.zeros((128, NCM, 128), np.float64)
    cm[:, CM_ONES128, :] = 1.0 / 128
    cm[:, CM_BONES, :] = same
    cm[:, CM_BONES64, :] = same / 64.0
    for h in range(4):
        cm[h, CM_SEL + h, :] = 1.0
    for b in range(2):
        for mcol in range(128):
            cm[2 * b + mcol // 64, CM_SEL2 + b, mcol] = 1.0
    cm[0:4, CM_I4, 0:4] = np.eye(4)
    cm[:, CM_IDENT, :] = np.eye(128)
    cb = np.zeros((128, NCB, 128), np.float32)
    cb[:, CB_IDENT, :] = np.eye(128)
    cb[(idx + 64) % 128, CB_SWAP, idx] = 1.0
    gc = gamma ** 128
    zeta = gamma ** (127.0 - idx) * (128 ** -0.5)
    return cf.astype(np.float32), cm.astype(np.float32), cb.astype(ml_dtypes.bfloat16), np.float32(gc), zeta.astype(np.float32)


def rope_tables(S):
    half = 64
    inv_freq = (10000.0 ** (-np.arange(half, dtype=np.float32) / half)).astype(np.float32)
    ang = np.arange(S, dtype=np.float32)[:, None] * inv_freq[None, :]
    cos = np.cos(ang.astype(np.float64)); sin = np.sin(ang.astype(np.float64))
    t = np.zeros((2, 128, S), np.float32)
    t[0, :64] = cos.T; t[0, 64:] = cos.T
    t[1, :64] = -sin.T; t[1, 64:] = sin.T
    return t


def mixer_params(c, l, P, gc, zeta):
    pp = np.zeros((128, 64), np.float32); p = np.arange(128)
    g = c // 2
    chans = [c * 256 + p, c * 256 + 128 + p, 1024 + g * 128 + p, 1280 + g * 128 + p]
    for b, ch in enumerate(chans):
        for k in range(4):
            pp[:, PP_CONVW + b * 4 + k] = P['ssm_conv_w'][l, k, ch]
        pp[:, PP_CONVB + b] = P['ssm_conv_b'][l, ch]
    for b in range(2):
        pp[:, PP_DSKIP + b] = P['ssm_d'][l, 4 * c + 2 * b + p // 64]
    pp[0:4, PP_DTB] = P['ssm_dt_bias'][l, 4 * c:4 * c + 4]
    pp[0:4, PP_ALOG] = P['ssm_a_log'][l, 4 * c:4 * c + 4]
    mu = P['rwkv_mu'][l]
    for b in range(3):
        pp[:, PP_MU + b] = mu[b * 512 + c * 128 + p]
    pp[0:96, PP_MU + 3] = mu[1536:1632]; pp[0:96, PP_MU + 4] = mu[1632:1728]
    pp[:, PP_MU + 5] = mu[1728:1856]; pp[:, PP_MU + 6] = mu[1856:1984]
    sl = slice(c * 128, (c + 1) * 128)
    if l > 0:
        pp[0:64, PP_MU + 7] = P['rwkv_mu_v'][l - 1]
        pp[:, PP_V0] = P['rwkv_v0'][l - 1, sl]
    pp[:, PP_W0] = P['rwkv_w0'][l, sl]; pp[:, PP_A0] = P['rwkv_a0'][l, sl]
    pp[:, PP_KK] = P['rwkv_k_k'][l, sl]; pp[:, PP_KA] = P['rwkv_k_a'][l, sl]
    pp[:, PP_RK] = P['rwkv_r_k'][l].reshape(512)[sl]
    pp[:, PP_LNW] = P['rwkv_ln_w'][l, sl]; pp[:, PP_LNB] = P['rwkv_ln_b'][l, sl]
    pp[:, PP_GC] = gc; pp[:, PP_ZETA] = zeta
    rww = np.zeros((128, 5, 128), np.float32)
    rww[0:96, 0] = P['rwkv_w2'][l][:, sl]; rww[0:96, 1] = P['rwkv_a2'][l][:, sl]
    rww[:, 2] = P['rwkv_g2'][l][0:128, sl]; rww[:, 3] = P['rwkv_g2'][l][128:256, sl]
    if l > 0:
        rww[0:64, 4] = P['rwkv_v2'][l - 1][:, sl]
    return pp, rww


def mixer_inputs(c, PJ):
    S = PJ.shape[1]; g = c // 2
    pad = lambda a, n: np.concatenate([np.zeros((a.shape[0], n), np.float32), a], axis=1)
    rows = lambda a: np.concatenate([a, np.zeros((128 - a.shape[0], a.shape[1]), np.float32)], axis=0) if a.shape[0] < 128 else a
    ret_in = np.stack([PJ[f * 512 + c * 128:f * 512 + (c + 1) * 128] for f in range(4)])
    ssd_z = np.stack([PJ[2048 + c * 256 + b * 128:2048 + c * 256 + (b + 1) * 128] for b in range(2)])
    ssd_c = np.stack([pad(PJ[3072 + c * 256:3072 + c * 256 + 128], 3), pad(PJ[3072 + c * 256 + 128:3072 + c * 256 + 256], 3),
                      pad(PJ[4096 + g * 128:4096 + (g + 1) * 128], 3), pad(PJ[4352 + g * 128:4352 + (g + 1) * 128], 3)])
    ssd_dt = np.ascontiguousarray(PJ[4608 + 4 * c:4608 + 4 * c + 4])
    R0 = 4624
    blocks = [PJ[R0 + c * 128:R0 + (c + 1) * 128], PJ[R0 + 512 + c * 128:R0 + 512 + (c + 1) * 128],
              PJ[R0 + 1024 + c * 128:R0 + 1024 + (c + 1) * 128], rows(PJ[6160:6256]), rows(PJ[6256:6352]),
              PJ[6352:6480], PJ[6480:6608], rows(PJ[6608:6672])]
    rw_in = np.stack([pad(b_, 1) for b_ in blocks])
    return {"ret_in": np.ascontiguousarray(ret_in), "ssd_z": np.ascontiguousarray(ssd_z), "ssd_c": np.ascontiguousarray(ssd_c),
            "ssd_dt": ssd_dt, "rw_in": np.ascontiguousarray(rw_in)}


_PROGS = {}


def _prog(key, fn):
    if key not in _PROGS:
        _PROGS[key] = fn()
    return _PROGS[key]


def kernel(**inputs):
    P = {k: np.asarray(v, dtype=np.float32) for k, v in inputs.items()}
    x = P['x']
    NCORE = 8
    cores = [(b, c) for b in range(BATCH) for c in range(4)]
    xT = [np.ascontiguousarray(x[b, c * NTOK:(c + 1) * NTOK, :].T) for b, c in cores]
    consts = [mixer_consts(c) for c in range(4)]
    rope = rope_tables(SEQ)
    vfirst = [None] * NCORE
    yT = [None] * NCORE
    out = np.zeros((BATCH, SEQ, D_MODEL), np.float32)
    for l in range(DEPTH + 1):
        has_c, has_a, final = l > 0, l < DEPTH, l == DEPTH
        nc = _prog(("dense", has_c, has_a, final), lambda: build_dense(has_c, has_a, final))
        vecs = np.zeros((128, 64), np.float32)
        common = {"vecs": vecs}
        if has_a:
            vecs[:, 0:16] = vec_pk(P['norm_mix_w'][l])
            common["win"] = arrange_w(P['w_in_first'] if l == 0 else P['w_in_rest'][l - 1], GW, NG_IN)
        if has_c:
            vecs[:, 16:32] = vec_pk(P['norm_ffn_w'][l - 1])
            vecs[:, 32:48] = vec_pk(P['final_norm_w'])
            vecs[:, 48:56] = vec_pk(P['ssm_norm_w'][l - 1])
            common["wout"] = arrange_w(P['w_out'][l - 1], GW)
            common["wg"] = arrange_w(P['ffn_w_gate'][l - 1], GW)
            common["wu"] = arrange_w(P['ffn_w_up'][l - 1], GW)
            common["wd"] = arrange_wd(P['ffn_w_down'][l - 1])
        in_maps = []
        for i in range(NCORE):
            d = dict(common); d["xT"] = xT[i]
            if has_c:
                d["yT"] = yT[i]
            in_maps.append(d)
        res = run_bass_kernel_spmd(nc, in_maps, core_ids=list(range(NCORE))).results
        if has_c:
            xT = [np.asarray(res[i]["xo"]) for i in range(NCORE)]
        if final:
            break
        ncm = _prog(("mixer", l > 0), lambda: build_mixer(SEQ, l > 0))
        in_maps = []
        for b in range(BATCH):
            PJ = np.concatenate([np.asarray(res[b * 4 + c]["pj"]) for c in range(4)], axis=1)
            for c in range(4):
                cf, cm, cb, gc, zeta = consts[c]
                pp, rww = mixer_params(c, l, P, gc, zeta)
                d = mixer_inputs(c, PJ)
                d.update({"rope": rope, "pp": pp, "cf": cf, "cm": cm, "cb": cb, "rww": rww})
                if l > 0:
                    d["vfirst"] = vfirst[b * 4 + c]
                in_maps.append(d)
            del PJ
        res2 = run_bass_kernel_spmd(ncm, in_maps, core_ids=list(range(NCORE))).results
        for b in range(BATCH):
            Y = np.zeros((D_MODEL, SEQ), np.float32)
            for c in range(4):
                r = res2[b * 4 + c]
                Y[c * 128:(c + 1) * 128] = np.asarray(r["y_ret"])
                Y[512 + c * 256:512 + (c + 1) * 256] = np.asarray(r["y_ssd"]).reshape(256, SEQ)
                Y[1536 + c * 128:1536 + (c + 1) * 128] = np.asarray(r["y_rw"])
                if l == 0:
                    vfirst[b * 4 + c] = np.ascontiguousarray(np.asarray(r["v_out"]))
            for c in range(4):
                yT[b * 4 + c] = np.ascontiguousarray(Y[:, c * NTOK:(c + 1) * NTOK])
    for i, (b, c) in enumerate(cores):
        out[b, c * NTOK:(c + 1) * NTOK, :] = xT[i].T
    return out
```

```python
from contextlib import ExitStack
import numpy as np
import concourse.bass as bass
import concourse.mybir as mybir
from concourse.bass_utils import run_bass_kernel_spmd

F32 = mybir.dt.float32
BF16 = mybir.dt.bfloat16
AF = mybir.ActivationFunctionType
ALU = mybir.AluOpType

D_MODEL = 2048; BATCH = 2; SEQ = 8192; DEPTH = 4
IN_COLS = 6608; MV = 64; D_FF = 5632
TT = 512
NTOK = 2048
KC = 16
FC = 44
GW = 256
NG_IN = 27
PROJ_ROWS = NG_IN * GW


class Buf:
    def __init__(self, ap, name, parent=None):
        self.ap = ap; self.name = name
        self.root = parent.root if parent is not None else self
        if parent is None:
            self._w = None
            self._r = {}

    w = property(lambda self: self.root._w, lambda self, v: setattr(self.root, '_w', v))
    r = property(lambda self: self.root._r, lambda self, v: setattr(self.root, '_r', v))

    def __getitem__(self, idx):
        return self.ap[idx]


class KB:
    ROT = 24000

    def __init__(self, nc, es):
        self.nc = nc; self.es = es
        self.eng = {'pe': nc.tensor, 'dve': nc.vector, 'act': nc.scalar, 'pool': nc.gpsimd, 'sp': nc.sync}
        self.sems = {}
        self.cur = {}
        self.dcnt = {}
        self.waited = {e: {} for e in self.eng}
        self.nsem = 0
        self.out_sems = []
        self.dq = 0
        self.ninst = 0
        self.rec = None

    def _newsem(self, key):
        h = self.es.enter_context(self.nc.semaphore("s%d" % self.nsem)); self.nsem += 1
        self.sems[key] = h
        return h

    def sb(self, name, shape, dtype):
        t = self.es.enter_context(self.nc.sbuf_tensor("sb_" + name, list(shape), dtype))
        return Buf(t, name)

    def ps(self, name, shape=(128, 512), dtype=F32):
        t = self.es.enter_context(self.nc.psum_tensor("pp_" + name, list(shape), dtype))
        b = Buf(t, name); b.psum = True
        return b

    def _eng_token(self, e):
        key, cnt = self.cur.get(e, (None, 0))
        if key is None or cnt >= self.ROT:
            key = (e, len([k for k in self.sems if k[0] == e]))
            self._newsem(key); cnt = 0
        cnt += 1
        self.cur[e] = (key, cnt)
        return key, cnt

    def _wait(self, e, deps):
        w = self.waited[e]
        for key, val in deps.items():
            if w.get(key, 0) >= val:
                continue
            self.eng[e].wait_ge(self.sems[key], val)
            w[key] = val

    def _deps(self, reads, writes, e):
        deps = {}
        def add(k, v):
            if deps.get(k, 0) < v:
                deps[k] = v
        for b in reads:
            if b.w is not None:
                add(*b.w)
            if getattr(b.root, 'psum', False):
                for k, v in b.r.items():
                    if k[0] != e:
                        add(k, v)
        for b in writes:
            if getattr(b.root, 'multi', False):
                continue
            if b.w is not None:
                add(*b.w)
            for k, v in b.r.items():
                add(k, v)
        return deps

    def op(self, e, fn, reads=(), writes=(), pe_acc=False):
        if self.rec is not None:
            self.rec.append(('op', (e, fn, list(reads), list(writes), pe_acc)))
            return None
        deps = self._deps(reads, writes, e)
        if pe_acc:
            deps = {k: v for k, v in deps.items() if k[0] != 'pe'}
        self._wait(e, deps)
        key, cnt = self._eng_token(e)
        ins = fn(self.eng[e])
        ins.then_inc(self.sems[key], 1)
        self.ninst += 1
        for b in reads:
            if b.r.get(key, 0) < cnt:
                b.r[key] = cnt
        for b in writes:
            b.w = (key, cnt); b.r = {}
        return ins

    def dma(self, out_ap, in_ap, reads=(), writes=(), q=None, is_out=False):
        if q is None:
            q = ('sp', 'pool')[self.dq % 2]; self.dq += 1
        if self.rec is not None:
            self.rec.append(('dma', (out_ap, in_ap, list(reads), list(writes), q, is_out)))
            return
        deps = self._deps(reads, writes, q)
        self._wait(q, deps)
        tgt = writes[0] if writes else reads[0]
        key = ('dma', tgt.root.name, 'w' if writes else 'r', q)
        if key not in self.sems:
            self._newsem(key); self.dcnt[key] = 0
        self.dcnt[key] += 16
        val = self.dcnt[key]
        self.eng[q].dma_start(out=out_ap, in_=in_ap).then_inc(self.sems[key], 16)
        self.ninst += 1
        for b in reads:
            if b.r.get(key, 0) < val:
                b.r[key] = val
        for b in writes:
            b.w = (key, val); b.r = {}
        if is_out:
            self.out_sems = [(k, v) for k, v in self.out_sems if k != key] + [(key, val)]

    def record(self, fn):
        assert self.rec is None
        self.rec = []
        fn()
        r, self.rec = self.rec, None
        return r

    def emit(self, *streams):
        streams = [st for st in streams if st]
        pos = [0] * len(streams)
        total = sum(len(st) for st in streams)
        for _ in range(total):
            i = min((i for i in range(len(streams)) if pos[i] < len(streams[i])), key=lambda i: pos[i] / len(streams[i]))
            kind, a = streams[i][pos[i]]; pos[i] += 1
            if kind == 'op':
                self.op(a[0], a[1], a[2], a[3], a[4])
            else:
                self.dma(a[0], a[1], a[2], a[3], a[4], a[5])

    def finish(self):
        for key, val in self.out_sems:
            self.eng['sp'].wait_ge(self.sems[key], val)


class Dense:
    def __init__(self, kb, sq=None):
        self.kb = kb
        self.xT = kb.sb("xT", [128, KC, TT], F32)
        self.hT = kb.sb("hT", [128, KC, TT], BF16)
        self.sq = sq if sq is not None else kb.sb("sq", [128, KC, TT], BF16)
        self.rstd = kb.sb("rstd", [128, TT], F32)
        self.ones = kb.sb("ones", [128, 128], BF16)
        self.wst = [kb.sb("wst%d" % i, [128, KC * GW], F32) for i in range(2)]
        self.wbf = [kb.sb("wbf%d" % i, [128, KC * GW], BF16) for i in range(2)]
        self.wi = 0
        self.ost = [kb.sb("ost%d" % i, [128, TT], F32) for i in range(2)]
        self.oi = 0
        self.pss = kb.ps("ps_ss")
        self.psm = [kb.ps("ps_m%d" % i) for i in range(4)]
        self.pi = 0
        self.ci = 0
        kb.op('dve', lambda e: e.memset(self.ones[:], 1.0), writes=[self.ones])

    def load_w(self, w_ap, nk, gw, cache=None, first=True):
        kb = self.kb
        i = self.wi; self.wi ^= 1
        st, bf = self.wst[i], self.wbf[i]
        n = nk * gw
        if cache is not None and not first:
            kb.dma(bf[:, 0:n], cache[1], reads=[cache[0]], writes=[bf], q='sp')
            return bf, bf[:, 0:n].rearrange("p (k m) -> p k m", m=gw)
        kb.dma(st[:, 0:n], w_ap, writes=[st], q='sp')
        ce = ('dve', 'pool', 'act')[self.ci % 3]; self.ci += 1
        if ce == 'act':
            kb.op(ce, lambda e: e.copy(out=bf[:, 0:n], in_=st[:, 0:n]), reads=[st], writes=[bf])
        else:
            kb.op(ce, lambda e: e.tensor_copy(out=bf[:, 0:n], in_=st[:, 0:n]), reads=[st], writes=[bf])
        if cache is not None:
            kb.dma(cache[1], bf[:, 0:n], reads=[bf], writes=[cache[0]], q='act')
        return bf, bf[:, 0:n].rearrange("p (k m) -> p k m", m=gw)

    def next_ps(self):
        p = self.psm[self.pi]; self.pi = (self.pi + 1) % 4
        return p

    def rmsnorm(self, nw, off, eps, dst_f32=None):
        kb = self.kb; xT, sq, pss, rstd = self.xT, self.sq, self.pss, self.rstd
        kb.op('act', lambda e: e.activation(out=sq[:], in_=xT[:], func=AF.Square), reads=[xT], writes=[sq])
        for k in range(KC):
            kb.op('pe', lambda e, k=k: e.matmul(pss[:], lhsT=self.ones[:], rhs=sq[:, k, :], start=(k == 0), stop=(k == KC - 1)),
                  reads=[self.ones, sq], writes=[pss], pe_acc=(k > 0))
        kb.op('act', lambda e: e.activation(out=rstd[:], in_=pss[:], func=AF.Sqrt, scale=1.0 / D_MODEL, bias=self.epsb(eps)),
              reads=[pss, self.eps_buf], writes=[rstd])
        kb.op('dve', lambda e: e.reciprocal(out=rstd[:], in_=rstd[:]), reads=[rstd], writes=[rstd])
        dst = self.hT if dst_f32 is None else dst_f32
        for k in range(KC):
            en = 'dve'
            kb.op(en, lambda e, k=k: e.scalar_tensor_tensor(out=dst[:, k, :], in0=xT[:, k, :], scalar=nw[:, off + k:off + k + 1], in1=rstd[:],
                                                          op0=ALU.mult, op1=ALU.mult),
                  reads=[xT, nw, rstd], writes=[dst])

    def epsb(self, eps):
        return self.eps_buf[:, self.eps_idx[eps]:self.eps_idx[eps] + 1]

    def init_eps(self, vals):
        kb = self.kb
        self.eps_buf = kb.sb("epsb", [128, len(vals)], F32)
        self.eps_idx = {v: i for i, v in enumerate(vals)}
        for i, v in enumerate(vals):
            kb.op('dve', lambda e, i=i, v=v: e.memset(self.eps_buf[:, i:i + 1], v), writes=[self.eps_buf])

    def gemm(self, w_dram, ng, nk, gw, rhs, consume, cache=None, first=True):
        kb = self.kb
        for g in range(ng):
            bf, wv = self.load_w(w_dram[g], nk, gw, None if cache is None else (cache[0], cache[1][g]), first)
            for m in range(gw // 128):
                ps = self.next_ps()
                for k in range(nk):
                    kb.op('pe', lambda e, k=k, m=m: e.matmul(ps[:], lhsT=wv[:, k, m * 128:(m + 1) * 128], rhs=rhs[:, k, :],
                                                            start=(k == 0), stop=(k == nk - 1)),
                          reads=[bf, rhs], writes=[ps], pe_acc=(k > 0))
                consume(g * (gw // 128) + m, ps)

    def gemm_down(self, w_dram, rhs, consume, cache=None, first=True):
        kb = self.kb
        for g in range(16):
            ps = self.next_ps()
            for hf in range(2):
                bf, wv = self.load_w(w_dram[g, hf], FC // 2, 128, None if cache is None else (cache[0], cache[1][g, hf]), first)
                for k in range(FC // 2):
                    kk = hf * (FC // 2) + k
                    kb.op('pe', lambda e, k=k, kk=kk: e.matmul(ps[:], lhsT=wv[:, k, :], rhs=rhs[:, kk, :],
                                                              start=(kk == 0), stop=(kk == FC - 1)),
                          reads=[bf, rhs], writes=[ps], pe_acc=(kk > 0))
            consume(g, ps)


def build_dense(has_c, has_a, final):
    nc = bass.Bass("TRN2", target_bir_lowering=False)
    dr = lambda n, s, kind="ExternalInput": nc.dram_tensor(n, list(s), F32, kind=kind).ap()
    xT_d = dr("xT", [D_MODEL, NTOK])
    vecs = dr("vecs", [128, 64])
    if has_c:
        yT_d = dr("yT", [D_MODEL, NTOK])
        wout_d = dr("wout", [8, 128, KC * GW])
        wg_d = dr("wg", [22, 128, KC * GW])
        wu_d = dr("wu", [22, 128, KC * GW])
        wd_d = dr("wd", [16, 2, 128, (FC // 2) * 128])
        xo_d = dr("xo", [D_MODEL, NTOK], "ExternalOutput")
    if has_a:
        win_d = dr("win", [NG_IN, 128, KC * GW])
        pj_d = dr("pj", [PROJ_ROWS, NTOK], "ExternalOutput")
    scr = lambda n, shp: nc.dram_tensor(n, list(shp), BF16).ap()
    wscr = Buf(None, "wscr"); wscr.multi = True
    if has_c:
        c_out_s = (wscr, scr("wout_bf", [8, 128, KC * GW])); c_g = (wscr, scr("wg_bf", [22, 128, KC * GW]))
        c_u = (wscr, scr("wu_bf", [22, 128, KC * GW])); c_d = (wscr, scr("wd_bf", [16, 2, 128, (FC // 2) * 128]))
    if has_a:
        c_in_s = (wscr, scr("win_bf", [NG_IN, 128, KC * GW]))
    with ExitStack() as es:
        kb = KB(nc, es)
        if has_c:
            aT = kb.sb("aT", [128, FC, TT], BF16)
            dn = Dense(kb, sq=Buf(aT.ap[:, 0:KC, :], "aT", parent=aT))
        else:
            dn = Dense(kb)
        dn.init_eps([1e-6, 1e-5])
        vb = kb.sb("vecs_sb", [128, 64], F32)
        kb.dma(vb[:], vecs, writes=[vb], q='sp')
        xT = dn.xT
        if has_c:
            yT = kb.sb("yT_sb", [128, KC, TT], F32)
            yb = dn.hT
            sg = [kb.sb("sg%d" % i, [128, TT], F32) for i in range(2)]
            psg = [kb.ps("ps_g%d" % i) for i in range(2)]
        for t in range(NTOK // TT):
            ts = slice(t * TT, (t + 1) * TT)
            kb.dma(xT[:], xT_d.rearrange("(k p) n -> p k n", p=128)[:, :, ts], writes=[xT], q='sp')
            if has_c:
                kb.dma(yT[:], yT_d.rearrange("(k p) n -> p k n", p=128)[:, :, ts], writes=[yT], q='pool')
                sq = dn.sq
                kb.op('act', lambda e: e.activation(out=sq[:, 4:12, :], in_=yT[:, 4:12, :], func=AF.Square), reads=[yT], writes=[sq])
                for g in range(2):
                    for j in range(4):
                        k = 4 + g * 4 + j
                        kb.op('pe', lambda e, k=k, j=j: e.matmul(dn.pss[:], lhsT=dn.ones[:], rhs=sq[:, k, :], start=(j == 0), stop=(j == 3)),
                              reads=[dn.ones, sq], writes=[dn.pss], pe_acc=(j > 0))
                    kb.op('act', lambda e: e.activation(out=dn.rstd[:], in_=dn.pss[:], func=AF.Sqrt, scale=1.0 / 512, bias=dn.epsb(1e-5)),
                          reads=[dn.pss, dn.eps_buf], writes=[dn.rstd])
                    kb.op('dve', lambda e: e.reciprocal(out=dn.rstd[:], in_=dn.rstd[:]), reads=[dn.rstd], writes=[dn.rstd])
                    for j in range(4):
                        k = 4 + g * 4 + j
                        kb.op('dve', lambda e, k=k: e.scalar_tensor_tensor(out=yb[:, k, :], in0=yT[:, k, :], scalar=vb[:, 48 + k - 4:49 + k - 4],
                                                                      in1=dn.rstd[:], op0=ALU.mult, op1=ALU.mult),
                              reads=[yT, vb, dn.rstd], writes=[yb])
                for k in list(range(0, 4)) + list(range(12, 16)):
                    kb.op('pool', lambda e, k=k: e.tensor_copy(out=yb[:, k, :], in_=yT[:, k, :]), reads=[yT], writes=[yb])
                def c_out(r, ps):
                    kb.op('dve', lambda e: e.tensor_tensor(out=xT[:, r, :], in0=ps[:], in1=xT[:, r, :], op=ALU.add), reads=[ps, xT], writes=[xT])
                dn.gemm(wout_d, 8, KC, GW, yb, c_out, c_out_s, t == 0)
                dn.rmsnorm(vb, 16, 1e-6)
                for g in range(22):
                    bfg, wgv = dn.load_w(wg_d[g], KC, GW, (wscr, c_g[1][g]), t == 0)
                    bfu, wuv = dn.load_w(wu_d[g], KC, GW, (wscr, c_u[1][g]), t == 0)
                    for m in range(2):
                        pg, pu = psg
                        for k in range(KC):
                            kb.op('pe', lambda e, k=k, m=m: e.matmul(pg[:], lhsT=wgv[:, k, m * 128:(m + 1) * 128], rhs=dn.hT[:, k, :],
                                                                    start=(k == 0), stop=(k == KC - 1)),
                                  reads=[bfg, dn.hT], writes=[pg], pe_acc=(k > 0))
                        for k in range(KC):
                            kb.op('pe', lambda e, k=k, m=m: e.matmul(pu[:], lhsT=wuv[:, k, m * 128:(m + 1) * 128], rhs=dn.hT[:, k, :],
                                                                    start=(k == 0), stop=(k == KC - 1)),
                                  reads=[bfu, dn.hT], writes=[pu], pe_acc=(k > 0))
                        s_ = sg[m]
                        kb.op('act', lambda e: e.activation(out=s_[:], in_=pg[:], func=AF.Silu), reads=[pg], writes=[s_])
                        r = g * 2 + m
                        kb.op('dve', lambda e, r=r: e.tensor_tensor(out=aT[:, r, :], in0=pu[:], in1=s_[:], op=ALU.mult),
                              reads=[pu, s_], writes=[aT])
                dn.gemm_down(wd_d, aT, c_out, c_d, t == 0)
                if final:
                    dn.rmsnorm(vb, 32, 1e-6, dst_f32=yT)
                    kb.dma(xo_d.rearrange("(k p) n -> p k n", p=128)[:, :, ts], yT[:], reads=[yT], is_out=True, q='sp')
                else:
                    kb.dma(xo_d.rearrange("(k p) n -> p k n", p=128)[:, :, ts], xT[:], reads=[xT], is_out=True, q='sp')
            if has_a:
                dn.rmsnorm(vb, 0, 1e-6)
                def c_in(r, ps):
                    o = dn.ost[dn.oi]; dn.oi ^= 1
                    if r % 2 == 0:
                        kb.op('act', lambda e: e.copy(out=o[:], in_=ps[:]), reads=[ps], writes=[o])
                    else:
                        kb.op('dve', lambda e: e.tensor_copy(out=o[:], in_=ps[:]), reads=[ps], writes=[o])
                    kb.dma(pj_d[r * 128:(r + 1) * 128, ts], o[:], reads=[o], is_out=True)
                dn.gemm(win_d, NG_IN, KC, GW, dn.hT, c_in, c_in_s, t == 0)
        kb.finish()
    return nc


def arrange_w(W, gw, ng=None):
    K, M = W.shape
    if ng is None:
        ng = (M + gw - 1) // gw
    if ng * gw != M:
        Wp = np.zeros((K, ng * gw), np.float32); Wp[:, :M] = W; W = Wp
    return np.ascontiguousarray(W.reshape(K // 128, 128, ng, gw).transpose(2, 1, 0, 3)).reshape(ng, 128, (K // 128) * gw)


def vec_pk(v):
    return np.ascontiguousarray(v.reshape(-1, 128).T)


class V:
    def __init__(self, b, ap):
        self.b = b; self.ap = ap

    def __getitem__(self, idx):
        return V(self.b, self.ap[idx])

    def rr(self, pat, **kw):
        return V(self.b, self.ap.rearrange(pat, **kw))


class TB(Buf):
    def __getitem__(self, idx):
        return V(self, self.ap[idx])


class MX:
    def __init__(self, kb):
        self.kb = kb
        self.bank = [TB(kb.es.enter_context(kb.nc.psum_tensor("pp_b%d" % i, [128, 512], F32)), "bank%d" % i) for i in range(8)]
        self.bi = 0
        for b in self.bank:
            b.psum = True

    def sb(self, name, shape, dtype=F32):
        t = self.kb.es.enter_context(self.kb.nc.sbuf_tensor("sb_" + name, list(shape), dtype))
        return TB(t, name)

    def pz(self):
        b = self.bank[self.bi]; self.bi = (self.bi + 1) % 4
        return b

    @staticmethod
    def _rw(vs):
        return [v.b for v in vs if isinstance(v, V)]

    @staticmethod
    def _a(x):
        return x.ap if isinstance(x, V) else x

    def tt(self, e, out, a, b, op):
        self.kb.op(e, lambda en: en.tensor_tensor(out=out.ap, in0=a.ap, in1=b.ap, op=op), reads=self._rw([a, b]), writes=[out.b])

    def ts(self, e, out, a, s1, op0, s2=None, op1=None):
        if op1 is None:
            self.kb.op(e, lambda en: en.tensor_scalar(out=out.ap, in0=a.ap, scalar1=self._a(s1), scalar2=None, op0=op0),
                       reads=self._rw([a, s1]), writes=[out.b])
        else:
            self.kb.op(e, lambda en: en.tensor_scalar(out=out.ap, in0=a.ap, scalar1=self._a(s1), scalar2=self._a(s2), op0=op0, op1=op1),
                       reads=self._rw([a, s1, s2]), writes=[out.b])

    def stt(self, out, a, s, b, op0, op1):
        self.kb.op('dve', lambda en: en.scalar_tensor_tensor(out=out.ap, in0=a.ap, scalar=self._a(s), in1=b.ap, op0=op0, op1=op1),
                   reads=self._rw([a, s, b]), writes=[out.b])

    def act(self, out, a, func, bias=None, scale=1.0):
        if bias is None:
            self.kb.op('act', lambda en: en.activation(out=out.ap, in_=a.ap, func=func, scale=self._a(scale)),
                       reads=self._rw([a, scale]), writes=[out.b])
        else:
            self.kb.op('act', lambda en: en.activation(out=out.ap, in_=a.ap, func=func, bias=self._a(bias), scale=self._a(scale)),
                       reads=self._rw([a, bias, scale]), writes=[out.b])

    def cp(self, e, out, a):
        if e == 'act':
            self.kb.op(e, lambda en: en.copy(out=out.ap, in_=a.ap), reads=[a.b], writes=[out.b])
        else:
            self.kb.op(e, lambda en: en.tensor_copy(out=out.ap, in_=a.ap), reads=[a.b], writes=[out.b])

    def mm(self, out, lhsT, rhs, start=True, stop=True):
        self.kb.op('pe', lambda en: en.matmul(out.ap, lhsT=lhsT.ap, rhs=rhs.ap, start=start, stop=stop),
                   reads=[lhsT.b, rhs.b], writes=[out.b], pe_acc=(not start))

    def scan(self, out, d0, d1, init, op0, op1):
        self.kb.op('dve', lambda en: en.tensor_tensor_scan(out=out.ap, data0=d0.ap, data1=d1.ap, initial=init, op0=op0, op1=op1),
                   reads=[d0.b, d1.b], writes=[out.b])

    def memset(self, e, out, val):
        self.kb.op(e, lambda en: en.memset(out.ap, val), writes=[out.b])

    def dma_in(self, out, src_ap, q=None):
        self.kb.dma(out.ap, src_ap, writes=[out.b], q=q)

    def dma_out(self, dst_ap, src, q=None):
        self.kb.dma(dst_ap, src.ap, reads=[src.b], is_out=True, q=q)


PP_CONVW = 0
PP_CONVB = 16
PP_DSKIP = 20
PP_DTB = 22
PP_ALOG = 23
PP_MU = 24
PP_W0 = 32; PP_A0 = 33; PP_V0 = 34; PP_KK = 35; PP_KA = 36; PP_RK = 37; PP_LNW = 38; PP_LNB = 39
PP_GC = 40
PP_ZETA = 41
CF_MASKD = 0; CF_XI = 1; CF_NEGM = 2; CF_NML = 3; CF_MU = 4; CF_MUI = 5
NCF = 6
CM_ONES128 = 0; CM_BONES = 1; CM_BONES64 = 2; CM_SEL = 3; CM_SEL2 = 7; CM_I4 = 9; CM_IDENT = 10
NCM = 11
CB_IDENT = 0; CB_SWAP = 1
NCB = 2
EXPM05 = float(np.exp(-0.5))


def build_mixer(S, has_vres):
    nc = bass.Bass("TRN2", target_bir_lowering=False)
    dr = lambda n, s, kind="ExternalInput", dt=F32: nc.dram_tensor(n, list(s), dt, kind=kind).ap()
    ret_in = dr("ret_in", [4, 128, S]); rope = dr("rope", [2, 128, S])
    ssd_zx = dr("ssd_z", [2, 128, S]); ssd_c = dr("ssd_c", [4, 128, 3 + S]); ssd_dt = dr("ssd_dt", [4, S])
    rw_in = dr("rw_in", [8, 128, 1 + S])
    if has_vres:
        vfirst = dr("vfirst", [128, S])
    pp_d = dr("pp", [128, 64]); cf_d = dr("cf", [128, NCF, 512]); cm_d = dr("cm", [128, NCM, 128])
    cb_d = dr("cb", [128, NCB, 128], dt=BF16); rww_d = dr("rww", [128, 5, 128])
    y_ret = dr("y_ret", [128, S], "ExternalOutput"); y_ssd = dr("y_ssd", [2, 128, S], "ExternalOutput")
    y_rw = dr("y_rw", [128, S], "ExternalOutput")
    if not has_vres:
        v_out = dr("v_out", [128, S], "ExternalOutput")
    NTL = S // TT
    with ExitStack() as es:
        kb = KB(nc, es); m = MX(kb)
        T = TT
        pp = m.sb("pp", [128, 64]); cf = m.sb("cf", [128, NCF, 512]); cm = m.sb("cm", [128, NCM, 128])
        cb = m.sb("cb", [128, NCB, 128], BF16); rww = m.sb("rww", [128, 5, 128]); rwb = m.sb("rwb", [128, 5, 128], BF16)
        m.dma_in(pp[:], pp_d); m.dma_in(cf[:], cf_d); m.dma_in(cm[:], cm_d); m.dma_in(cb[:], cb_d); m.dma_in(rww[:], rww_d)
        m.cp('dve', rwb[:], rww[:])
        ident = cb[:, CB_IDENT, :]; swp = cb[:, CB_SWAP, :]
        col = lambda c, n=128: pp[0:n, c:c + 1]
        epsb = m.sb("epsb", [128, 4])
        for i, v_ in enumerate((1e-6, 64e-5, 1e-24)):
            m.memset('dve', epsb[:, i:i + 1], v_)
        ones_f = m.sb("ones_f", [128, 128])
        m.memset('dve', ones_f[:], 1.0)
        omu = m.sb("omu", [128, 8])
        m.ts('dve', omu[:], pp[:, PP_MU:PP_MU + 8], -1.0, ALU.mult, 1.0, ALU.add)
        negA = m.sb("negA", [4, 1])
        m.act(negA[:], pp[0:4, PP_ALOG:PP_ALOG + 1], AF.Exp)
        m.ts('dve', negA[:], negA[:], -1.0, ALU.mult)

        big1 = m.sb("big1", [128, 4112]); big2 = m.sb("big2", [128, 8 * T])
        vw = lambda big, lo, hi, f, nm: TB(big.ap[:, lo:hi].rearrange("p (f s) -> p f s", f=f), nm, parent=big)
        r_in = vw(big2, 0, 4 * T, 4, "r_in"); r_cs = vw(big2, 4 * T, 6 * T, 2, "r_cs")
        qb = m.sb("qb", [128, T], BF16); kbb = m.sb("kbb", [128, T], BF16); vbb = m.sb("vbb", [128, T], BF16)
        rt1 = m.sb("rt1", [128, T]); rt2 = m.sb("rt2", [128, T])
        qr = m.sb("qr", [128, T], BF16); kr = m.sb("kr", [128, T], BF16); qx = m.sb("qx", [128, T], BF16)
        vtok = m.sb("vtok", [128, T], BF16); kztok = m.sb("kztok", [128, T], BF16); pT = m.sb("pT", [128, T], BF16)
        rS = m.sb("rS", [128, 128]); rSb = [m.sb("rSb%d" % i, [128, 128], BF16) for i in range(2)]
        ry = m.sb("ry", [128, T]); rysq = m.sb("rysq", [128, T]); rmean = m.sb("rmean", [128, T]); rvar = m.sb("rvar", [128, T])
        rsg = m.sb("rsg", [128, T])
        m.memset('dve', rS[:], 0.0); m.memset('pool', rSb[0][:], 0.0)
        rsi = [0]

        def retention(t):
            ts_ = slice(t * T, (t + 1) * T)
            m.dma_in(r_in[:], ret_in.rearrange("f p s -> p f s")[:, :, ts_])
            m.dma_in(r_cs[:], rope.rearrange("f p s -> p f s")[:, :, ts_])
            q, k, v, g = (r_in[:, i, :] for i in range(4))
            cos, sin = r_cs[:, 0, :], r_cs[:, 1, :]
            m.cp('pool', qb[:], q); m.cp('pool', kbb[:], k); m.cp('act', vbb[:], v)
            for src, srcb, dst in ((q, qb, qr), (k, kbb, kr)):
                p = m.pz()
                m.mm(p[:], swp, srcb[:])
                m.tt('pool', rt1[:], src, cos, ALU.mult)
                m.tt('dve', rt2[:], p[:], sin, ALU.mult)
                m.tt('dve', dst[:], rt1[:], rt2[:], ALU.add)
            m.tt('pool', qx[:], qr[:], cf[:, CF_XI, :], ALU.mult)
            pv_, pk_, psc = m.pz(), m.pz(), m.pz()
            for c in range(4):
                cs = slice(c * 128, (c + 1) * 128)
                m.mm(pv_[:, cs], vbb[:, cs], ident)
                m.mm(pk_[:, cs], kr[:, cs], ident)
                m.mm(psc[:, cs], kr[:, cs], qr[:, cs])
            m.cp('act', vtok[:], pv_[:])
            m.ts('dve', kztok[:], pk_[:], col(PP_ZETA), ALU.mult)
            m.tt('dve', pT[:], psc[:], cf[:, CF_MASKD, :], ALU.mult)
            po, pst = m.pz(), m.pz()
            for c in range(4):
                cs = slice(c * 128, (c + 1) * 128)
                m.mm(pst[:, cs], kztok[:, cs], vtok[:, cs])
            for c in range(4):
                cs = slice(c * 128, (c + 1) * 128)
                sb_ = rSb[rsi[0]]
                m.mm(po[:, cs], vtok[:, cs], pT[:, cs], start=True, stop=False)
                m.mm(po[:, cs], sb_[:], qx[:, cs], start=False, stop=True)
                m.stt(rS[:], rS[:], col(PP_GC), pst[:, cs], ALU.mult, ALU.add)
                rsi[0] ^= 1
                m.cp('act', rSb[rsi[0]][:], rS[:])
            m.cp('act', ry[:], po[:])
            m.act(rysq[:], po[:], AF.Square)
            pm, pq = m.pz(), m.pz()
            m.mm(pm[:], cm[:, CM_ONES128, :], ry[:])
            m.mm(pq[:], cm[:, CM_ONES128, :], rysq[:])
            m.cp('act', rmean[:], pm[:])
            m.tt('dve', rvar[:], rmean[:], rmean[:], ALU.mult)
            m.tt('dve', rvar[:], pq[:], rvar[:], ALU.subtract)
            m.act(rvar[:], rvar[:], AF.Sqrt, bias=epsb[:, 0:1])
            m.kb.op('dve', lambda en: en.reciprocal(out=rvar.ap[:], in_=rvar.ap[:]), reads=[rvar], writes=[rvar])
            m.tt('dve', ry[:], ry[:], rmean[:], ALU.subtract)
            m.tt('dve', ry[:], ry[:], rvar[:], ALU.mult)
            m.act(rsg[:], g, AF.Silu)
            m.tt('dve', ry[:], ry[:], rsg[:], ALU.mult)
            m.dma_out(y_ret[:, ts_], ry[:])

        s_z = vw(big2, 6 * T, 8 * T, 2, "s_z"); s_c = vw(big1, 0, 4 * (3 + T), 4, "s_c"); s_dt = m.sb("s_dt", [4, T])
        s_cv = vw(big1, 4 * (3 + T), 4 * (3 + T) + 4 * T, 4, "s_cv"); s_Bb = m.sb("s_Bb", [128, T], BF16); s_Cb = m.sb("s_Cb", [128, T], BF16)
        s_a = m.sb("s_a", [4, T]); s_ac = m.sb("s_ac", [4, T])
        s_ab = m.sb("s_ab", [128, 2, T]); s_E = m.sb("s_E", [128, 2, T]); s_dte = m.sb("s_dte", [128, 2, T])
        s_xdt = m.sb("s_xdt", [128, 2, T]); s_xdtb = m.sb("s_xdtb", [128, 2, T], BF16); s_xdteb = m.sb("s_xdteb", [128, 2, T], BF16)
        s_xtokP = m.sb("s_xtokP", [128, 4, 4, 128], BF16)
        s_xetok = m.sb("s_xetok", [128, 4, 256], BF16)
        s_Btok = m.sb("s_Btok", [128, T], BF16)
        s_negcol = m.sb("s_negcol", [128, 16]); s_cdec = m.sb("s_cdec", [128, 4, 4])
        s_cbT = m.sb("s_cbT", [128, T]); s_seg = rt2; s_G = [m.sb("s_G%d" % i, [128, T], BF16) for i in range(2)]
        s_S = m.sb("s_S", [128, 256]); s_Sb = [m.sb("s_Sb%d" % i, [128, 256], BF16) for i in range(2)]
        s_t1 = ry; s_sz = rt1; s_y = m.sb("s_y", [128, 2, T])
        m.memset('dve', s_S[:], 0.0); m.memset('pool', s_Sb[0][:], 0.0); m.memset('pool', s_xtokP[:], 0.0)
        ssi = [0]

        def ssd(t):
            ts_ = slice(t * T, (t + 1) * T)
            m.dma_in(s_z[:], ssd_zx.rearrange("f p s -> p f s")[:, :, ts_])
            m.dma_in(s_c[:], ssd_c.rearrange("f p s -> p f s")[:, :, t * T:t * T + T + 3])
            m.dma_in(s_dt[:], ssd_dt[:, ts_])
            for b in range(4):
                o = s_cv[:, b, :]
                m.act(o, s_c[:, b, 0:T], AF.Identity, bias=col(PP_CONVB + b), scale=col(PP_CONVW + b * 4))
                for k in range(1, 4):
                    m.stt(o, s_c[:, b, k:k + T], col(PP_CONVW + b * 4 + k), o, ALU.mult, ALU.add)
                m.act(o, o, AF.Silu)
            m.cp('pool', s_Bb[:], s_cv[:, 2, :]); m.cp('pool', s_Cb[:], s_cv[:, 3, :])
            m.act(s_dt[:], s_dt[:], AF.Exp, bias=pp[0:4, PP_DTB:PP_DTB + 1])
            m.act(s_dt[:], s_dt[:], AF.Ln, bias=1.0)
            m.ts('dve', s_a[:], s_dt[:], negA[:, 0:1], ALU.mult)
            for c in range(4):
                cs = slice(c * 128, (c + 1) * 128)
                m.scan(s_ac[:, cs], ones_f[0:4, :], s_a[:, cs], 0.0, ALU.mult, ALU.add)
            for b in range(2):
                p1, p2 = m.pz(), m.pz()
                m.mm(p1[:], cm[0:4, CM_SEL2 + b, :], s_ac[:])
                m.mm(p2[:], cm[0:4, CM_SEL2 + b, :], s_dt[:])
                m.cp('act', s_ab[:, b, :], p1[:])
                m.act(s_E[:, b, :], p1[:], AF.Exp)
                m.tt('dve', s_xdt[:, b, :], s_cv[:, b, :], p2[:], ALU.mult)
                for c in range(4):
                    cs = slice(c * 128, (c + 1) * 128)
                    m.act(s_dte[:, b, cs], s_ab[:, b, cs], AF.Exp, bias=s_ab[:, b, c * 128 + 127:c * 128 + 128], scale=-1.0)
                m.cp('pool', s_xdtb[:, b, :], s_xdt[:, b, :])
                m.tt('pool', s_xdteb[:, b, :], s_xdt[:, b, :], s_dte[:, b, :], ALU.mult)
            for b in range(2):
                p1, p2 = m.pz(), m.pz()
                for c in range(4):
                    cs = slice(c * 128, (c + 1) * 128)
                    m.mm(p1[:, cs], s_xdtb[:, b, cs], ident)
                    m.mm(p2[:, cs], s_xdteb[:, b, cs], ident)
                for hl in range(2):
                    h = b * 2 + hl
                    m.cp('act', s_xtokP[:, :, h, hl * 64:(hl + 1) * 64], p1[:].rr("p (c x) -> p c x", c=4)[:, :, hl * 64:(hl + 1) * 64])
                m.cp('dve', s_xetok[:, :, b * 128:(b + 1) * 128], p2[:].rr("p (c x) -> p c x", c=4))
            pB, pcb, pcol = m.pz(), m.pz(), m.pz()
            for c in range(4):
                cs = slice(c * 128, (c + 1) * 128)
                m.mm(pB[:, cs], s_Bb[:, cs], ident)
                m.mm(pcb[:, cs], s_Bb[:, cs], s_Cb[:, cs])
                m.mm(pcol[:, c * 4:(c + 1) * 4], s_ac[:, cs], cm[0:4, CM_I4, 0:4])
            m.cp('act', s_Btok[:], pB[:])
            m.cp('act', s_cbT[:], pcb[:])
            m.ts('dve', s_negcol[:], pcol[:, 0:16], -1.0, ALU.mult)
            py = [m.bank[4], m.bank[5]]
            for h in range(4):
                b, hl = h // 2, h % 2
                pab = m.pz()
                m.mm(pab[:], cm[0:4, CM_SEL + h, :], s_ac[:])
                m.tt('dve', s_seg[:], pab[:], cf[:, CF_NEGM, :], ALU.add)
                m.act(s_cdec[:, h, :], pab[:].rr("p (c x) -> p c x", c=4)[:, :, 127], AF.Exp)
                for c in range(4):
                    cs = slice(c * 128, (c + 1) * 128)
                    m.act(s_seg[:, cs], s_seg[:, cs], AF.Exp, bias=s_negcol[:, c * 4 + h:c * 4 + h + 1])
                G = s_G[h % 2]
                m.tt('dve', G[:], s_cbT[:], s_seg[:], ALU.mult)
                if hl == 1:
                    for c in range(4):
                        cs = slice(c * 128, (c + 1) * 128)
                        m.mm(py[b][:, cs], s_xtokP[:, c, h - 1, :], s_G[0][:, cs], start=True, stop=False)
                        m.mm(py[b][:, cs], s_xtokP[:, c, h, :], s_G[1][:, cs], start=False, stop=True)
            pS = [m.pz(), m.pz()]
            for c in range(4):
                m.mm(pS[c // 2][:, (c % 2) * 256:(c % 2 + 1) * 256], s_Btok[:, c * 128:(c + 1) * 128], s_xetok[:, c, :])
            poff = [m.pz(), m.pz()]
            for c in range(4):
                cs = slice(c * 128, (c + 1) * 128)
                sb_ = s_Sb[ssi[0]]
                for b in range(2):
                    m.mm(poff[b][:, cs], sb_[:, b * 128:(b + 1) * 128], s_Cb[:, cs])
                for h in range(4):
                    hs = slice(h * 64, (h + 1) * 64)
                    m.stt(s_S[:, hs], s_S[:, hs], s_cdec[:, h, c:c + 1], pS[c // 2][:, (c % 2) * 256 + h * 64:(c % 2) * 256 + (h + 1) * 64],
                          ALU.mult, ALU.add)
                ssi[0] ^= 1
                m.cp('act', s_Sb[ssi[0]][:], s_S[:])
            for b in range(2):
                m.tt('dve', s_t1[:], poff[b][:], s_E[:, b, :], ALU.mult)
                m.tt('dve', s_t1[:], s_t1[:], py[b][:], ALU.add)
                m.stt(s_t1[:], s_cv[:, b, :], col(PP_DSKIP + b), s_t1[:], ALU.mult, ALU.add)
                m.act(s_sz[:], s_z[:, b, :], AF.Silu)
                m.tt('dve', s_y[:, b, :], s_t1[:], s_sz[:], ALU.mult)
            m.dma_out(y_ssd.rearrange("f p s -> p f s")[:, :, ts_], s_y[:])

        w_in = vw(big1, 0, 8 * (1 + T), 8, "w_in"); w_mx = vw(big2, 0, 8 * T, 8, "w_mx"); w_tmp = rt1
        w_thb = m.sb("w_thb", [128, T], BF16); w_xab = m.sb("w_xab", [128, T], BF16); w_sgb = m.sb("w_sgb", [128, 2, T], BF16)
        w_pvb = m.sb("w_pvb", [128, T], BF16)
        w_lw = m.sb("w_lw", [128, T]); w_a = m.sb("w_a", [128, T]); w_g = m.sb("w_g", [128, T]); w_v = m.sb("w_v", [128, T])
        w_kk = m.sb("w_kk", [128, T]); w_km = m.sb("w_km", [128, T]); w_b = m.sb("w_b", [128, T]); w_bon = m.sb("w_bon", [128, T])
        w_L = rt2; w_W = m.sb("w_W", [128, T]); w_Wi = m.sb("w_Wi", [128, T]); w_Wp = rsg
        w_vf = m.sb("w_vf", [128, T])
        NCH = T // 64
        BDn = ("RT", "KT", "BT", "KA", "VF")
        BDf = {n: m.sb("w_bd" + n, [128, NCH, 128], BF16) for n in BDn}
        for n in BDn:
            m.memset('pool', BDf[n][:], 0.0)
        w_Vt = m.sb("w_Vt", [128, 4, 128], BF16); w_KTt = m.sb("w_KTt", [128, 4, 128], BF16); w_nBTt = m.sb("w_nBTt", [128, 4, 128], BF16)
        w_N = [m.sb("w_N%d" % i, [128, 4, 128], BF16) for i in range(2)]
        w_NT = [m.sb("w_NT%d" % i, [128, 4, 128], BF16) for i in range(2)]
        w_AkkT = m.sb("w_AkkT", [128, 4, 128], BF16); w_ArkT = m.sb("w_ArkT", [128, 4, 128], BF16); w_nArbT = m.sb("w_nArbT", [128, 4, 128], BF16)
        w_Z = m.sb("w_Z", [128, 4, 256]); w_Zb = m.sb("w_Zb", [128, 4, 256], BF16)
        rww16 = rww.ap[:].bitcast(BF16)
        w_RpT_ = [m.sb("w_RpT0", [128, 4, 128], BF16), TB(rww16[:, 0:2, :].rearrange("p a (b x) -> p (a b) x", x=128), "w_RpT1", parent=rww)]
        w_Y0T_ = [m.sb("w_Y0T%d" % i, [128, 4, 128]) for i in range(2)]
        w_Gp_ = [m.sb("w_Gp0", [128, 4, 128], BF16), TB(rww16[:, 2:4, :].rearrange("p a (b x) -> p (a b) x", x=128), "w_Gp1", parent=rww)]
        w_DdW_ = [m.sb("w_DdW%d" % i, [128, 4, 128]) for i in range(2)]; w_st1 = m.sb("w_st1", [128, 128])
        w_ST = m.sb("w_ST", [128, 128]); w_STb = [m.sb("w_STb%d" % i, [128, 128], BF16) for i in range(2)]
        w_y = m.sb("w_y", [128, T]); w_ysq = rysq; w_mean = rmean; w_var = rvar
        m.memset('dve', w_ST[:], 0.0); m.memset('pool', w_STb[0][:], 0.0)
        wsi = [0]

        def rw_prep(t):
            ts_ = slice(t * T, (t + 1) * T)
            m.dma_in(w_in[:], rw_in.rearrange("f p s -> p f s")[:, :, t * T:t * T + T + 1])
            if has_vres:
                m.dma_in(w_vf[:], vfirst[:, ts_])
            for b in range(8):
                m.ts('pool', w_tmp[:], w_in[:, b, 0:T], pp[:, PP_MU + b:PP_MU + b + 1], ALU.mult)
                m.stt(w_mx[:, b, :], w_in[:, b, 1:T + 1], omu[:, b:b + 1], w_tmp[:], ALU.mult, ALU.add)
            r_, k_, vm = w_mx[:, 0, :], w_mx[:, 1, :], w_mx[:, 2, :]
            m.act(w_thb[:], w_mx[:, 3, :], AF.Tanh)
            m.cp('pool', w_xab[:], w_mx[:, 4, :])
            m.act(w_sgb[:], w_mx[:, 5:7, :], AF.Sigmoid)
            p = m.pz(); m.mm(p[:], rwb[:, 0, :], w_thb[:])
            m.act(w_lw[:], p[:], AF.Sigmoid, bias=col(PP_W0))
            m.ts('dve', w_lw[:], w_lw[:], -EXPM05, ALU.mult)
            p = m.pz(); m.mm(p[:], rwb[:, 1, :], w_xab[:])
            m.act(w_a[:], p[:], AF.Sigmoid, bias=col(PP_A0))
            p = m.pz()
            m.mm(p[:], rwb[:, 2, :], w_sgb[:, 0, :], start=True, stop=False)
            m.mm(p[:], rwb[:, 3, :], w_sgb[:, 1, :], start=False, stop=True)
            m.cp('act', w_g[:], p[:])
            if has_vres:
                m.cp('pool', w_pvb[:], w_mx[:, 7, :])
                p = m.pz(); m.mm(p[:], rwb[:, 4, :], w_pvb[:])
                m.act(w_tmp[:], p[:], AF.Sigmoid, bias=col(PP_V0))
                m.tt('dve', w_v[:], w_vf[:], vm, ALU.subtract)
                m.tt('dve', w_v[:], w_v[:], w_tmp[:], ALU.mult)
                m.tt('dve', w_v[:], w_v[:], vm, ALU.add)
            else:
                m.cp('pool', w_v[:], vm)
                m.dma_out(v_out[:, ts_], w_v[:])
            m.ts('dve', w_kk[:], k_, col(PP_KK), ALU.mult)
            m.tt('pool', w_tmp[:], w_kk[:], w_kk[:], ALU.mult)
            p = m.pz(); m.mm(p[:], cm[:, CM_BONES, :], w_tmp[:])
            m.act(w_tmp[:], p[:], AF.Sqrt)
            m.ts('dve', w_tmp[:], w_tmp[:], 1e-12, ALU.max)
            m.kb.op('dve', lambda en: en.reciprocal(out=w_tmp.ap[:], in_=w_tmp.ap[:]), reads=[w_tmp], writes=[w_tmp])
            m.tt('dve', w_kk[:], w_kk[:], w_tmp[:], ALU.mult)
            m.ts('dve', w_km[:], w_a[:], -1.0, ALU.add, col(PP_KA), ALU.mult)
            m.stt(w_km[:], w_km[:], 1.0, k_, ALU.add, ALU.mult)
            m.tt('pool', w_b[:], w_kk[:], w_a[:], ALU.mult)
            m.stt(w_tmp[:], r_, col(PP_RK), w_km[:], ALU.mult, ALU.mult)
            p = m.pz(); m.mm(p[:], cm[:, CM_BONES, :], w_tmp[:])
            m.tt('dve', w_bon[:], p[:], w_v[:], ALU.mult)
            for c in range(NCH):
                cs = slice(c * 64, (c + 1) * 64)
                m.scan(w_L[:, cs], ones_f[:, 0:64], w_lw[:, cs], 0.0, ALU.mult, ALU.add)
            m.act(w_W[:], w_L[:], AF.Exp)
            m.act(w_Wi[:], w_L[:], AF.Exp, scale=-1.0)
            m.tt('pool', w_Wp[:], w_L[:], w_lw[:], ALU.subtract)
            m.act(w_Wp[:], w_Wp[:], AF.Exp)
            for name, x0, x1 in (("RT", r_, w_W[:]), ("KT", w_km[:], w_Wi[:]), ("BT", w_b[:], w_Wi[:]), ("KA", w_kk[:], w_Wp[:])):
                for hh in range(2):
                    ps_ = slice(hh * 64, (hh + 1) * 64)
                    m.tt('dve', BDf[name][ps_, :, hh * 64:(hh + 1) * 64], x0[ps_, :].rr("p (c x) -> p c x", x=64),
                         x1[ps_, :].rr("p (c x) -> p c x", x=64), ALU.mult)
            for hh in range(2):
                ps_ = slice(hh * 64, (hh + 1) * 64)
                m.cp('pool', BDf["VF"][ps_, :, hh * 64:(hh + 1) * 64], w_v[ps_, :].rr("p (c x) -> p c x", x=64))
        RT, KT, BT, KA, VF = (BDf[n] for n in BDn)
        mk = lambda i: cf[:, i, :].rr("p (c x) -> p c x", c=4)
        p4 = lambda pb: pb[:].rr("p (c x) -> p c x", c=4)
        pY = m.bank[6]

        def rw_pre(qd):
            if True:
                w_RpT, w_Y0T, w_Gp, w_DdW = w_RpT_[qd % 2], w_Y0T_[qd % 2], w_Gp_[qd % 2], w_DdW_[qd % 2]
                c0 = qd * 4
                pa, pb_, pc, pd = m.pz(), m.pz(), m.pz(), m.pz()
                for c in range(4):
                    cs = slice(c * 128, (c + 1) * 128)
                    m.mm(pa[:, cs], VF[:, c0 + c, :], ident)
                    m.mm(pb_[:, cs], KA[:, c0 + c, :], ident)
                    m.mm(pc[:, cs], KT[:, c0 + c, :], ident)
                    m.mm(pd[:, cs], BT[:, c0 + c, :], ident)
                m.cp('act', w_Vt[:], p4(pa))
                m.cp('act', w_Z[:, :, 128:256], p4(pb_))
                m.cp('dve', w_Zb[:, :, 128:256], p4(pb_))
                m.cp('act', w_KTt[:], p4(pc))
                m.ts('dve', w_nBTt[:], p4(pd), -1.0, ALU.mult)
                pa, pb_, pc = m.pz(), m.pz(), m.pz()
                for c in range(4):
                    cs = slice(c * 128, (c + 1) * 128)
                    m.mm(pa[:, cs], KA[:, c0 + c, :], BT[:, c0 + c, :])
                    m.mm(pb_[:, cs], BT[:, c0 + c, :], KA[:, c0 + c, :])
                    m.mm(pc[:, cs], KT[:, c0 + c, :], KA[:, c0 + c, :])
                m.tt('dve', w_N[0][:], p4(pa), mk(CF_NML), ALU.mult)
                m.stt(w_NT[0][:], p4(pb_), -1.0, mk(CF_MU), ALU.mult, ALU.mult)
                m.tt('dve', w_AkkT[:], p4(pc), mk(CF_MU), ALU.mult)
                pd, pe_ = m.pz(), m.pz()
                for c in range(4):
                    cs = slice(c * 128, (c + 1) * 128)
                    m.mm(pd[:, cs], KT[:, c0 + c, :], RT[:, c0 + c, :])
                    m.mm(pe_[:, cs], BT[:, c0 + c, :], RT[:, c0 + c, :])
                m.tt('dve', w_ArkT[:], p4(pd), mk(CF_MUI), ALU.mult)
                m.stt(w_nArbT[:], p4(pe_), -1.0, mk(CF_MUI), ALU.mult, ALU.mult)
                pa = m.pz()
                for c in range(4):
                    m.mm(pa[:, c * 128:(c + 1) * 128], w_AkkT[:, c, :], w_Vt[:, c, :])
                m.cp('act', w_Z[:, :, 0:128], p4(pa))
                m.cp('dve', w_Zb[:, :, 0:128], p4(pa))
                for j in range(6):
                    cur, nxt = j % 2, (j + 1) % 2
                    pz1, pz2 = m.pz(), m.pz()
                    for c in range(4):
                        pzz = (pz1, pz2)[c // 2]
                        m.mm(pzz[:, (c % 2) * 256:(c % 2 + 1) * 256], w_NT[cur][:, c, :], w_Zb[:, c, :])
                    for hf, pzz in enumerate((pz1, pz2)):
                        zs = w_Z[:, hf * 2:hf * 2 + 2, :]
                        m.tt('dve', zs, pzz[:].rr("p (c x) -> p c x", c=2), zs, ALU.add)
                        m.cp('act', w_Zb[:, hf * 2:hf * 2 + 2, :], zs)
                    if j < 5:
                        pn, pnt = m.pz(), m.pz()
                        for c in range(4):
                            cs = slice(c * 128, (c + 1) * 128)
                            m.mm(pn[:, cs], w_NT[cur][:, c, :], w_N[cur][:, c, :])
                            m.mm(pnt[:, cs], w_N[cur][:, c, :], w_NT[cur][:, c, :])
                        m.cp('act', w_N[nxt][:], p4(pn))
                        m.cp('dve', w_NT[nxt][:], p4(pnt))
                U0 = lambda c: w_Zb[:, c, 0:128]
                Ktp = lambda c: w_Zb[:, c, 128:256]
                pa, pb_, pc, pd = m.pz(), m.pz(), m.pz(), m.pz()
                for c in range(4):
                    cs = slice(c * 128, (c + 1) * 128)
                    m.mm(pa[:, cs], Ktp(c), w_nArbT[:, c, :])
                    m.mm(pb_[:, cs], w_Vt[:, c, :], w_ArkT[:, c, :], start=True, stop=False)
                    m.mm(pb_[:, cs], U0(c), w_nArbT[:, c, :], start=False, stop=True)
                    m.mm(pc[:, cs], Ktp(c), w_nBTt[:, c, :])
                    m.mm(pd[:, cs], w_KTt[:, c, :], w_Vt[:, c, :], start=True, stop=False)
                    m.mm(pd[:, cs], w_nBTt[:, c, :], U0(c), start=False, stop=True)
                m.tt('dve', w_RpT[:], p4(pa), RT[:, c0:c0 + 4, :], ALU.add)
                m.cp('act', w_Y0T[:], p4(pb_))
                m.cp('act', w_Gp[:], p4(pc))
                for c in range(4):
                    wc = w_W[:, (c0 + c) * 64 + 63:(c0 + c) * 64 + 64]
                    m.ts('dve', w_DdW[:, c, :], pd[:, c * 128:(c + 1) * 128], wc, ALU.mult)

        def rw_state(qd):
            if True:
                c0 = qd * 4
                w_RpT, w_Y0T, w_Gp, w_DdW = w_RpT_[qd % 2], w_Y0T_[qd % 2], w_Gp_[qd % 2], w_DdW_[qd % 2]
                for c in range(4):
                    cs = slice(c * 128, (c + 1) * 128)
                    wc = w_W[:, (c0 + c) * 64 + 63:(c0 + c) * 64 + 64]
                    stb = w_STb[wsi[0]]
                    m.mm(pY[:, cs], stb[:], w_RpT[:, c, :])
                    pst = m.bank[7]
                    m.mm(pst[:, 0:128], w_Gp[:, c, :], stb[:])
                    m.stt(w_st1[:], pst[:, 0:128], wc, w_DdW[:, c, :], ALU.mult, ALU.add)
                    m.stt(w_ST[:], w_ST[:], wc, w_st1[:], ALU.mult, ALU.add)
                    wsi[0] ^= 1
                    m.cp('act', w_STb[wsi[0]][:], w_ST[:])
                for hh in range(2):
                    ps_ = slice(hh * 64, (hh + 1) * 64)
                    m.tt('dve', w_y[ps_, qd * 256:(qd + 1) * 256].rr("p (c x) -> p c x", x=64),
                         p4(pY)[ps_, :, hh * 64:(hh + 1) * 64], w_Y0T[ps_, :, hh * 64:(hh + 1) * 64], ALU.add)

        def rw_post(t):
            ts_ = slice(t * T, (t + 1) * T)
            m.tt('pool', w_ysq[:], w_y[:], w_y[:], ALU.mult)
            pm, pq = m.bank[7], m.bank[6]
            m.mm(pm[:], cm[:, CM_BONES64, :], w_y[:])
            m.mm(pq[:], cm[:, CM_BONES64, :], w_ysq[:])
            m.cp('act', w_mean[:], pm[:])
            m.tt('dve', w_var[:], w_mean[:], w_mean[:], ALU.mult)
            m.tt('dve', w_var[:], pq[:], w_var[:], ALU.subtract)
            m.act(w_var[:], w_var[:], AF.Sqrt, bias=epsb[:, 1:2])
            m.kb.op('dve', lambda en: en.reciprocal(out=w_var.ap[:], in_=w_var.ap[:]), reads=[w_var], writes=[w_var])
            m.tt('dve', w_y[:], w_y[:], w_mean[:], ALU.subtract)
            m.tt('dve', w_y[:], w_y[:], w_var[:], ALU.mult)
            m.ts('dve', w_y[:], w_y[:], col(PP_LNW), ALU.mult, col(PP_LNB), ALU.add)
            m.tt('dve', w_y[:], w_y[:], w_bon[:], ALU.add)
            m.tt('dve', w_y[:], w_y[:], w_g[:], ALU.mult)
            m.dma_out(y_rw[:, ts_], w_y[:])

        which = getattr(build_mixer, "which", "rsw")
        pending = []

        def front(t):
            if "r" in which:
                retention(t)
            if "s" in which:
                ssd(t)

        for t in range(NTL):
            R = kb.record(lambda: front(t))
            kb.emit(R, pending)
            pending = []
            if "w" in which:
                kb.emit(kb.record(lambda: (rw_prep(t), rw_pre(0))))
                kb.emit(kb.record(lambda: rw_pre(1)), kb.record(lambda: rw_state(0)))
                pending = kb.record(lambda: (rw_state(1), rw_post(t)))
        kb.emit(pending)
        kb.finish()
        print("mixer instrs", kb.ninst, "sems", kb.nsem, flush=True)
    return nc


def arrange_wd(W):
    return np.ascontiguousarray(W.reshape(2, 22, 128, 16, 128).transpose(3, 0, 2, 1, 4)).reshape(16, 2, 128, 22 * 128)


import ml_dtypes


def mixer_consts(c):
    gamma = 1.0 - 2.0 ** (-5 - c)
    idx = np.arange(128)
    cf = np.zeros((128, NCF, 512), np.float64)
    i4 = np.tile(idx, 4)
    rel = i4[None, :] - idx[:, None]
    cf[:, CF_MASKD, :] = np.where(rel >= 0, gamma ** np.maximum(rel, 0), 0.0) * (128 ** -0.5)
    cf[:, CF_XI, :] = (gamma ** (i4 + 1.0))[None, :]
    cf[:, CF_NEGM, :] = np.where(rel >= 0, 0.0, -30000.0)
    same = (idx[:, None] // 64) == (idx[None, :] // 64)
    lo = same & ((idx[:, None] % 64) > (idx[None, :] % 64))
    up = same & ((idx[:, None] % 64) < (idx[None, :] % 64))
    upi = same & ((idx[:, None] % 64) <= (idx[None, :] % 64))
    cf[:, CF_NML, :] = np.tile(-lo.astype(np.float64), (1, 4))
    cf[:, CF_MU, :] = np.tile(up.astype(np.float64), (1, 4))
    cf[:, CF_MUI, :] = np.tile(upi.astype(np.float64), (1, 4))
    cm = np.zeros((128, NCM, 128), np.float64)
    cm[:, CM_ONES128, :] = 1.0 / 128
    cm[:, CM_BONES, :] = same
    cm[:, CM_BONES64, :] = same / 64.0
    for h in range(4):
        cm[h, CM_SEL + h, :] = 1.0
    for b in range(2):
        for mcol in range(128):
            cm[2 * b + mcol // 64, CM_SEL2 + b, mcol] = 1.0
    cm[0:4, CM_I4, 0:4] = np.eye(4)
    cm[:, CM_IDENT, :] = np.eye(128)
    cb = np.zeros((128, NCB, 128), np.float32)
    cb[:, CB_IDENT, :] = np.eye(128)
    cb[(idx + 64) % 128, CB_SWAP, idx] = 1.0
    gc = gamma ** 128
    zeta = gamma ** (127.0 - idx) * (128 ** -0.5)
    return cf.astype(np.float32), cm.astype(np.float32), cb.astype(ml_dtypes.bfloat16), np.float32(gc), zeta.astype(np.float32)


def rope_tables(S):
    half = 64
    inv_freq = (10000.0 ** (-np.arange(half, dtype=np.float32) / half)).astype(np.float32)
    ang = np.arange(S, dtype=np.float32)[:, None] * inv_freq[None, :]
    cos = np.cos(ang.astype(np.float64)); sin = np.sin(ang.astype(np.float64))
    t = np.zeros((2, 128, S), np.float32)
    t[0, :64] = cos.T; t[0, 64:] = cos.T
    t[1, :64] = -sin.T; t[1, 64:] = sin.T
    return t


def mixer_params(c, l, P, gc, zeta):
    pp = np.zeros((128, 64), np.float32); p = np.arange(128)
    g = c // 2
    chans = [c * 256 + p, c * 256 + 128 + p, 1024 + g * 128 + p, 1280 + g * 128 + p]
    for b, ch in enumerate(chans):
        for k in range(4):
            pp[:, PP_CONVW + b * 4 + k] = P['ssm_conv_w'][l, k, ch]
        pp[:, PP_CONVB + b] = P['ssm_conv_b'][l, ch]
    for b in range(2):
        pp[:, PP_DSKIP + b] = P['ssm_d'][l, 4 * c + 2 * b + p // 64]
    pp[0:4, PP_DTB] = P['ssm_dt_bias'][l, 4 * c:4 * c + 4]
    pp[0:4, PP_ALOG] = P['ssm_a_log'][l, 4 * c:4 * c + 4]
    mu = P['rwkv_mu'][l]
    for b in range(3):
        pp[:, PP_MU + b] = mu[b * 512 + c * 128 + p]
    pp[0:96, PP_MU + 3] = mu[1536:1632]; pp[0:96, PP_MU + 4] = mu[1632:1728]
    pp[:, PP_MU + 5] = mu[1728:1856]; pp[:, PP_MU + 6] = mu[1856:1984]
    sl = slice(c * 128, (c + 1) * 128)
    if l > 0:
        pp[0:64, PP_MU + 7] = P['rwkv_mu_v'][l - 1]
        pp[:, PP_V0] = P['rwkv_v0'][l - 1, sl]
    pp[:, PP_W0] = P['rwkv_w0'][l, sl]; pp[:, PP_A0] = P['rwkv_a0'][l, sl]
    pp[:, PP_KK] = P['rwkv_k_k'][l, sl]; pp[:, PP_KA] = P['rwkv_k_a'][l, sl]
    pp[:, PP_RK] = P['rwkv_r_k'][l].reshape(512)[sl]
    pp[:, PP_LNW] = P['rwkv_ln_w'][l, sl]; pp[:, PP_LNB] = P['rwkv_ln_b'][l, sl]
    pp[:, PP_GC] = gc; pp[:, PP_ZETA] = zeta
    rww = np.zeros((128, 5, 128), np.float32)
    rww[0:96, 0] = P['rwkv_w2'][l][:, sl]; rww[0:96, 1] = P['rwkv_a2'][l][:, sl]
    rww[:, 2] = P['rwkv_g2'][l][0:128, sl]; rww[:, 3] = P['rwkv_g2'][l][128:256, sl]
    if l > 0:
        rww[0:64, 4] = P['rwkv_v2'][l - 1][:, sl]
    return pp, rww


def mixer_inputs(c, PJ):
    S = PJ.shape[1]; g = c // 2
    pad = lambda a, n: np.concatenate([np.zeros((a.shape[0], n), np.float32), a], axis=1)
    rows = lambda a: np.concatenate([a, np.zeros((128 - a.shape[0], a.shape[1]), np.float32)], axis=0) if a.shape[0] < 128 else a
    ret_in = np.stack([PJ[f * 512 + c * 128:f * 512 + (c + 1) * 128] for f in range(4)])
    ssd_z = np.stack([PJ[2048 + c * 256 + b * 128:2048 + c * 256 + (b + 1) * 128] for b in range(2)])
    ssd_c = np.stack([pad(PJ[3072 + c * 256:3072 + c * 256 + 128], 3), pad(PJ[3072 + c * 256 + 128:3072 + c * 256 + 256], 3),
                      pad(PJ[4096 + g * 128:4096 + (g + 1) * 128], 3), pad(PJ[4352 + g * 128:4352 + (g + 1) * 128], 3)])
    ssd_dt = np.ascontiguousarray(PJ[4608 + 4 * c:4608 + 4 * c + 4])
    R0 = 4624
    blocks = [PJ[R0 + c * 128:R0 + (c + 1) * 128], PJ[R0 + 512 + c * 128:R0 + 512 + (c + 1) * 128],
              PJ[R0 + 1024 + c * 128:R0 + 1024 + (c + 1) * 128], rows(PJ[6160:6256]), rows(PJ[6256:6352]),
              PJ[6352:6480], PJ[6480:6608], rows(PJ[6608:6672])]
    rw_in = np.stack([pad(b_, 1) for b_ in blocks])
    return {"ret_in": np.ascontiguousarray(ret_in), "ssd_z": np.ascontiguousarray(ssd_z), "ssd_c": np.ascontiguousarray(ssd_c),
            "ssd_dt": ssd_dt, "rw_in": np.ascontiguousarray(rw_in)}


_PROGS = {}


def _prog(key, fn):
    if key not in _PROGS:
        _PROGS[key] = fn()
    return _PROGS[key]


def kernel(**inputs):
    P = {k: np.asarray(v, dtype=np.float32) for k, v in inputs.items()}
    x = P['x']
    NCORE = 8
    cores = [(b, c) for b in range(BATCH) for c in range(4)]
    xT = [np.ascontiguousarray(x[b, c * NTOK:(c + 1) * NTOK, :].T) for b, c in cores]
    consts = [mixer_consts(c) for c in range(4)]
    rope = rope_tables(SEQ)
    vfirst = [None] * NCORE
    yT = [None] * NCORE
    out = np.zeros((BATCH, SEQ, D_MODEL), np.float32)
    for l in range(DEPTH + 1):
        has_c, has_a, final = l > 0, l < DEPTH, l == DEPTH
        nc = _prog(("dense", has_c, has_a, final), lambda: build_dense(has_c, has_a, final))
        vecs = np.zeros((128, 64), np.float32)
        common = {"vecs": vecs}
        if has_a:
            vecs[:, 0:16] = vec_pk(P['norm_mix_w'][l])
            common["win"] = arrange_w(P['w_in_first'] if l == 0 else P['w_in_rest'][l - 1], GW, NG_IN)
        if has_c:
            vecs[:, 16:32] = vec_pk(P['norm_ffn_w'][l - 1])
            vecs[:, 32:48] = vec_pk(P['final_norm_w'])
            vecs[:, 48:56] = vec_pk(P['ssm_norm_w'][l - 1])
            common["wout"] = arrange_w(P['w_out'][l - 1], GW)
            common["wg"] = arrange_w(P['ffn_w_gate'][l - 1], GW)
            common["wu"] = arrange_w(P['ffn_w_up'][l - 1], GW)
            common["wd"] = arrange_wd(P['ffn_w_down'][l - 1])
        in_maps = []
        for i in range(NCORE):
            d = dict(common); d["xT"] = xT[i]
            if has_c:
                d["yT"] = yT[i]
            in_maps.append(d)
        res = run_bass_kernel_spmd(nc, in_maps, core_ids=list(range(NCORE))).results
        if has_c:
            xT = [np.asarray(res[i]["xo"]) for i in range(NCORE)]
        if final:
            break
        ncm = _prog(("mixer", l > 0), lambda: build_mixer(SEQ, l > 0))
        in_maps = []
        for b in range(BATCH):
            PJ = np.concatenate([np.asarray(res[b * 4 + c]["pj"]) for c in range(4)], axis=1)
            for c in range(4):
                cf, cm, cb, gc, zeta = consts[c]
                pp, rww = mixer_params(c, l, P, gc, zeta)
                d = mixer_inputs(c, PJ)
                d.update({"rope": rope, "pp": pp, "cf": cf, "cm": cm, "cb": cb, "rww": rww})
                if l > 0:
                    d["vfirst"] = vfirst[b * 4 + c]
                in_maps.append(d)
            del PJ
        res2 = run_bass_kernel_spmd(ncm, in_maps, core_ids=list(range(NCORE))).results
        for b in range(BATCH):
            Y = np.zeros((D_MODEL, SEQ), np.float32)
            for c in range(4):
                r = res2[b * 4 + c]
                Y[c * 128:(c + 1) * 128] = np.asarray(r["y_ret"])
                Y[512 + c * 256:512 + (c + 1) * 256] = np.asarray(r["y_ssd"]).reshape(256, SEQ)
                Y[1536 + c * 128:1536 + (c + 1) * 128] = np.asarray(r["y_rw"])
                if l == 0:
                    vfirst[b * 4 + c] = np.ascontiguousarray(np.asarray(r["v_out"]))
            for c in range(4):
                yT[b * 4 + c] = np.ascontiguousarray(Y[:, c * NTOK:(c + 1) * NTOK])
    for i, (b, c) in enumerate(cores):
        out[b, c * NTOK:(c + 1) * NTOK, :] = xT[i].T
    return out
```
